# Optimizing a Trainium2 kernel written in Bass

```python
import math
import jax, jax.numpy as jnp
from jax import lax
import numpy as np

D_MODEL = 1024
BATCH = 32
SEQ = 2048
DEPTH = 1

CHUNK = 64
Q_BLOCK = 128
HEAD_DIM = 64
ROPE_THETA = 10000.0
EPS = 1e-6
DIFF_HEADS = 4
DIFF_WIDTH = DIFF_HEADS * 2 * HEAD_DIM
DSA_HEADS = 8
DSA_WIDTH = DSA_HEADS * HEAD_DIM
IDX_HEADS = 8
DSA_TOPK_MAX = 256
N_BRANCH = 2
PEER_HEADS = 8
PEER_NKEYS = 128
PEER_EXPERTS = PEER_NKEYS * PEER_NKEYS
PEER_DKEY = 256
PEER_TOPK = 16
PEER_BLOCK = 128
IN_SIZES = (
    DIFF_WIDTH,
    DIFF_WIDTH,
    DIFF_WIDTH,
    DSA_WIDTH,
    HEAD_DIM,
    HEAD_DIM,
    IDX_HEADS * HEAD_DIM,
    HEAD_DIM,
    IDX_HEADS,
    N_BRANCH * D_MODEL,
)
IN_COLS = sum(IN_SIZES)

kernel_name = "chunk_causal_hybrid_diff_dsa_peer_block"


def rmsnorm(x, g):
    x32 = x.astype(jnp.float32)
    y = x32 * lax.rsqrt(jnp.mean(x32 * x32, axis=-1, keepdims=True) + EPS)
    return (y * g.astype(jnp.float32)).astype(x.dtype)


def rope_cos_sin(positions, dim):
    inv = ROPE_THETA ** (-(jnp.arange(0, dim, 2, dtype=jnp.float32) / dim))
    ang = positions.astype(jnp.float32)[..., None] * inv
    return jnp.cos(ang), jnp.sin(ang)


def apply_rope(x, cos, sin):
    x32 = x.astype(jnp.float32)
    half = x.shape[-1] // 2
    x1, x2 = x32[..., :half], x32[..., half:]
    c = cos[:, :, None, :]
    s = sin[:, :, None, :]
    return jnp.concatenate([x1 * c - x2 * s, x2 * c + x1 * s], axis=-1).astype(x.dtype)


def diff_attention(q, k, v, lam, out_g, lambda_init):
    B, T, H, _, d = q.shape
    nb = T // Q_BLOCK
    qb = q.reshape(B, nb, Q_BLOCK, H, 2, d).transpose(1, 0, 2, 3, 4, 5)
    k_chunk = jnp.arange(T) // CHUNK
    scale = d ** -0.5

    def block(args):
        i, q_i = args
        q_chunk = (i * Q_BLOCK + jnp.arange(Q_BLOCK)) // CHUNK
        mask = k_chunk[None, :] <= q_chunk[:, None]
        s = jnp.einsum('bqhmd,bkhmd->bhmqk', q_i, k).astype(jnp.float32) * scale
        s = jnp.where(mask, s, -jnp.inf)
        p = jax.nn.softmax(s, axis=-1)
        p = p[:, :, 0] - lam * p[:, :, 1]
        return jnp.einsum('bhqk,bkhd->bqhd', p.astype(v.dtype), v)

    o = lax.map(block, (jnp.arange(nb), qb))
    o = o.transpose(1, 0, 2, 3, 4).reshape(B, T, H, 2 * d)
    o = rmsnorm(o, out_g) * (1.0 - lambda_init)
    return o.reshape(B, T, H * 2 * d)


def dsa_attention(q, k, v, q_idx, k_idx, w_idx):
    B, T, H, d = q.shape
    topk = min(DSA_TOPK_MAX, T // 4)
    nb = T // Q_BLOCK
    to_blocks = lambda a: a.reshape((B, nb, Q_BLOCK) + a.shape[2:]).swapaxes(0, 1)
    k_chunk = jnp.arange(T) // CHUNK
    scale = d ** -0.5
    idx_scale = (IDX_HEADS ** -0.5) * (HEAD_DIM ** -0.5)
    gather = jax.vmap(lambda table, idx: table[idx])

    def block(args):
        i, q_b, qi_b, wi_b = args
        q_chunk = (i * Q_BLOCK + jnp.arange(Q_BLOCK)) // CHUNK
        mask = k_chunk[None, :] <= q_chunk[:, None]
        dots = jnp.einsum('bqhd,bkd->bqhk', qi_b, k_idx).astype(jnp.float32)
        score = jnp.einsum('bqhk,bqh->bqk', jax.nn.relu(dots), wi_b.astype(jnp.float32)) * idx_scale
        score = jnp.where(mask[None], score, -jnp.inf)
        _, sel = lax.top_k(score, topk)
        valid = (sel // CHUNK) <= q_chunk[None, :, None]
        k_sel = gather(k, sel)
        v_sel = gather(v, sel)
        s = jnp.einsum('bqhd,bqkd->bqhk', q_b, k_sel).astype(jnp.float32) * scale
        s = jnp.where(valid[:, :, None, :], s, -jnp.inf)
        p = jax.nn.softmax(s, axis=-1)
        o = jnp.einsum('bqhk,bqkd->bqhd', p.astype(v.dtype), v_sel)
        return o.reshape(B, Q_BLOCK, H * d)

    o = lax.map(block, (jnp.arange(nb), to_blocks(q), to_blocks(q_idx), to_blocks(w_idx)))
    return o.swapaxes(0, 1).reshape(B, T, H * d)


def peer(h, w_q, sub_keys, expert_u, expert_v):
    B, T, D = h.shape
    n_tok = B * T
    hb = h.reshape(n_tok // PEER_BLOCK, PEER_BLOCK, D)

    def block(xb):
        q = (xb @ w_q).reshape(PEER_BLOCK, PEER_HEADS, 2, PEER_DKEY // 2)
        s = jnp.einsum('nhpd,hpkd->nhpk', q, sub_keys).astype(jnp.float32)
        v1, i1 = lax.top_k(s[:, :, 0], PEER_TOPK)
        v2, i2 = lax.top_k(s[:, :, 1], PEER_TOPK)
        cand = (v1[..., :, None] + v2[..., None, :]).reshape(PEER_BLOCK, PEER_HEADS, PEER_TOPK * PEER_TOPK)
        cidx = (i1[..., :, None] * PEER_NKEYS + i2[..., None, :]).reshape(PEER_BLOCK, PEER_HEADS, PEER_TOPK * PEER_TOPK)
        sc, pos = lax.top_k(cand, PEER_TOPK)
        eidx = jnp.take_along_axis(cidx, pos, axis=-1)
        g = jax.nn.softmax(sc, axis=-1)
        u_sel = expert_u[eidx]
        a = jax.nn.gelu(jnp.einsum('nd,nhkd->nhk', xb, u_sel), approximate=False)
        wgt = (g * a.astype(jnp.float32)).astype(xb.dtype)
        return jnp.einsum('nhk,nhkd->nd', wgt, expert_v[eidx])

    return lax.map(block, hb).reshape(B, T, D)


def setup_inputs(seed: int = 0) -> dict:
    key = jax.random.key(seed)
    ks = jax.random.split(key, 24)
    f32 = jnp.float32
    L, D = DEPTH, D_MODEL

    def nrm(k, shape, scale):
        return jax.random.normal(k, shape, f32) * scale

    def gain(k, shape):
        return 1.0 + 0.02 * jax.random.normal(k, shape, f32)

    x = nrm(ks[0], (BATCH, SEQ, D), 1.0)
    c = nrm(ks[1], (BATCH, D), 1.0)
    offs = jax.random.randint(ks[2], (BATCH, 1), 0, 64, dtype=jnp.int32) * CHUNK
    positions = offs + jnp.arange(SEQ, dtype=jnp.int32)[None, :]
    return {
        'x': x,
        'c': c,
        'positions': positions,
        'w_ada': nrm(ks[3], (L, D, 6 * D), D ** -0.5),
        'b_ada': nrm(ks[4], (L, 6 * D), 0.01),
        'norm1_g': gain(ks[5], (L, D)),
        'w_in': nrm(ks[6], (L, D, IN_COLS), D ** -0.5),
        'diff_q_g': gain(ks[7], (L, HEAD_DIM)),
        'diff_k_g': gain(ks[8], (L, HEAD_DIM)),
        'diff_lam_q1': nrm(ks[9], (L, HEAD_DIM), 0.1),
        'diff_lam_k1': nrm(ks[10], (L, HEAD_DIM), 0.1),
        'diff_lam_q2': nrm(ks[11], (L, HEAD_DIM), 0.1),
        'diff_lam_k2': nrm(ks[12], (L, HEAD_DIM), 0.1),
        'diff_out_g': gain(ks[13], (L, 2 * HEAD_DIM)),
        'dsa_q_g': gain(ks[14], (L, HEAD_DIM)),
        'dsa_k_g': gain(ks[15], (L, HEAD_DIM)),
        'w_branch_a': nrm(ks[16], (L, DIFF_WIDTH, D), DIFF_WIDTH ** -0.5),
        'w_branch_b': nrm(ks[17], (L, DSA_WIDTH, D), DSA_WIDTH ** -0.5),
        'w_out': nrm(ks[18], (L, D, D), D ** -0.5),
        'norm2_g': gain(ks[19], (L, D)),
        'peer_w_q': nrm(ks[20], (L, D, PEER_HEADS * PEER_DKEY), D ** -0.5),
        'peer_sub_keys': nrm(ks[21], (L, PEER_HEADS, 2, PEER_NKEYS, PEER_DKEY // 2), (PEER_DKEY // 2) ** -0.5),
        'peer_u': nrm(ks[22], (L, PEER_EXPERTS, D), D ** -0.5),
        'peer_v': nrm(ks[23], (L, PEER_EXPERTS, D), PEER_HEADS ** -0.5),
    }


def reference(x, c, positions, w_ada, b_ada, norm1_g, w_in, diff_q_g, diff_k_g,
              diff_lam_q1, diff_lam_k1, diff_lam_q2, diff_lam_k2, diff_out_g,
              dsa_q_g, dsa_k_g, w_branch_a, w_branch_b, w_out, norm2_g,
              peer_w_q, peer_sub_keys, peer_u, peer_v):
    B, T, D = x.shape
    cos, sin = rope_cos_sin(positions, HEAD_DIM)
    offsets = np.cumsum(IN_SIZES)[:-1].tolist()
    for l in range(DEPTH):
        lambda_init = 0.8 - 0.6 * math.exp(-0.3 * l)
        mod = jax.nn.silu(c) @ w_ada[l] + b_ada[l]
        sh1, sc1, g1, sh2, sc2, g2 = jnp.split(mod, 6, axis=-1)

        h = rmsnorm(x, norm1_g[l]) * (1.0 + sc1[:, None, :]) + sh1[:, None, :]
        proj = h @ w_in[l]
        (dq, dk, dv, sq, sk, sv, iq, ik, iw, gates) = jnp.split(proj, offsets, axis=-1)

        dq = apply_rope(rmsnorm(dq.reshape(B, T, DIFF_HEADS * 2, HEAD_DIM), diff_q_g[l]), cos, sin)
        dk = apply_rope(rmsnorm(dk.reshape(B, T, DIFF_HEADS * 2, HEAD_DIM), diff_k_g[l]), cos, sin)
        lam = (jnp.exp(jnp.sum(diff_lam_q1[l] * diff_lam_k1[l]).astype(jnp.float32))
               - jnp.exp(jnp.sum(diff_lam_q2[l] * diff_lam_k2[l]).astype(jnp.float32))
               + lambda_init)
        o_a = diff_attention(dq.reshape(B, T, DIFF_HEADS, 2, HEAD_DIM),
                             dk.reshape(B, T, DIFF_HEADS, 2, HEAD_DIM),
                             dv.reshape(B, T, DIFF_HEADS, 2 * HEAD_DIM),
                             lam, diff_out_g[l], lambda_init)

        sq = apply_rope(rmsnorm(sq.reshape(B, T, DSA_HEADS, HEAD_DIM), dsa_q_g[l]), cos, sin)
        sk = apply_rope(rmsnorm(sk.reshape(B, T, 1, HEAD_DIM), dsa_k_g[l]), cos, sin)[:, :, 0]
        iq = apply_rope(iq.reshape(B, T, IDX_HEADS, HEAD_DIM), cos, sin)
        ik = apply_rope(ik.reshape(B, T, 1, HEAD_DIM), cos, sin)[:, :, 0]
        o_b = dsa_attention(sq, sk, sv, iq, ik, iw)

        gates = jax.nn.sigmoid(gates.reshape(B, T, N_BRANCH, D))
        merged = gates[:, :, 0] * (o_a @ w_branch_a[l]) + gates[:, :, 1] * (o_b @ w_branch_b[l])
        x = x + g1[:, None, :] * (merged @ w_out[l])

        h2 = rmsnorm(x, norm2_g[l]) * (1.0 + sc2[:, None, :]) + sh2[:, None, :]
        x = x + g2[:, None, :] * peer(h2, peer_w_q[l], peer_sub_keys[l], peer_u[l], peer_v[l])
    return x
```

```python
import numpy as np, math
from contextlib import ExitStack
import concourse.bass as bass
import concourse.mybir as mybir
from concourse.bass_utils import run_bass_kernel_spmd

F32 = mybir.dt.float32
BF16 = mybir.dt.bfloat16
I32 = mybir.dt.int32
U32 = mybir.dt.uint32
ALU = mybir.AluOpType
AF = mybir.ActivationFunctionType
AX = mybir.AxisListType


class Res:
    __slots__ = ("name", "w", "r", "dsem", "dcnt")

    def __init__(self, name):
        self.name = name
        self.w = []
        self.r = []
        self.dsem = None
        self.dcnt = 0


class Sched:
    ENGS = ("pe", "act", "dve", "pool", "sp")

    def __init__(self, nc, es):
        self.nc = nc
        self.es = es
        self.items = {e: [] for e in self.ENGS}
        self.cnt = {e: 0 for e in self.ENGS}
        self.sems = {}
        for e in self.ENGS:
            if e != "sp":
                self.sems[e] = es.enter_context(nc.semaphore("sem_" + e))
        self.known = {e: {} for e in self.ENGS}
        self.dstate = {}
        self.ninst = 0

    def _dstate(self, res):
        st = self.dstate.get(res.name)
        if st is None:
            sem = self.es.enter_context(self.nc.semaphore("dsem_%d_%s" % (len(self.dstate), res.name)))
            st = [sem, 0]
            self.dstate[res.name] = st
            self.sems[("d", res.name)] = sem
        return st

    def _collect(self, eng, reads, writes):
        deps = []
        for r in reads:
            deps += r.w
        for w in writes:
            deps += w.w
            deps += w.r
        out = {}
        for (k, v) in deps:
            if eng == "pe" and k == "pe":
                continue
            if self.known[eng].get(k, 0) >= v:
                continue
            if out.get(k, 0) < v:
                out[k] = v
        for k, v in out.items():
            self.known[eng][k] = v
        return list(out.items())

    def _update(self, dep, reads, writes):
        for r in reads:
            if r not in writes:
                r.r.append(dep)
        for w in writes:
            w.w = [dep]
            w.r = []

    def op(self, eng, fn, reads=(), writes=()):
        reads = list(reads); writes = list(writes)
        waits = self._collect(eng, reads, writes)
        self.cnt[eng] += 1
        dep = (eng, self.cnt[eng])
        self.items[eng].append((waits, fn, (eng, 1)))
        self._update(dep, reads, writes)
        self.ninst += 1

    def dma(self, fn, reads=(), writes=(), primary=None, eng="sp"):
        reads = list(reads); writes = list(writes)
        waits = self._collect(eng, reads, writes)
        st = self._dstate(primary)
        st[1] += 16
        key = ("d", primary.name)
        dep = (key, st[1])
        self.items[eng].append((waits, fn, (key, 16)))
        self._update(dep, reads, writes)
        self.ninst += 1

    def barrier(self):
        allv = []
        for e in self.ENGS:
            if e != "sp" and self.cnt[e] > 0:
                allv.append((e, self.cnt[e]))
        for nm, st in self.dstate.items():
            allv.append((("d", nm), st[1]))
        for e in self.ENGS:
            waits = []
            for (k, v) in allv:
                if self.known[e].get(k, 0) < v:
                    waits.append((k, v))
                    self.known[e][k] = v
            if waits:
                self.items[e].append((waits, None, None))

    def flush(self, name=None):
        nc = self.nc
        items = self.items
        sems = self.sems

        def replay(engname):
            def run(eng):
                for (waits, fn, inc) in items[engname]:
                    for (k, v) in waits:
                        eng.wait_ge(sems[k], v)
                    if fn is not None:
                        inst = fn(eng)
                        inst.then_inc(sems[inc[0]], inc[1])
            return run

        with nc.Block() as block:
            if items["sp"]:
                block.sync(replay("sp"))
            if items["act"]:
                block.scalar(replay("act"))
            if items["dve"]:
                block.vector(replay("dve"))
            if items["pool"]:
                block.gpsimd(replay("pool"))
            if items["pe"]:
                block.tensor(replay("pe"))
        self.items = {e: [] for e in self.ENGS}
NS_FULL = 4
T_SEQ = 2048
NT = 16
DM = 1024
IN_COLS = 4808
NPROJ = 2760
GATE0 = 2760
EPS = 1e-6
LAMBDA_INIT = 0.8 - 0.6 * math.exp(-0.0)
NEG = -1.0e30
TWO_PI = 2.0 * math.pi


class T_:
    __slots__ = ("t", "r")

    def __init__(self, t, name):
        self.t = t
        self.r = Res(name)

    def __getitem__(self, k):
        return self.t[k]


class Alloc:
    def __init__(self, nc, es, prefix):
        self.nc, self.es, self.p = nc, es, prefix
        self.n = 0

    def sb(self, name, shape, dt):
        self.n += 1
        return T_(self.es.enter_context(self.nc.sbuf_tensor("%s_%s_%d" % (self.p, name, self.n), shape, dt)), name)

    def ps(self, name, shape, dt):
        self.n += 1
        return T_(self.es.enter_context(self.nc.psum_tensor("%s_%s_%d" % (self.p, name, self.n), shape, dt)), name)


def _rs(lst):
    return [x.r if isinstance(x, T_) else x for x in lst]


class K:
    def __init__(self, nc, S, ns):
        self.nc, self.S, self.ns = nc, S, ns

    def op(self, eng, fn, reads=(), writes=()):
        self.S.op(eng, fn, _rs(reads), _rs(writes))

    def dma(self, fn, reads=(), writes=(), primary=None, eng="sp"):
        self.S.dma(fn, _rs(reads), _rs(writes), primary.r if isinstance(primary, T_) else primary, eng)

def build(ns, phases=(0, 1, 2, 3), dbg=False, stages="ABC"):
    nc = bass.Bass("TRN2", target_bir_lowering=False)
    NTOK = ns * T_SEQ

    def din(name, shape, dt=F32):
        return nc.dram_tensor(name, shape, dt, kind="ExternalInput").ap()

    def dscr(name, shape, dt):
        return nc.dram_tensor(name, shape, dt, kind=("ExternalOutput" if dbg else "Internal")).ap()

    x = din("x", [NTOK, DM]); c_in = din("c", [ns, DM]); pos = din("pos", [ns, 128, NT], I32); inv = din("inv", [1, 32])
    w_ada = din("w_ada", [DM, 6 * DM]); b_ada = din("b_ada", [1, 6 * DM]); n1g = din("norm1_g", [1, DM]); w_in = din("w_in", [DM, IN_COLS])
    gains_in = [din(n, [1, 64]) for n in ("diff_q_g", "diff_k_g", "dsa_q_g", "dsa_k_g")]
    lam_in = [din(n, [1, 64]) for n in ("diff_lam_q1", "diff_lam_k1", "diff_lam_q2", "diff_lam_k2")]
    og_in = din("diff_out_g", [128, 1])
    w_ba = din("w_branch_a", [512, DM]); w_bb = din("w_branch_b", [512, DM]); w_o = din("w_out", [DM, DM]); n2g = din("norm2_g", [1, DM])
    w_q = din("peer_w_q", [DM, 2048]); subkT = din("subkT", [128, 16, 128]); pu = din("peer_u", [16384, DM]); pv = din("peer_v", [16384, DM])
    out = nc.dram_tensor("out", [NTOK, DM], F32, kind="ExternalOutput").ap()

    uv_bf = dscr("uv_bf", [16384, 2 * DM], BF16)
    rows_d = dscr("rows_d", [ns, 6, DM], F32)
    qT_s = dscr("qT_s", [ns, 64, 34, T_SEQ], BF16)
    dv_s = dscr("dv_s", [ns, T_SEQ, 576], BF16)
    iw_s = dscr("iw_s", [ns, T_SEQ, 8], F32)
    gT_s = dscr("gT_s", [ns, 128, 16, T_SEQ], BF16)

    ges = ExitStack()
    with ges:
        S = Sched(nc, ges)
        kk = K(nc, S, ns)
        op, dma = kk.op, kk.dma
        GA = Alloc(nc, ges, "g")
        ident_f = GA.sb("ident_f", [128, 128], F32); ident_b = GA.sb("ident_b", [128, 128], BF16)
        ones_b = GA.sb("ones_b", [128, 128], BF16)
        cols = GA.sb("cols", [128, 4, 8, ns], F32)
        neglam = GA.sb("neglam", [128, 1], F32); ogcol = GA.sb("ogcol", [128, 1], F32)
        gains_b = GA.sb("gains_b", [128, 4, 64], F32); inv_b = GA.sb("inv_b", [128, 32], F32)
        iota16 = GA.sb("iota16", [128, 16], F32)

        with ExitStack() as es0:
            A = Alloc(nc, es0, "p0")
            if 3 in phases:
                R_uv = Res("uvcast")
                for (src, c0) in ((pu, 0), (pv, DM)):
                    for i in range(16):
                        dma(lambda e, src=src, c0=c0, i=i: e.dma_start(out=uv_bf[i * 1024:(i + 1) * 1024, c0:c0 + DM], in_=src[i * 1024:(i + 1) * 1024, :]), [], [], R_uv, eng="pool")
            op("pool", lambda e: e.memset(ident_f[:], 0.0), [], [ident_f])
            op("pool", lambda e: e.affine_select(out=ident_f[:], in_=ident_f[:], compare_op=ALU.not_equal, fill=1.0, base=0, pattern=[[-1, 128]], channel_multiplier=1), [ident_f], [ident_f])
            op("dve", lambda e: e.tensor_copy(out=ident_b[:], in_=ident_f[:]), [ident_f], [ident_b])
            op("dve", lambda e: e.memset(ones_b[:], 1.0), [], [ones_b])
            op("pool", lambda e: e.iota(iota16[:], pattern=[[1, 16]], base=0, channel_multiplier=0, allow_small_or_imprecise_dtypes=True), [], [iota16])
            for i in range(4):
                dma(lambda e, i=i: e.dma_start(out=gains_b[:, i, :], in_=gains_in[i].partition_broadcast(128)), [], [gains_b], gains_b)
            dma(lambda e: e.dma_start(out=inv_b[:], in_=inv.partition_broadcast(128)), [], [inv_b], inv_b)
            dma(lambda e: e.dma_start(out=ogcol[:], in_=og_in), [], [ogcol], ogcol)
            op("dve", lambda e: e.tensor_scalar(out=ogcol[:], in0=ogcol[:], scalar1=1.0 - LAMBDA_INIT, scalar2=None, op0=ALU.mult), [ogcol], [ogcol])
            lamv = A.sb("lamv", [128, 4, 64], F32); lamp = A.sb("lamp", [128, 2, 64], F32); lams = A.sb("lams", [128, 2], F32)
            for i in range(4):
                dma(lambda e, i=i: e.dma_start(out=lamv[:, i, :], in_=lam_in[i].partition_broadcast(128)), [], [lamv], lamv)
            lv4 = lamv[:, :, :].rearrange("p (a b) d -> p a b d", b=2)
            op("dve", lambda e: e.tensor_tensor(out=lamp[:], in0=lv4[:, :, 0, :], in1=lv4[:, :, 1, :], op=ALU.mult), [lamv], [lamp])
            op("dve", lambda e: e.tensor_reduce(out=lams[:], in_=lamp[:], axis=AX.X, op=ALU.add), [lamp], [lams])
            op("act", lambda e: e.activation(out=lams[:], in_=lams[:], func=AF.Exp), [lams], [lams])
            op("dve", lambda e: e.tensor_tensor(out=neglam[:], in0=lams[:, 1:2], in1=lams[:, 0:1], op=ALU.subtract), [lams], [neglam])
            op("dve", lambda e: e.tensor_scalar(out=neglam[:], in0=neglam[:], scalar1=-LAMBDA_INIT, scalar2=None, op0=ALU.add), [neglam], [neglam])

            c_sb = A.sb("c_sb", [ns, DM], F32); sg = A.sb("sg", [ns, DM], F32); scT = A.sb("scT", [128, 8, ns], F32)
            pc0 = A.ps("pc0", [128, 8, ns], F32)
            dma(lambda e: e.dma_start(out=c_sb[:], in_=c_in), [], [c_sb], c_sb)
            op("act", lambda e: e.activation(out=sg[:], in_=c_sb[:], func=AF.Sigmoid), [c_sb], [sg])
            op("dve", lambda e: e.tensor_tensor(out=c_sb[:], in0=c_sb[:], in1=sg[:], op=ALU.mult), [c_sb, sg], [c_sb])
            for kc in range(8):
                op("pe", lambda e, kc=kc: e.transpose(out=pc0[:, kc, :], in_=c_sb[0:ns, kc * 128:(kc + 1) * 128], identity=ident_f[0:ns, 0:ns]), [c_sb, ident_f], [pc0])
            op("dve", lambda e: e.tensor_copy(out=scT[:], in_=pc0[:]), [pc0], [scT])
            wst = [A.sb("wst%d" % i, [128, 8, 512], F32) for i in range(2)]
            b_b = A.sb("b_b", [ns, 6 * DM], F32); mod_sb = A.sb("mod_sb", [ns, 6 * DM], F32)
            pm = [A.ps("pm%d" % i, [ns, 512], F32) for i in range(2)]
            dma(lambda e: e.dma_start(out=b_b[:], in_=b_ada.partition_broadcast(ns)), [], [b_b], b_b)
            w_ada_v = w_ada.rearrange("(kc p) n -> p kc n", p=128)
            for j in range(12):
                ws = wst[j % 2]; pmj = pm[j % 2]
                dma(lambda e, ws=ws, j=j: e.dma_start(out=ws[:], in_=w_ada_v[:, :, j * 512:(j + 1) * 512]), [], [ws], ws)
                for kc in range(8):
                    op("pe", lambda e, ws=ws, pmj=pmj, kc=kc: e.matmul(pmj[:], lhsT=scT[:, kc, :], rhs=ws[:, kc, :], start=(kc == 0), stop=(kc == 7)), [scT, ws], [pmj])
                op("dve", lambda e, pmj=pmj, j=j: e.tensor_tensor(out=mod_sb[:, j * 512:(j + 1) * 512], in0=pmj[:], in1=b_b[:, j * 512:(j + 1) * 512], op=ALU.add), [pmj, b_b], [mod_sb])
            rows_sb = A.sb("rows_sb", [ns, 6, DM], F32); ng_b = A.sb("ng_b", [ns, 2, DM], F32)
            dma(lambda e: e.dma_start(out=ng_b[:, 0, :], in_=n1g.partition_broadcast(ns)), [], [ng_b], ng_b)
            dma(lambda e: e.dma_start(out=ng_b[:, 1, :], in_=n2g.partition_broadcast(ns)), [], [ng_b], ng_b)
            for half in range(2):
                o = half * 3 * DM
                op("dve", lambda e, o=o, half=half: e.scalar_tensor_tensor(out=rows_sb[:, half * 3 + 0, :], in0=mod_sb[:, o + DM:o + 2 * DM], scalar=1.0, in1=ng_b[:, half, :], op0=ALU.add, op1=ALU.mult), [mod_sb, ng_b], [rows_sb])
                op("dve", lambda e, o=o, half=half: e.tensor_copy(out=rows_sb[:, half * 3 + 1, :], in_=mod_sb[:, o:o + DM]), [mod_sb], [rows_sb])
                op("dve", lambda e, o=o, half=half: e.tensor_copy(out=rows_sb[:, half * 3 + 2, :], in_=mod_sb[:, o + 2 * DM:o + 3 * DM]), [mod_sb], [rows_sb])
            dma(lambda e: e.dma_start(out=rows_d, in_=rows_sb[:]), [rows_sb], [], rows_sb)
            pc1 = A.ps("pc1", [128, 4, 8, ns], F32)
            for wi, ri in enumerate((0, 1, 3, 4)):
                for kc in range(8):
                    op("pe", lambda e, wi=wi, ri=ri, kc=kc: e.transpose(out=pc1[:, wi, kc, :], in_=rows_sb[0:ns, ri, kc * 128:(kc + 1) * 128], identity=ident_f[0:ns, 0:ns]), [rows_sb, ident_f], [pc1])
            op("dve", lambda e: e.tensor_copy(out=cols[:], in_=pc1[:]), [pc1], [cols])

            S.barrier()
            S.flush()

        def rope_alloc(A):
            return (A.sb("posi", [128, NT], I32), A.sb("posf", [128, NT], F32), A.sb("ang", [128, NT, 32], F32),
                    A.sb("a2", [128, NT, 32], F32), A.sb("ki", [128, NT, 32], I32), A.sb("kf", [128, NT, 32], F32))

        def rope_tables(rb, s, cos_t, sin_t):
            posi, posf, ang, a2, ki, kf = rb
            dma(lambda e: e.dma_start(out=posi[:], in_=pos[s]), [], [posi], posi)
            op("dve", lambda e: e.tensor_copy(out=posf[:], in_=posi[:]), [posi], [posf])
            op("dve", lambda e: e.tensor_tensor(out=ang[:], in0=posf[:, :].unsqueeze(2).to_broadcast([128, NT, 32]), in1=inv_b[:, :].unsqueeze(1).to_broadcast([128, NT, 32]), op=ALU.mult), [posf, inv_b], [ang])
            for (shift, dst) in ((0.0, sin_t), (0.5 * math.pi, cos_t)):
                op("dve", lambda e, shift=shift: e.tensor_scalar(out=a2[:], in0=ang[:], scalar1=shift, scalar2=None, op0=ALU.add), [ang], [a2])
                op("dve", lambda e: e.tensor_scalar(out=ki[:], in0=a2[:], scalar1=1.0 / TWO_PI, scalar2=None, op0=ALU.mult), [a2], [ki])
                op("dve", lambda e: e.tensor_copy(out=kf[:], in_=ki[:]), [ki], [kf])
                op("dve", lambda e: e.scalar_tensor_tensor(out=a2[:], in0=kf[:], scalar=-TWO_PI, in1=a2[:], op0=ALU.mult, op1=ALU.add), [kf, a2], [a2])
                op("dve", lambda e: e.tensor_scalar(out=kf[:], in0=a2[:], scalar1=math.pi, scalar2=TWO_PI, op0=ALU.is_gt, op1=ALU.mult), [a2], [kf])
                op("dve", lambda e: e.tensor_tensor(out=a2[:], in0=a2[:], in1=kf[:], op=ALU.subtract), [a2, kf], [a2])
                op("dve", lambda e: e.tensor_scalar(out=kf[:], in0=a2[:], scalar1=-math.pi, scalar2=TWO_PI, op0=ALU.is_lt, op1=ALU.mult), [a2], [kf])
                op("dve", lambda e: e.tensor_tensor(out=a2[:], in0=a2[:], in1=kf[:], op=ALU.add), [a2, kf], [a2])
                op("act", lambda e, dst=dst: e.activation(out=dst[:], in_=a2[:], func=AF.Sin), [a2], [dst])

        def rms_rstd(A, ssq_ap, n, res, nm):
            op("dve", lambda e: e.tensor_scalar(out=ssq_ap, in0=ssq_ap, scalar1=1.0 / n, scalar2=EPS, op0=ALU.mult, op1=ALU.add), [res], [res])
            op("act", lambda e: e.activation(out=ssq_ap, in_=ssq_ap, func=AF.Sqrt), [res], [res])
            op("dve", lambda e: e.reciprocal(out=ssq_ap, in_=ssq_ap), [res], [res])

        if 1 in phases:
          with ExitStack() as es1:
            A = Alloc(nc, es1, "p1")
            w_in_bf = A.sb("w_in_bf", [128, 8, IN_COLS], BF16)
            w_in_v = w_in.rearrange("(kc p) n -> p kc n", p=128)
            nch = (IN_COLS + 511) // 512
            for j in range(nch):
                lo = j * 512; hi = min(IN_COLS, lo + 512)
                dma(lambda e, lo=lo, hi=hi: e.dma_start(out=w_in_bf[:, :, lo:hi], in_=w_in_v[:, :, lo:hi]), [], [w_in_bf], w_in_bf, eng="pool")
            cos_tr = [A.sb("cos_t%d" % i, [128, NT, 32], F32) for i in range(2)]; sin_tr = [A.sb("sin_t%d" % i, [128, NT, 32], F32) for i in range(2)]
            xt = [A.sb("xt%d" % i, [128, DM], F32) for i in range(2)]
            junk = A.sb("junk", [128, DM], BF16); xnr = [A.sb("xn%d" % i, [128, DM], BF16) for i in range(2)]
            ssq = [A.sb("ssq%d" % i, [128, 1], F32) for i in range(2)]
            hT = [A.sb("hT%d" % i, [128, 8, 128], BF16) for i in range(2)]
            projr = [A.sb("proj%d" % i, [128, NPROJ], F32) for i in range(2)]
            sqbr = [A.sb("sqb%d" % i, [128, 1600], F32) for i in range(2)]; ssqgr = [A.sb("ssqg%d" % i, [128, 25], F32) for i in range(2)]
            rqr = [A.sb("rq%d" % i, [128, 34, 64], BF16) for i in range(2)]
            tv = [A.sb("tv%d" % i, [128, 16, 32], F32) for i in range(2)]
            tp = [A.sb("tp%d" % i, [128, 9, 32], F32) for i in range(2)]
            dvb = [A.sb("dvb%d" % i, [128, 576], BF16) for i in range(2)]
            iwb = [A.sb("iwb%d" % i, [128, 8], F32) for i in range(2)]
            gs = [A.sb("gs%d" % i, [128, 16, 128], BF16) for i in range(2)]
            qTs = [A.sb("qTs%d" % i, [64, 34, 128], BF16) for i in range(2)]
            pTr = [A.ps("pT%d" % i, [128, 8, 128], BF16) for i in range(2)]
            pp = [A.ps("pp%d" % i, [128, 512], F32) for i in range(2)]
            pg = [A.ps("pg%d" % i, [128, 4, 128], F32) for i in range(2)]
            pq = [A.ps("pq%d" % i, [64, 8, 128], BF16) for i in range(2)]
            bounds = [0, 512, 1024, 1536, 2048, 2560, NPROJ]
            rb = rope_alloc(A)
            def p1_tile(s, t, gi):
                i2 = gi % 2
                r0 = s * T_SEQ + t * 128
                xti = xt[i2]; ssi = ssq[i2]; hTi = hT[i2]
                xn = xnr[i2]; proj = projr[i2]; sqb = sqbr[i2]; ssqg = ssqgr[i2]; rq = rqr[i2]; pT = pTr[i2]
                cos_t = cos_tr[s % 2]; sin_t = sin_tr[s % 2]
                dma(lambda e, xti=xti, r0=r0: e.dma_start(out=xti[:], in_=x[r0:r0 + 128, :]), [], [xti], xti)
                op("act", lambda e, xti=xti, ssi=ssi: e.activation(out=junk[:], in_=xti[:], func=AF.Square, accum_out=ssi[:, 0:1]), [xti], [junk, ssi])
                rms_rstd(A, ssi[:, 0:1], float(DM), ssi, "x")
                op("act", lambda e, xti=xti, ssi=ssi: e.activation(out=xn[:], in_=xti[:], func=AF.Copy, scale=ssi[:, 0:1]), [xti, ssi], [xn])
                yield
                for kc in range(8):
                    op("pe", lambda e, kc=kc: e.transpose(out=pT[:, kc, :], in_=xn[:, kc * 128:(kc + 1) * 128], identity=ident_b[:]), [xn, ident_b], [pT])
                for kc in range(8):
                    op("dve", lambda e, kc=kc, hTi=hTi, s=s: e.tensor_scalar(out=hTi[:, kc, :], in0=pT[:, kc, :], scalar1=cols[:, 0, kc, s:s + 1], scalar2=cols[:, 1, kc, s:s + 1], op0=ALU.mult, op1=ALU.add), [pT, cols], [hTi])
                yield
                for j in range(6):
                    lo, hi = bounds[j], bounds[j + 1]; w = hi - lo
                    ppj = pp[j % 2]
                    for kc in range(8):
                        op("pe", lambda e, ppj=ppj, kc=kc, lo=lo, hi=hi, w=w, hTi=hTi: e.matmul(ppj[:, 0:w], lhsT=hTi[:, kc, :], rhs=w_in_bf[:, kc, lo:hi], start=(kc == 0), stop=(kc == 7)), [hTi, w_in_bf], [ppj])
                    op("act", lambda e, ppj=ppj, lo=lo, hi=hi, w=w: e.activation(out=proj[:, lo:hi], in_=ppj[:, 0:w], func=AF.Copy), [ppj], [proj])
                    if j == 2 or j == 5:
                        yield
                gsi = gs[i2]

                def gates_half(ra, rb_):
                    for r in range(ra, rb_):
                        pgr = pg[r % 2]
                        for j in range(4):
                            gc = r * 4 + j
                            for kc in range(8):
                                op("pe", lambda e, pgr=pgr, j=j, gc=gc, kc=kc: e.matmul(pgr[:, j, :], lhsT=w_in_bf[:, kc, GATE0 + gc * 128:GATE0 + (gc + 1) * 128], rhs=hTi[:, kc, :], start=(kc == 0), stop=(kc == 7)), [hTi, w_in_bf], [pgr])
                        op("act", lambda e, pgr=pgr, r=r: e.activation(out=gsi[:, r * 4:(r + 1) * 4, :], in_=pgr[:], func=AF.Sigmoid), [pgr], [gsi])
                gates_half(0, 2)
                yield
                dvi = dvb[i2]; iwi = iwb[i2]
                op("pool", lambda e, dvi=dvi: e.tensor_copy(out=dvi[:, 0:512], in_=proj[:, 1024:1536]), [proj], [dvi])
                op("pool", lambda e, dvi=dvi: e.tensor_copy(out=dvi[:, 512:576], in_=proj[:, 2112:2176]), [proj], [dvi])
                op("pool", lambda e, iwi=iwi: e.tensor_copy(out=iwi[:], in_=proj[:, 2752:2760]), [proj], [iwi])
                dma(lambda e, dvi=dvi, s=s, t=t: e.dma_start(out=dv_s[s][t * 128:(t + 1) * 128, :], in_=dvi[:]), [dvi], [], dvi)
                dma(lambda e, iwi=iwi, s=s, t=t: e.dma_start(out=iw_s[s][t * 128:(t + 1) * 128, :], in_=iwi[:]), [iwi], [], iwi)
                op("pool", lambda e: e.tensor_tensor(out=sqb[:, 0:1024], in0=proj[:, 0:1024], in1=proj[:, 0:1024], op=ALU.mult), [proj], [sqb])
                op("pool", lambda e: e.tensor_tensor(out=sqb[:, 1024:1600], in0=proj[:, 1536:2112], in1=proj[:, 1536:2112], op=ALU.mult), [proj], [sqb])
                op("dve", lambda e: e.tensor_reduce(out=ssqg[:, 0:25], in_=sqb[:, :].rearrange("p (g d) -> p g d", d=64), axis=AX.X, op=ALU.add), [sqb], [ssqg])
                rms_rstd(A, ssqg[:, :], 64.0, ssqg, "g")
                pA = proj[:, 0:1024].rearrange("p (g d) -> p g d", d=64)
                pB = proj[:, 1536:2112].rearrange("p (g d) -> p g d", d=64)
                op("dve", lambda e, pA=pA: e.tensor_tensor(out=pA, in0=pA, in1=ssqg[:, 0:16].unsqueeze(2).to_broadcast([128, 16, 64]), op=ALU.mult), [proj, ssqg], [proj])
                op("dve", lambda e, pB=pB: e.tensor_tensor(out=pB, in0=pB, in1=ssqg[:, 16:25].unsqueeze(2).to_broadcast([128, 9, 64]), op=ALU.mult), [proj, ssqg], [proj])
                op("dve", lambda e, pA=pA: e.tensor_tensor(out=pA[:, 0:8, :], in0=pA[:, 0:8, :], in1=gains_b[:, 0, :].unsqueeze(1).to_broadcast([128, 8, 64]), op=ALU.mult), [proj, gains_b], [proj])
                op("dve", lambda e, pA=pA: e.tensor_tensor(out=pA[:, 8:16, :], in0=pA[:, 8:16, :], in1=gains_b[:, 1, :].unsqueeze(1).to_broadcast([128, 8, 64]), op=ALU.mult), [proj, gains_b], [proj])
                op("dve", lambda e, pB=pB: e.tensor_tensor(out=pB[:, 0:8, :], in0=pB[:, 0:8, :], in1=gains_b[:, 2, :].unsqueeze(1).to_broadcast([128, 8, 64]), op=ALU.mult), [proj, gains_b], [proj])
                op("dve", lambda e, pB=pB: e.tensor_tensor(out=pB[:, 8:9, :], in0=pB[:, 8:9, :], in1=gains_b[:, 3, :].unsqueeze(1).to_broadcast([128, 1, 64]), op=ALU.mult), [proj, gains_b], [proj])
                yield
                gates_half(2, 4)
                dma(lambda e: e.dma_start(out=gT_s[s][:, :, t * 128:(t + 1) * 128], in_=gsi[:]), [gsi], [], gsi)
                yield
                for (lo, G, g0, eng, tmps) in ((0, 16, 0, "dve", tv), (1536, 9, 16, "dve", tv), (2176, 9, 25, "pool", tp)):
                    X = proj[:, lo:lo + G * 64].rearrange("p (g h d) -> p g h d", h=2, d=32)
                    O = rq[:, g0:g0 + G, :].rearrange("p g (h d) -> p g h d", h=2)
                    cb = cos_t[:, t, :].unsqueeze(1).to_broadcast([128, G, 32])
                    sb_ = sin_t[:, t, :].unsqueeze(1).to_broadcast([128, G, 32])
                    t1 = tmps[0][:, 0:G, :]; t2 = tmps[1][:, 0:G, :]
                    rr = [proj, cos_t, sin_t]
                    op(eng, lambda e, X=X, cb=cb, t1=t1: e.tensor_tensor(out=t1, in0=X[:, :, 0, :], in1=cb, op=ALU.mult), rr, [tmps[0]])
                    op(eng, lambda e, X=X, sb_=sb_, t2=t2: e.tensor_tensor(out=t2, in0=X[:, :, 1, :], in1=sb_, op=ALU.mult), rr, [tmps[1]])
                    op(eng, lambda e, O=O, t1=t1, t2=t2: e.tensor_tensor(out=O[:, :, 0, :], in0=t1, in1=t2, op=ALU.subtract), [tmps[0], tmps[1]], [rq])
                    op(eng, lambda e, X=X, cb=cb, t1=t1: e.tensor_tensor(out=t1, in0=X[:, :, 1, :], in1=cb, op=ALU.mult), rr, [tmps[0]])
                    op(eng, lambda e, X=X, sb_=sb_, t2=t2: e.tensor_tensor(out=t2, in0=X[:, :, 0, :], in1=sb_, op=ALU.mult), rr, [tmps[1]])
                    op(eng, lambda e, O=O, t1=t1, t2=t2: e.tensor_tensor(out=O[:, :, 1, :], in0=t1, in1=t2, op=ALU.add), [tmps[0], tmps[1]], [rq])
                yield
                qTi = qTs[i2]
                for bi, (g0, g1) in enumerate(((0, 8), (8, 16), (16, 24), (24, 32), (32, 34))):
                    pqb = pq[bi % 2]
                    for g in range(g0, g1):
                        op("pe", lambda e, pqb=pqb, g=g, g0=g0: e.transpose(out=pqb[:, g - g0, :], in_=rq[:, g, :], identity=ident_b[:]), [rq, ident_b], [pqb])
                    op("act", lambda e, pqb=pqb, g0=g0, g1=g1, qTi=qTi: e.activation(out=qTi[:, g0:g1, :], in_=pqb[:, 0:g1 - g0, :], func=AF.Copy), [pqb], [qTi])
                dma(lambda e, qTi=qTi, s=s, t=t: e.dma_start(out=qT_s[s][:, :, t * 128:(t + 1) * 128], in_=qTi[:]), [qTi], [], qTi)

            tl = [(s_, t_) for s_ in range(ns) for t_ in range(NT)]
            gens = {}

            def pull(i):
                if i < 0 or i >= len(tl):
                    return
                if i not in gens:
                    s_, t_ = tl[i]
                    if t_ == 0:
                        rope_tables(rb, s_, cos_tr[s_ % 2], sin_tr[s_ % 2])
                    gens[i] = p1_tile(s_, t_, i)
                next(gens[i], None)

            pull(0); pull(0); pull(1)
            for i in range(len(tl) + 1):
                pull(i)
                pull(i - 1)
                pull(i)
                pull(i + 1)
                pull(i)
                pull(i)
                pull(i)
                pull(i)
                pull(i + 2)
            S.barrier()
            S.flush()

        if 2 in phases:
          with ExitStack() as es2:
            P2 = Alloc(nc, es2, "p2")
            wba = P2.sb("wba", [128, 4, DM], BF16); wbb = P2.sb("wbb", [128, 4, DM], BF16); wo = P2.sb("wo", [128, 8, DM], BF16)
            oaT = P2.sb("oaT", [128, 4, T_SEQ], BF16); obT = P2.sb("obT", [128, 4, T_SEQ], BF16)
            NMT = NT * (NT + 1) // 2
            maskT_all = P2.sb("maskT_all", [128, NMT, 128], BF16)
            moff = [t * (t + 1) // 2 for t in range(NT)]
            for (src, dst) in ((w_ba, wba), (w_bb, wbb), (w_o, wo)):
                sv_ = src.rearrange("(j p) n -> p j n", p=128)
                for j0 in range(0, sv_.shape[1], 2):
                    dma(lambda e, sv_=sv_, dst=dst, j0=j0: e.dma_start(out=dst[:, j0:j0 + 2, :], in_=sv_[:, j0:j0 + 2, :]), [], [dst], dst, eng="pool")
            for s in range(ns):
                with ExitStack() as esa:
                  if 'A' in stages:
                      A = Alloc(nc, esa, "p2a%d" % s)
                      dvs = A.sb("dvs", [128, NT, 512], BF16)
                      dkq = [A.sb("dkq%d" % i, [64, 2, T_SEQ], BF16) for i in range(2)]
                      PT = [A.sb("PT%d" % i, [128, 512], BF16) for i in range(3)]
                      o0n = A.sb("o0n", [128, T_SEQ], F32)
                      rz = A.sb("rz", [128, 512], F32); o1n = A.sb("o1n", [128, 512], F32); dd = A.sb("dd", [128, 512], F32)
                      dsq = A.sb("dsq", [128, 512], BF16); rstd = A.sb("rstd", [128, 512], F32)
                      ps_s = [A.ps("ps_s%d" % i, [128, 512], F32) for i in range(2)]
                      ps_o = [A.ps("ps_o%d" % i, [128, 512], F32) for i in range(2)]; ps_z = [A.ps("ps_z%d" % i, [128, 512], F32) for i in range(2)]
                      ikT = A.sb("ikT", [64, T_SEQ], BF16); iws = A.sb("iws", [128, NT, 8], F32)
                      iqT = [A.sb("iqT%d" % i, [64, 8, 128], BF16) for i in range(2)]
                      score = A.sb("score", [128, T_SEQ], F32)
                      rl = [A.sb("rl%d" % i, [128, 512], F32) for i in range(2)]
                      blo = A.sb("blo", [128, 1], F32); bd0 = A.sb("bd0", [128, 1], F32); bmid = A.sb("bmid", [128, 1], F32)
                      bcnt = A.sb("bcnt", [128, 1], F32); bg = A.sb("bg", [128, 1], F32); bjunk = A.sb("bjunk", [128, T_SEQ], BF16)
                      maskb = A.sb("maskb", [128, T_SEQ], BF16)
                      ps_i = A.ps("ps_i", [128, 512], F32); ps_m = A.ps("ps_m", [128, 8, 128], BF16)
                      dma(lambda e: e.dma_start(out=ikT[:], in_=qT_s[s][:, 33, :]), [], [ikT], ikT)
                      dma(lambda e: e.dma_start(out=iws[:], in_=iw_s[s].rearrange("(t p) c -> p t c", p=128)), [], [iws], iws)

                      def b_part1_gen():
                          ii = 0
                          for t in range(NT):
                              iq_ = iqT[t % 2]
                              tsl = slice(t * 128, (t + 1) * 128)
                              dma(lambda e, iq_=iq_, tsl=tsl: e.dma_start(out=iq_[:], in_=qT_s[s][:, 25:33, tsl]), [], [iq_], iq_)
                              nk = (t + 1) * 128
                              for kc in range((nk + 511) // 512):
                                  w = min(512, nk - kc * 512)
                                  ksl = slice(kc * 512, kc * 512 + w)
                                  for hh in range(8):
                                      rli = rl[ii % 2]; ii += 1
                                      op("pe", lambda e, iq_=iq_, hh=hh, ksl=ksl, w=w: e.matmul(ps_i[:, 0:w], lhsT=iq_[:, hh, :], rhs=ikT[:, ksl], start=True, stop=True), [iq_, ikT], [ps_i])
                                      op("act", lambda e, rli=rli, w=w: e.activation(out=rli[:, 0:w], in_=ps_i[:, 0:w], func=AF.Relu), [ps_i], [rli])
                                      if hh == 0:
                                          op("dve", lambda e, rli=rli, ksl=ksl, w=w, hh=hh, t=t: e.tensor_scalar(out=score[:, ksl], in0=rli[:, 0:w], scalar1=iws[:, t, hh:hh + 1], scalar2=None, op0=ALU.mult), [rli, iws], [score])
                                      else:
                                          op("dve", lambda e, rli=rli, ksl=ksl, w=w, hh=hh, t=t: e.scalar_tensor_tensor(out=score[:, ksl], in0=rli[:, 0:w], scalar=iws[:, t, hh:hh + 1], in1=score[:, ksl], op0=ALU.mult, op1=ALU.add), [rli, iws, score], [score])
                                      yield
                              op("dve", lambda e, t=t: e.memset(score[0:64, t * 128 + 64:(t + 1) * 128], NEG), [], [score])
                              if t >= 2:
                                  op("dve", lambda e, nk=nk: e.tensor_reduce(out=blo[:], in_=score[:, 0:nk - 64], axis=AX.X, op=ALU.min), [score], [blo])
                                  op("dve", lambda e, nk=nk: e.tensor_reduce(out=bd0[:], in_=score[:, 0:nk], axis=AX.X, op=ALU.max), [score], [bd0])
                                  op("dve", lambda e: e.tensor_tensor(out=bd0[:], in0=bd0[:], in1=blo[:], op=ALU.subtract), [bd0, blo], [bd0])
                                  for k in range(24):
                                      ck = 2.0 ** (-(k + 1))
                                      op("dve", lambda e, ck=ck: e.scalar_tensor_tensor(out=bmid[:], in0=bd0[:], scalar=ck, in1=blo[:], op0=ALU.mult, op1=ALU.add), [bd0, blo], [bmid])
                                      op("dve", lambda e, nk=nk: e.tensor_scalar(out=bjunk[:, 0:nk], in0=score[:, 0:nk], scalar1=bmid[:, 0:1], scalar2=None, op0=ALU.is_ge, op1=ALU.add, accum_out=bcnt[:, 0:1]), [score, bmid], [bcnt])
                                      op("dve", lambda e, ck=ck: e.tensor_scalar(out=bg[:], in0=bcnt[:], scalar1=255.5, scalar2=ck, op0=ALU.is_ge, op1=ALU.mult), [bcnt], [bg])
                                      op("dve", lambda e: e.scalar_tensor_tensor(out=blo[:], in0=bg[:], scalar=bd0[:, 0:1], in1=blo[:], op0=ALU.mult, op1=ALU.add), [bg, bd0, blo], [blo])
                                      if k % 2 == 1:
                                          yield
                                  op("dve", lambda e, nk=nk: e.tensor_scalar(out=maskb[:, 0:nk], in0=score[:, 0:nk], scalar1=blo[:, 0:1], scalar2=None, op0=ALU.is_ge), [score, blo], [maskb])
                              else:
                                  op("dve", lambda e, nk=nk: e.tensor_scalar(out=maskb[:, 0:nk], in0=score[:, 0:nk], scalar1=-1.0e29, scalar2=None, op0=ALU.is_ge), [score], [maskb])
                              yield
                              for kb in range((t + 8) // 8):
                                  k1 = min(t + 1, kb * 8 + 8)
                                  for kt in range(kb * 8, k1):
                                      op("pe", lambda e, kt=kt, kb=kb: e.transpose(out=ps_m[:, kt - kb * 8, :], in_=maskb[:, kt * 128:(kt + 1) * 128], identity=ident_b[:]), [maskb, ident_b], [ps_m])
                                  op("act", lambda e, kb=kb, k1=k1, t=t: e.activation(out=maskT_all[:, moff[t] + kb * 8:moff[t] + k1, :], in_=ps_m[:, 0:k1 - kb * 8, :], func=AF.Copy), [ps_m], [maskT_all])
                                  yield
                      dma(lambda e: e.dma_start(out=dvs[:], in_=dv_s[s].rearrange("(t p) c -> p t c", p=128)[:, :, 0:512]), [], [dvs], dvs)
                      its = []
                      for h in range(4):
                          for m in range(2):
                              for G in range(4):
                                  for kt in range(4 * G + 4):
                                      its.append((h, m, G, kt, 4 * G + 4))

                      def a_loads(g):
                          dk_ = dkq[g % 2]
                          dma(lambda e, dk_=dk_, g=g: e.dma_start(out=dk_[:, 0, :], in_=qT_s[s][:, g, :]), [], [dk_], dk_)
                          dma(lambda e, dk_=dk_, g=g: e.dma_start(out=dk_[:, 1, :], in_=qT_s[s][:, 8 + g, :]), [], [dk_], dk_)

                      def a_stage1(i):
                          h, m, G, kt, nkt = its[i]
                          dk_ = dkq[(2 * h + m) % 2]; pss = ps_s[acnt["s"] % 2]; acnt["s"] += 1; pt = PT[i % 3]
                          op("pe", lambda e: e.matmul(pss[:], lhsT=dk_[:, 1, kt * 128:(kt + 1) * 128], rhs=dk_[:, 0, G * 512:(G + 1) * 512], start=True, stop=True), [dk_], [pss])
                          op("act", lambda e: e.activation(out=pt[:], in_=pss[:], func=AF.Exp, scale=0.125), [pss], [pt])
                          if kt >= 4 * G:
                              j = kt - 4 * G
                              if j > 0:
                                  op("pool", lambda e: e.memset(pt[:, 0:j * 128], 0.0), [], [pt])
                              op("pool", lambda e: e.memset(pt[64:128, j * 128:j * 128 + 64], 0.0), [], [pt])

                      def a_stage2(i):
                          h, m, G, kt, nkt = its[i]
                          pt = PT[i % 3]
                          grp = (h * 2 + m) * 4 + G
                          pso_ = ps_o[grp % 2]; psz_ = ps_z[grp % 2]
                          op("pe", lambda e: e.matmul(pso_[:], lhsT=dvs[:, kt, h * 128:(h + 1) * 128], rhs=pt[:], start=(kt == 0), stop=(kt == nkt - 1)), [dvs, pt], [pso_])
                          op("pe", lambda e: e.matmul(psz_[:], lhsT=ones_b[:], rhs=pt[:], start=(kt == 0), stop=(kt == nkt - 1)), [ones_b, pt], [psz_])
                          if kt != nkt - 1:
                              return
                          op("dve", lambda e: e.reciprocal(out=rz[:], in_=psz_[:]), [psz_], [rz])
                          gsl = slice(G * 512, (G + 1) * 512)
                          if m == 0:
                              op("dve", lambda e: e.tensor_tensor(out=o0n[:, gsl], in0=pso_[:], in1=rz[:], op=ALU.mult), [pso_, rz], [o0n])
                          else:
                              op("dve", lambda e: e.tensor_tensor(out=o1n[:], in0=pso_[:], in1=rz[:], op=ALU.mult), [pso_, rz], [o1n])
                              op("dve", lambda e: e.scalar_tensor_tensor(out=dd[:], in0=o1n[:], scalar=neglam[:, 0:1], in1=o0n[:, gsl], op0=ALU.mult, op1=ALU.add), [o1n, neglam, o0n], [dd])
                              op("act", lambda e: e.activation(out=dsq[:], in_=dd[:], func=AF.Square), [dd], [dsq])
                              ps_q = ps_s[acnt["s"] % 2]; acnt["s"] += 1
                              op("pe", lambda e: e.matmul(ps_q[:], lhsT=ones_b[:], rhs=dsq[:], start=True, stop=True), [ones_b, dsq], [ps_q])
                              op("dve", lambda e: e.tensor_scalar(out=rstd[:], in0=ps_q[:], scalar1=1.0 / 128, scalar2=EPS, op0=ALU.mult, op1=ALU.add), [ps_q], [rstd])
                              op("act", lambda e: e.activation(out=rstd[:], in_=rstd[:], func=AF.Sqrt), [rstd], [rstd])
                              op("dve", lambda e: e.reciprocal(out=rstd[:], in_=rstd[:]), [rstd], [rstd])
                              op("dve", lambda e: e.tensor_tensor(out=dd[:], in0=dd[:], in1=rstd[:], op=ALU.mult), [dd, rstd], [dd])
                              op("dve", lambda e: e.tensor_scalar(out=oaT[:, h, gsl], in0=dd[:], scalar1=ogcol[:, 0:1], scalar2=None, op0=ALU.mult), [dd, ogcol], [oaT])

                      acnt = {"s": 0}
                      bgen = b_part1_gen()
                      a_loads(0)
                      a_stage1(0)
                      for i in range(len(its)):
                          h, m, G, kt, nkt = its[i]
                          if G == 0 and kt == 0 and 2 * h + m + 1 < 8:
                              a_loads(2 * h + m + 1)
                          if i + 1 < len(its):
                              a_stage1(i + 1)
                          a_stage2(i)
                          next(bgen, None); next(bgen, None)
                      for _ in bgen:
                          pass
                      S.barrier(); S.flush()

                with ExitStack() as esb:
                  if 'B' in stages:
                      A = Alloc(nc, esb, "p2b%d" % s)
                      skT = A.sb("skT", [64, T_SEQ], BF16)
                      svd = A.sb("svd", [128, NT, 128], BF16)
                      sqT = [A.sb("sqT%d" % i, [64, 8, 128], BF16) for i in range(3)]
                      PT = [A.sb("PTb%d" % i, [128, 4, 128], BF16) for i in range(3)]
                      oS = [A.sb("oS%d" % i, [128, 4, 128], F32) for i in range(2)]; zS = [A.sb("zS%d" % i, [128, 4, 128], F32) for i in range(2)]
                      ps_s = [A.ps("ps_sb%d" % i, [128, 4, 128], F32) for i in range(2)]
                      ps_o = [A.ps("ps_ob%d" % i, [128, 4, 128], F32) for i in range(2)]; ps_z = [A.ps("ps_zb%d" % i, [128, 4, 128], F32) for i in range(2)]
                      dma(lambda e: e.dma_start(out=skT[:], in_=qT_s[s][:, 24, :]), [], [skT], skT)
                      svv = dv_s[s].rearrange("(t p) c -> p t c", p=128)
                      dma(lambda e: e.dma_start(out=svd[:, :, 0:64], in_=svv[:, :, 512:576]), [], [svd], svd)
                      dma(lambda e: e.dma_start(out=svd[:, :, 64:128], in_=svv[:, :, 512:576]), [], [svd], svd)
                      its = [(t, half, kt) for t in range(NT) for half in range(2) for kt in range(t + 1)]

                      def sq_load(t):
                          sq_ = sqT[t % 3]
                          dma(lambda e: e.dma_start(out=sq_[:], in_=qT_s[s][:, 16:24, t * 128:(t + 1) * 128]), [], [sq_], sq_)

                      def st1(i):
                          t, half, kt = its[i]
                          sq_ = sqT[t % 3]
                          pss = ps_s[i % 2]; pt = PT[i % 3]
                          op("pe", lambda e: e.matmul(pss[:], lhsT=skT[:, kt * 128:(kt + 1) * 128], rhs=sq_[:, 4 * half:4 * half + 4, :], start=True, stop=True), [skT, sq_], [pss])
                          op("act", lambda e: e.activation(out=pt[:], in_=pss[:], func=AF.Exp, scale=0.125), [pss], [pt])
                          op("pool", lambda e: e.tensor_tensor(out=pt[:], in0=pt[:], in1=maskT_all[:, moff[t] + kt, :].unsqueeze(1).to_broadcast([128, 4, 128]), op=ALU.mult), [pt, maskT_all], [pt])

                      def st2(i):
                          t, half, kt = its[i]
                          pt = PT[i % 3]
                          grp = 2 * t + half
                          pso_ = ps_o[grp % 2]; psz_ = ps_z[grp % 2]
                          tsl = slice(t * 128, (t + 1) * 128)
                          op("pe", lambda e: e.matmul(pso_[:], lhsT=svd[:, kt, :], rhs=pt[:], start=(kt == 0), stop=(kt == t)), [svd, pt], [pso_])
                          op("pe", lambda e: e.matmul(psz_[:], lhsT=ones_b[:], rhs=pt[:], start=(kt == 0), stop=(kt == t)), [ones_b, pt], [psz_])
                          if kt != t:
                              return
                          oS_ = oS[grp % 2]; zS_ = zS[grp % 2]
                          op("act", lambda e: e.activation(out=oS_[:], in_=pso_[:], func=AF.Copy), [pso_], [oS_])
                          op("dve", lambda e: e.reciprocal(out=zS_[:], in_=psz_[:]), [psz_], [zS_])
                          for par in range(2):
                              psl = slice(par * 64, par * 64 + 64)
                              op("dve", lambda e, psl=psl, par=par: e.tensor_tensor(out=obT[psl, 2 * half:2 * half + 2, tsl], in0=oS_[psl, par::2, :], in1=zS_[psl, par::2, :], op=ALU.mult), [oS_, zS_], [obT])

                      sq_load(0); sq_load(1)
                      st1(0)
                      for i in range(len(its)):
                          t, half, kt = its[i]
                          if half == 0 and kt == 0 and t + 2 < NT:
                              sq_load(t + 2)
                          if i + 1 < len(its):
                              st1(i + 1)
                          st2(i)
                      S.barrier(); S.flush()

                with ExitStack() as esc:
                  if 'C' in stages:
                      A = Alloc(nc, esc, "p2c%d" % s)
                      gT = [A.sb("gT%d" % i, [128, 2, 512], BF16) for i in range(2)]
                      t1 = A.sb("t1", [128, 512], F32); t2 = A.sb("t2", [128, 512], F32)
                      mT = A.sb("mT", [128, 8, 512], BF16)
                      xt = [A.sb("xtc%d" % i, [128, DM], F32) for i in range(2)]
                      g1b = A.sb("g1b", [128, DM], F32); tmp3 = A.sb("tmp3", [128, DM], F32)
                      ps_a = A.ps("ps_a", [128, 512], F32); ps_b = A.ps("ps_b", [128, 512], F32)
                      ps_x = [A.ps("ps_x%d" % i, [128, 512], F32) for i in range(2)]
                      dma(lambda e: e.dma_start(out=g1b[:], in_=rows_d[s, 2:3, :].partition_broadcast(128)), [], [g1b], g1b)
                      ic = 0
                      for G in range(4):
                          gsl = slice(G * 512, (G + 1) * 512)
                          for c in range(8):
                              gTi = gT[ic % 2]; ic += 1
                              dma(lambda e, gTi=gTi, c=c, gsl=gsl: e.dma_start(out=gTi[:], in_=gT_s[s][:, c::8, gsl]), [], [gTi], gTi)
                              for j in range(4):
                                  op("pe", lambda e, j=j, c=c, gsl=gsl: e.matmul(ps_a[:], lhsT=wba[:, j, c * 128:(c + 1) * 128], rhs=oaT[:, j, gsl], start=(j == 0), stop=(j == 3)), [wba, oaT], [ps_a])
                              for j in range(4):
                                  op("pe", lambda e, j=j, c=c, gsl=gsl: e.matmul(ps_b[:], lhsT=wbb[:, j, c * 128:(c + 1) * 128], rhs=obT[:, j, gsl], start=(j == 0), stop=(j == 3)), [wbb, obT], [ps_b])
                              op("dve", lambda e, gTi=gTi: e.tensor_tensor(out=t1[:], in0=ps_a[:], in1=gTi[:, 0, :], op=ALU.mult), [ps_a, gTi], [t1])
                              op("dve", lambda e, gTi=gTi: e.tensor_tensor(out=t2[:], in0=ps_b[:], in1=gTi[:, 1, :], op=ALU.mult), [ps_b, gTi], [t2])
                              op("pool", lambda e, c=c: e.tensor_tensor(out=mT[:, c, :], in0=t1[:], in1=t2[:], op=ALU.add), [t1, t2], [mT])
                          for q in range(4):
                              tt = 4 * G + q
                              r0 = s * T_SEQ + tt * 128
                              xti = xt[tt % 2]
                              dma(lambda e, xti=xti, r0=r0: e.dma_start(out=xti[:], in_=x[r0:r0 + 128, :]), [], [xti], xti)
                              for half in range(2):
                                  psx = ps_x[half]
                                  for c in range(8):
                                      op("pe", lambda e, psx=psx, c=c, q=q, half=half: e.matmul(psx[:], lhsT=mT[:, c, q * 128:(q + 1) * 128], rhs=wo[:, c, half * 512:(half + 1) * 512], start=(c == 0), stop=(c == 7)), [mT, wo], [psx])
                                  op("dve", lambda e, psx=psx, half=half: e.tensor_tensor(out=tmp3[:, half * 512:(half + 1) * 512], in0=psx[:], in1=g1b[:, half * 512:(half + 1) * 512], op=ALU.mult), [psx, g1b], [tmp3])
                              op("pool", lambda e, xti=xti: e.tensor_tensor(out=xti[:], in0=xti[:], in1=tmp3[:], op=ALU.add), [xti, tmp3], [xti])
                              dma(lambda e, xti=xti, r0=r0: e.dma_start(out=out[r0:r0 + 128, :], in_=xti[:]), [xti], [], xti)
                      S.barrier(); S.flush()

        if 3 in phases:
          with ExitStack() as es3:
            A = Alloc(nc, es3, "p3")
            wq_bf = A.sb("wq_bf", [128, 8, 2048], BF16); skb = A.sb("skb", [128, 16, 128], BF16)
            w_q_v = w_q.rearrange("(kc p) n -> p kc n", p=128)
            for j in range(4):
                dma(lambda e, j=j: e.dma_start(out=wq_bf[:, :, j * 512:(j + 1) * 512], in_=w_q_v[:, :, j * 512:(j + 1) * 512]), [], [wq_bf], wq_bf, eng="pool")
            dma(lambda e: e.dma_start(out=skb[:], in_=subkT), [], [skb], skb, eng="pool")
            xt = [A.sb("xt%d" % i, [128, DM], F32) for i in range(2)]
            junk = A.sb("junk", [128, DM], BF16); xn = A.sb("xn", [128, DM], BF16)
            ssq = [A.sb("ssq%d" % i, [128, 1], F32) for i in range(2)]
            h2T = A.sb("h2T", [128, 8, 128], BF16)
            h2 = [A.sb("h2_%d" % i, [128, 8, 128], BF16) for i in range(2)]
            tmpf = A.sb("tmpf", [128, DM], F32)
            G2b = [A.sb("G2b%d" % i, [128, DM], F32) for i in range(2)]
            qT = A.sb("qT", [128, 16, 128], BF16)
            sc_sb = A.sb("sc_sb", [128, 16, 128], F32); scw = A.sb("scw", [128, 16, 128], F32)
            v16 = A.sb("v16", [128, 16, 16], F32); i16 = A.sb("i16", [128, 16, 16], U32); i16f = A.sb("i16f", [128, 16, 16], F32)
            cand = A.sb("cand", [128, 8, 256], F32); candw = A.sb("candw", [128, 8, 256], F32)
            sc = A.sb("sc", [128, 8, 16], F32); posu = A.sb("posu", [128, 8, 16], U32)
            au = A.sb("au", [128, 8, 16], U32); bu = A.sb("bu", [128, 8, 16], U32)
            af = A.sb("af", [128, 8, 16], F32); bf_ = A.sb("bf_", [128, 8, 16], F32)
            eq = A.sb("eq", [128, 8, 16, 16], F32)
            e1 = A.sb("e1", [128, 8, 16], F32); e2 = A.sb("e2", [128, 8, 16], F32)
            eidx = [A.sb("eidx%d" % i, [128, 128], U32) for i in range(2)]
            ex = A.sb("ex", [128, 8, 16], F32); esum = A.sb("esum", [128, 8], F32)
            gg = [A.sb("gg%d" % i, [128, 128], F32) for i in range(2)]
            av = A.sb("av", [128, 128], F32)
            wg8 = [A.sb("wg8_%d" % i, [128, 8], F32) for i in range(2)]
            NR = 20
            uvg = [A.sb("uvg%d" % i, [128, 2 * DM], BF16) for i in range(NR)]
            djunk = A.sb("djunk", [128, DM], BF16)
            dg = [A.sb("dg%d" % i, [128, 128], BF16) for i in range(4)]
            pT = A.ps("pT3", [128, 8, 128], BF16)
            psq = [A.ps("psq%d" % i, [128, 4, 128], F32) for i in range(2)]
            pso = [[A.ps("pso%d_%d" % (i, j), [128, 512], F32) for j in range(2)] for i in range(2)]
            tiles = [(s, t) for s in range(ns) for t in range(NT)]

            def routing_pieces(gi):
                s, t = tiles[gi]
                i2 = gi % 2
                r0 = s * T_SEQ + t * 128
                xti = xt[i2]; ssi = ssq[i2]; eix = eidx[i2]; h2i = h2[i2]; ggi = gg[i2]
                P = []

                def p_load():
                    if t == 0:
                        gb = G2b[s % 2]
                        dma(lambda e: e.dma_start(out=gb[:], in_=rows_d[s, 5:6, :].partition_broadcast(128)), [], [gb], gb)
                    dma(lambda e: e.dma_start(out=xti[:], in_=out[r0:r0 + 128, :]), [], [xti], xti)
                    op("act", lambda e: e.activation(out=junk[:], in_=xti[:], func=AF.Square, accum_out=ssi[:, 0:1]), [xti], [junk, ssi])
                    rms_rstd(A, ssi[:, 0:1], float(DM), ssi, "x")
                    op("act", lambda e: e.activation(out=xn[:], in_=xti[:], func=AF.Copy, scale=ssi[:, 0:1]), [xti, ssi], [xn])
                    for kc in range(8):
                        op("pe", lambda e, kc=kc: e.transpose(out=pT[:, kc, :], in_=xn[:, kc * 128:(kc + 1) * 128], identity=ident_b[:]), [xn, ident_b], [pT])
                    for kc in range(8):
                        op("act", lambda e, kc=kc: e.activation(out=h2T[:, kc, :], in_=pT[:, kc, :], func=AF.Identity, scale=cols[:, 2, kc, s:s + 1], bias=cols[:, 3, kc, s:s + 1]), [pT, cols], [h2T])
                    for kc in range(8):
                        op("pe", lambda e, kc=kc: e.transpose(out=pT[:, kc, :], in_=h2T[:, kc, :], identity=ident_b[:]), [h2T, ident_b], [pT])
                    op("act", lambda e: e.activation(out=h2i[:], in_=pT[:], func=AF.Copy), [pT], [h2i])
                P.append(p_load)

                def p_q(r0_, r1_):
                    def f():
                        for r in range(r0_, r1_):
                            pq_ = psq[r % 2]
                            for j in range(4):
                                hp = r * 4 + j
                                for kc in range(8):
                                    op("pe", lambda e, pq_=pq_, j=j, hp=hp, kc=kc: e.matmul(pq_[:, j, :], lhsT=wq_bf[:, kc, hp * 128:(hp + 1) * 128], rhs=h2T[:, kc, :], start=(kc == 0), stop=(kc == 7)), [wq_bf, h2T], [pq_])
                            op("act", lambda e, pq_=pq_, r=r: e.activation(out=qT[:, r * 4:(r + 1) * 4, :], in_=pq_[:], func=AF.Copy), [pq_], [qT])
                    return f
                P.append(p_q(0, 2)); P.append(p_q(2, 4))

                def p_sc():
                    for r in range(4):
                        pq_ = psq[r % 2]
                        for j in range(4):
                            hp = r * 4 + j
                            op("pe", lambda e, pq_=pq_, j=j, hp=hp: e.matmul(pq_[:, j, :], lhsT=qT[:, hp, :], rhs=skb[:, hp, :], start=True, stop=True), [qT, skb], [pq_])
                        op("act", lambda e, pq_=pq_, r=r: e.activation(out=sc_sb[:, r * 4:(r + 1) * 4, :], in_=pq_[:], func=AF.Copy), [pq_], [sc_sb])
                P.append(p_sc)

                def p_top(h0, h1):
                    def f():
                        for hp in range(h0, h1):
                            op("dve", lambda e, hp=hp: e.max(out=v16[:, hp, 0:8], in_=sc_sb[:, hp, :]), [sc_sb], [v16])
                            op("dve", lambda e, hp=hp: e.max_index(out=i16[:, hp, 0:8], in_max=v16[:, hp, 0:8], in_values=sc_sb[:, hp, :]), [sc_sb, v16], [i16])
                            op("dve", lambda e, hp=hp: e.match_replace(out=scw[:, hp, :], in_to_replace=v16[:, hp, 0:8], in_values=sc_sb[:, hp, :], imm_value=NEG), [sc_sb, v16], [scw])
                            op("dve", lambda e, hp=hp: e.max(out=v16[:, hp, 8:16], in_=scw[:, hp, :]), [scw], [v16])
                            op("dve", lambda e, hp=hp: e.max_index(out=i16[:, hp, 8:16], in_max=v16[:, hp, 8:16], in_values=scw[:, hp, :]), [scw, v16], [i16])
                    return f
                for q in range(4):
                    P.append(p_top(q * 4, q * 4 + 4))

                def p_cand():
                    v4 = v16[:, :, :].rearrange("p (h a) k -> p h a k", a=2)
                    c4 = cand[:, :, :].rearrange("p h (a b) -> p h a b", b=16)
                    op("dve", lambda e: e.tensor_tensor(out=c4, in0=v4[:, :, 0, :].unsqueeze(3).to_broadcast([128, 8, 16, 16]), in1=v4[:, :, 1, :].unsqueeze(2).to_broadcast([128, 8, 16, 16]), op=ALU.add), [v16], [cand])
                P.append(p_cand)

                def p_top2(h0, h1):
                    def f():
                        for h in range(h0, h1):
                            op("dve", lambda e, h=h: e.max(out=sc[:, h, 0:8], in_=cand[:, h, :]), [cand], [sc])
                            op("dve", lambda e, h=h: e.max_index(out=posu[:, h, 0:8], in_max=sc[:, h, 0:8], in_values=cand[:, h, :]), [cand, sc], [posu])
                            op("dve", lambda e, h=h: e.match_replace(out=candw[:, h, :], in_to_replace=sc[:, h, 0:8], in_values=cand[:, h, :], imm_value=NEG), [cand, sc], [candw])
                            op("dve", lambda e, h=h: e.max(out=sc[:, h, 8:16], in_=candw[:, h, :]), [candw], [sc])
                            op("dve", lambda e, h=h: e.max_index(out=posu[:, h, 8:16], in_max=sc[:, h, 8:16], in_values=candw[:, h, :]), [candw, sc], [posu])
                    return f
                for q in range(4):
                    P.append(p_top2(q * 2, q * 2 + 2))

                def p_idx():
                    op("dve", lambda e: e.tensor_single_scalar(out=au[:], in_=posu[:], scalar=4, op=ALU.logical_shift_right), [posu], [au])
                    op("dve", lambda e: e.tensor_single_scalar(out=bu[:], in_=posu[:], scalar=15, op=ALU.bitwise_and), [posu], [bu])
                    op("dve", lambda e: e.tensor_copy(out=af[:], in_=au[:]), [au], [af])
                    op("dve", lambda e: e.tensor_copy(out=bf_[:], in_=bu[:]), [bu], [bf_])
                    op("dve", lambda e: e.tensor_copy(out=i16f[:], in_=i16[:]), [i16], [i16f])
                    i4 = i16f[:, :, :].rearrange("p (h a) k -> p h a k", a=2)
                    io4 = iota16[:, :].unsqueeze(1).unsqueeze(1).to_broadcast([128, 8, 16, 16])
                    for (sel, which, dst) in ((af, 0, e1), (bf_, 1, e2)):
                        op("dve", lambda e, sel=sel: e.tensor_tensor(out=eq[:], in0=io4, in1=sel[:, :, :].unsqueeze(3).to_broadcast([128, 8, 16, 16]), op=ALU.is_equal), [iota16, sel], [eq])
                        op("dve", lambda e, which=which: e.tensor_tensor(out=eq[:], in0=eq[:], in1=i4[:, :, which, :].unsqueeze(2).to_broadcast([128, 8, 16, 16]), op=ALU.mult), [eq, i16f], [eq])
                        op("dve", lambda e, dst=dst: e.tensor_reduce(out=dst[:], in_=eq[:], axis=AX.X, op=ALU.add), [eq], [dst])
                    op("dve", lambda e: e.scalar_tensor_tensor(out=e1[:], in0=e1[:], scalar=128.0, in1=e2[:], op0=ALU.mult, op1=ALU.add), [e1, e2], [e1])
                    op("dve", lambda e: e.tensor_copy(out=eix[:], in_=e1[:, :, :].rearrange("p h k -> p (h k)")), [e1], [eix])
                P.append(p_idx)

                def p_soft():
                    op("dve", lambda e: e.tensor_tensor(out=ex[:], in0=sc[:], in1=sc[:, :, 0:1].to_broadcast([128, 8, 16]), op=ALU.subtract), [sc], [ex])
                    op("act", lambda e: e.activation(out=ex[:], in_=ex[:], func=AF.Exp), [ex], [ex])
                    op("dve", lambda e: e.tensor_reduce(out=esum[:], in_=ex[:], axis=AX.X, op=ALU.add), [ex], [esum])
                    op("dve", lambda e: e.reciprocal(out=esum[:], in_=esum[:]), [esum], [esum])
                    op("dve", lambda e: e.tensor_tensor(out=ggi[:, :].rearrange("p (h k) -> p h k", k=16), in0=ex[:], in1=esum[:, :].unsqueeze(2).to_broadcast([128, 8, 16]), op=ALU.mult), [ex, esum], [ggi])
                P.append(p_soft)
                return P

            gcnt = {"g": 0}
            avres = [[Res("avr%d_%d" % (i, j)) for j in range(8)] for i in range(2)]

            def gather_group(gi, grp):
                s, t = tiles[gi]
                i2 = gi % 2
                eix = eidx[i2]; h2i = h2[i2]; ggi = gg[i2]
                pso_ = pso[gi % 2]
                wg = wg8[grp % 2]
                bufs = []
                for j in range(8):
                    hk = grp * 8 + j
                    u_ = uvg[gcnt["g"] % NR]; gcnt["g"] += 1
                    bufs.append(u_)
                    dma(lambda e, u_=u_, hk=hk: e.indirect_dma_start(out=u_[:], out_offset=None, in_=uv_bf, in_offset=bass.IndirectOffsetOnAxis(ap=eix[:, hk:hk + 1], axis=0)), [eix], [u_], u_, eng="pool")
                    op("dve", lambda e, u_=u_, hk=hk: e.scalar_tensor_tensor(out=djunk[:], in0=u_[:, 0:DM], scalar=1.0, in1=h2i[:, :, :].rearrange("p a b -> p (a b)"), op0=ALU.mult, op1=ALU.mult, accum_out=av[:, hk:hk + 1]), [u_, h2i], [avres[grp % 2][j]])
                gs_ = slice(grp * 8, grp * 8 + 8)
                op("act", lambda e: e.activation(out=wg[:], in_=av[:, gs_], func=AF.Gelu), avres[grp % 2], [wg])
                op("dve", lambda e: e.tensor_tensor(out=wg[:], in0=wg[:], in1=ggi[:, gs_], op=ALU.mult), [wg, ggi], [wg])
                for j in range(8):
                    hk = grp * 8 + j
                    u_ = bufs[j]; d_ = dg[hk % 4]
                    op("act", lambda e, d_=d_, j=j: e.activation(out=d_[:], in_=ident_b[:], func=AF.Copy, scale=wg[:, j:j + 1]), [ident_b, wg], [d_])
                    for half in range(2):
                        op("pe", lambda e, d_=d_, u_=u_, half=half, hk=hk: e.matmul(pso_[half][:], lhsT=d_[:], rhs=u_[:, DM + half * 512:DM + (half + 1) * 512], start=(hk == 0), stop=(hk == 127)), [d_, u_], [pso_[half]])

            def final(gi):
                s, t = tiles[gi]
                r0 = s * T_SEQ + t * 128
                xti = xt[gi % 2]; pso_ = pso[gi % 2]; gb = G2b[s % 2]
                for half in range(2):
                    hs = slice(half * 512, (half + 1) * 512)
                    op("dve", lambda e, half=half, hs=hs: e.tensor_tensor(out=tmpf[:, hs], in0=pso_[half][:], in1=gb[:, hs], op=ALU.mult), [pso_[half], gb], [tmpf])
                op("dve", lambda e: e.tensor_tensor(out=xti[:], in0=xti[:], in1=tmpf[:], op=ALU.add), [xti, tmpf], [xti])
                dma(lambda e: e.dma_start(out=out[r0:r0 + 128, :], in_=xti[:]), [xti], [], xti)

            for p in routing_pieces(0):
                p()
            for gi in range(len(tiles)):
                nxt = routing_pieces(gi + 1) if gi + 1 < len(tiles) else []
                for grp in range(16):
                    gather_group(gi, grp)
                    if grp < len(nxt):
                        nxt[grp]()
                for p in nxt[16:]:
                    p()
                final(gi)
            S.barrier(); S.flush()
    return nc


def _core_inputs(inp, b0, ns):
    m = {}
    m["x"] = np.ascontiguousarray(inp["x"][b0:b0 + ns].reshape(ns * T_SEQ, DM))
    m["c"] = np.ascontiguousarray(inp["c"][b0:b0 + ns])
    m["pos"] = np.ascontiguousarray(np.asarray(inp["positions"][b0:b0 + ns]).reshape(ns, NT, 128).transpose(0, 2, 1)).astype(np.int32)
    return m


def kernel(**inputs):
    inp = {k: np.asarray(v) for k, v in inputs.items()}
    n_cores = 8
    ns = inp["x"].shape[0] // n_cores
    shared = {}
    shared["inv"] = (np.float32(10000.0) ** (-(np.arange(0, 64, 2, dtype=np.float32) / np.float32(64)))).astype(np.float32).reshape(1, 32)
    for k in ("w_ada", "w_in", "w_branch_a", "w_branch_b", "w_out", "peer_w_q", "peer_u", "peer_v"):
        shared[k] = np.ascontiguousarray(inp[k][0], dtype=np.float32)
    for k in ("b_ada", "norm1_g", "norm2_g", "diff_q_g", "diff_k_g", "dsa_q_g", "dsa_k_g",
              "diff_lam_q1", "diff_lam_k1", "diff_lam_q2", "diff_lam_k2"):
        shared[k] = np.ascontiguousarray(inp[k][0].reshape(1, -1), dtype=np.float32)
    shared["diff_out_g"] = np.ascontiguousarray(inp["diff_out_g"][0].reshape(128, 1), dtype=np.float32)
    shared["subkT"] = np.ascontiguousarray(inp["peer_sub_keys"][0].reshape(16, 128, 128).transpose(2, 0, 1), dtype=np.float32)
    in_maps = []
    for ci in range(n_cores):
        m = dict(shared)
        m.update(_core_inputs(inp, ci * ns, ns))
        in_maps.append(m)
    nc = build(ns)
    res = run_bass_kernel_spmd(nc, in_maps, core_ids=list(range(n_cores)))
    outs = [np.asarray(r["out"]).reshape(ns, T_SEQ, DM) for r in res.results]
    return np.concatenate(outs, axis=0).astype(np.float32)
```

```python
import numpy as np, math
from contextlib import ExitStack
import concourse.bass as bass
import concourse.mybir as mybir
from concourse.bass_utils import run_bass_kernel_spmd

F32 = mybir.dt.float32
BF16 = mybir.dt.bfloat16
I32 = mybir.dt.int32
U32 = mybir.dt.uint32
ALU = mybir.AluOpType
AF = mybir.ActivationFunctionType
AX = mybir.AxisListType


class Res:
    __slots__ = ("name", "w", "r", "dsem", "dcnt")

    def __init__(self, name):
        self.name = name
        self.w = []
        self.r = []
        self.dsem = None
        self.dcnt = 0


class Sched:
    ENGS = ("pe", "act", "dve", "pool", "sp")

    def __init__(self, nc, es):
        self.nc = nc
        self.es = es
        self.items = {e: [] for e in self.ENGS}
        self.cnt = {e: 0 for e in self.ENGS}
        self.sems = {}
        for e in self.ENGS:
            if e != "sp":
                self.sems[e] = es.enter_context(nc.semaphore("sem_" + e))
        self.known = {e: {} for e in self.ENGS}
        self.dstate = {}
        self.ninst = 0

    def _dstate(self, res):
        st = self.dstate.get(res.name)
        if st is None:
            sem = self.es.enter_context(self.nc.semaphore("dsem_%d_%s" % (len(self.dstate), res.name)))
            st = [sem, 0]
            self.dstate[res.name] = st
            self.sems[("d", res.name)] = sem
        return st

    def _collect(self, eng, reads, writes):
        deps = []
        for r in reads:
            deps += r.w
        for w in writes:
            deps += w.w
            deps += w.r
        out = {}
        for (k, v) in deps:
            if eng == "pe" and k == "pe":
                continue
            if self.known[eng].get(k, 0) >= v:
                continue
            if out.get(k, 0) < v:
                out[k] = v
        for k, v in out.items():
            self.known[eng][k] = v
        return list(out.items())

    def _update(self, dep, reads, writes):
        for r in reads:
            if r not in writes:
                r.r.append(dep)
        for w in writes:
            w.w = [dep]
            w.r = []

    def op(self, eng, fn, reads=(), writes=()):
        reads = list(reads); writes = list(writes)
        waits = self._collect(eng, reads, writes)
        self.cnt[eng] += 1
        dep = (eng, self.cnt[eng])
        self.items[eng].append((waits, fn, (eng, 1)))
        self._update(dep, reads, writes)
        self.ninst += 1

    def dma(self, fn, reads=(), writes=(), primary=None, eng="sp"):
        reads = list(reads); writes = list(writes)
        waits = self._collect(eng, reads, writes)
        st = self._dstate(primary)
        st[1] += 16
        key = ("d", primary.name)
        dep = (key, st[1])
        self.items[eng].append((waits, fn, (key, 16)))
        self._update(dep, reads, writes)
        self.ninst += 1

    def barrier(self):
        allv = []
        for e in self.ENGS:
            if e != "sp" and self.cnt[e] > 0:
                allv.append((e, self.cnt[e]))
        for nm, st in self.dstate.items():
            allv.append((("d", nm), st[1]))
        for e in self.ENGS:
            waits = []
            for (k, v) in allv:
                if self.known[e].get(k, 0) < v:
                    waits.append((k, v))
                    self.known[e][k] = v
            if waits:
                self.items[e].append((waits, None, None))

    def flush(self, name=None):
        nc = self.nc
        items = self.items
        sems = self.sems

        def replay(engname):
            def run(eng):
                for (waits, fn, inc) in items[engname]:
                    for (k, v) in waits:
                        eng.wait_ge(sems[k], v)
                    if fn is not None:
                        inst = fn(eng)
                        inst.then_inc(sems[inc[0]], inc[1])
            return run

        with nc.Block() as block:
            if items["sp"]:
                block.sync(replay("sp"))
            if items["act"]:
                block.scalar(replay("act"))
            if items["dve"]:
                block.vector(replay("dve"))
            if items["pool"]:
                block.gpsimd(replay("pool"))
            if items["pe"]:
                block.tensor(replay("pe"))
        self.items = {e: [] for e in self.ENGS}
NS_FULL = 4
T_SEQ = 2048
NT = 16
DM = 1024
IN_COLS = 4808
NPROJ = 2760
GATE0 = 2760
EPS = 1e-6
LAMBDA_INIT = 0.8 - 0.6 * math.exp(-0.0)
NEG = -1.0e30
TWO_PI = 2.0 * math.pi


class T_:
    __slots__ = ("t", "r")

    def __init__(self, t, name):
        self.t = t
        self.r = Res(name)

    def __getitem__(self, k):
        return self.t[k]


class Alloc:
    def __init__(self, nc, es, prefix):
        self.nc, self.es, self.p = nc, es, prefix
        self.n = 0

    def sb(self, name, shape, dt):
        self.n += 1
        return T_(self.es.enter_context(self.nc.sbuf_tensor("%s_%s_%d" % (self.p, name, self.n), shape, dt)), name)

    def ps(self, name, shape, dt):
        self.n += 1
        return T_(self.es.enter_context(self.nc.psum_tensor("%s_%s_%d" % (self.p, name, self.n), shape, dt)), name)


def _rs(lst):
    return [x.r if isinstance(x, T_) else x for x in lst]


class K:
    def __init__(self, nc, S, ns):
        self.nc, self.S, self.ns = nc, S, ns

    def op(self, eng, fn, reads=(), writes=()):
        self.S.op(eng, fn, _rs(reads), _rs(writes))

    def dma(self, fn, reads=(), writes=(), primary=None, eng="sp"):
        self.S.dma(fn, _rs(reads), _rs(writes), primary.r if isinstance(primary, T_) else primary, eng)

def build(ns, phases=(0, 1, 2, 3), dbg=False, stages="ABC"):
    nc = bass.Bass("TRN2", target_bir_lowering=False)
    NTOK = ns * T_SEQ

    def din(name, shape, dt=F32):
        return nc.dram_tensor(name, shape, dt, kind="ExternalInput").ap()

    def dscr(name, shape, dt):
        return nc.dram_tensor(name, shape, dt, kind=("ExternalOutput" if dbg else "Internal")).ap()

    x = din("x", [NTOK, DM]); c_in = din("c", [ns, DM]); pos = din("pos", [ns, 128, NT], I32); inv = din("inv", [1, 32])
    w_ada = din("w_ada", [DM, 6 * DM]); b_ada = din("b_ada", [1, 6 * DM]); n1g = din("norm1_g", [1, DM]); w_in = din("w_in", [DM, IN_COLS])
    gains_in = [din(n, [1, 64]) for n in ("diff_q_g", "diff_k_g", "dsa_q_g", "dsa_k_g")]
    lam_in = [din(n, [1, 64]) for n in ("diff_lam_q1", "diff_lam_k1", "diff_lam_q2", "diff_lam_k2")]
    og_in = din("diff_out_g", [128, 1])
    w_ba = din("w_branch_a", [512, DM]); w_bb = din("w_branch_b", [512, DM]); w_o = din("w_out", [DM, DM]); n2g = din("norm2_g", [1, DM])
    w_q = din("peer_w_q", [DM, 2048]); subkT = din("subkT", [128, 16, 128]); pu = din("peer_u", [16384, DM]); pv = din("peer_v", [16384, DM])
    out = nc.dram_tensor("out", [NTOK, DM], F32, kind="ExternalOutput").ap()

    uv_bf = dscr("uv_bf", [16384, 2 * DM], BF16)
    rows_d = dscr("rows_d", [ns, 6, DM], F32)
    qT_s = dscr("qT_s", [ns, 64, 34, T_SEQ], BF16)
    dv_s = dscr("dv_s", [ns, T_SEQ, 576], BF16)
    iw_s = dscr("iw_s", [ns, T_SEQ, 8], F32)
    gT_s = dscr("gT_s", [ns, 128, 16, T_SEQ], BF16)
    if dbg:
        dbg_oa = dscr("dbg_oa", [ns, 128, 4, T_SEQ], BF16); dbg_ob = dscr("dbg_ob", [ns, 128, 4, T_SEQ], BF16)
        dbg_mk = dscr("dbg_mk", [ns, 128, 136, 128], BF16)

    ges = ExitStack()
    with ges:
        S = Sched(nc, ges)
        kk = K(nc, S, ns)
        op, dma = kk.op, kk.dma
        GA = Alloc(nc, ges, "g")
        ident_f = GA.sb("ident_f", [128, 128], F32); ident_b = GA.sb("ident_b", [128, 128], BF16)
        ones_b = GA.sb("ones_b", [128, 128], BF16)
        cols = GA.sb("cols", [128, 4, 8, ns], F32)
        neglam = GA.sb("neglam", [128, 1], F32); ogcol = GA.sb("ogcol", [128, 1], F32)
        gains_b = GA.sb("gains_b", [128, 4, 64], F32); inv_b = GA.sb("inv_b", [128, 32], F32)
        iota16 = GA.sb("iota16", [128, 16], F32)

        with ExitStack() as es0:
            A = Alloc(nc, es0, "p0")
            if 3 in phases:
                R_uv = Res("uvcast")
                for (src, c0) in ((pu, 0), (pv, DM)):
                    for i in range(16):
                        dma(lambda e, src=src, c0=c0, i=i: e.dma_start(out=uv_bf[i * 1024:(i + 1) * 1024, c0:c0 + DM], in_=src[i * 1024:(i + 1) * 1024, :]), [], [], R_uv, eng="pool")
            op("pool", lambda e: e.memset(ident_f[:], 0.0), [], [ident_f])
            op("pool", lambda e: e.affine_select(out=ident_f[:], in_=ident_f[:], compare_op=ALU.not_equal, fill=1.0, base=0, pattern=[[-1, 128]], channel_multiplier=1), [ident_f], [ident_f])
            op("dve", lambda e: e.tensor_copy(out=ident_b[:], in_=ident_f[:]), [ident_f], [ident_b])
            op("dve", lambda e: e.memset(ones_b[:], 1.0), [], [ones_b])
            op("pool", lambda e: e.iota(iota16[:], pattern=[[1, 16]], base=0, channel_multiplier=0, allow_small_or_imprecise_dtypes=True), [], [iota16])
            for i in range(4):
                dma(lambda e, i=i: e.dma_start(out=gains_b[:, i, :], in_=gains_in[i].partition_broadcast(128)), [], [gains_b], gains_b)
            dma(lambda e: e.dma_start(out=inv_b[:], in_=inv.partition_broadcast(128)), [], [inv_b], inv_b)
            dma(lambda e: e.dma_start(out=ogcol[:], in_=og_in), [], [ogcol], ogcol)
            op("dve", lambda e: e.tensor_scalar(out=ogcol[:], in0=ogcol[:], scalar1=1.0 - LAMBDA_INIT, scalar2=None, op0=ALU.mult), [ogcol], [ogcol])
            lamv = A.sb("lamv", [128, 4, 64], F32); lamp = A.sb("lamp", [128, 2, 64], F32); lams = A.sb("lams", [128, 2], F32)
            for i in range(4):
                dma(lambda e, i=i: e.dma_start(out=lamv[:, i, :], in_=lam_in[i].partition_broadcast(128)), [], [lamv], lamv)
            lv4 = lamv[:, :, :].rearrange("p (a b) d -> p a b d", b=2)
            op("dve", lambda e: e.tensor_tensor(out=lamp[:], in0=lv4[:, :, 0, :], in1=lv4[:, :, 1, :], op=ALU.mult), [lamv], [lamp])
            op("dve", lambda e: e.tensor_reduce(out=lams[:], in_=lamp[:], axis=AX.X, op=ALU.add), [lamp], [lams])
            op("act", lambda e: e.activation(out=lams[:], in_=lams[:], func=AF.Exp), [lams], [lams])
            op("dve", lambda e: e.tensor_tensor(out=neglam[:], in0=lams[:, 1:2], in1=lams[:, 0:1], op=ALU.subtract), [lams], [neglam])
            op("dve", lambda e: e.tensor_scalar(out=neglam[:], in0=neglam[:], scalar1=-LAMBDA_INIT, scalar2=None, op0=ALU.add), [neglam], [neglam])

            c_sb = A.sb("c_sb", [ns, DM], F32); sg = A.sb("sg", [ns, DM], F32); scT = A.sb("scT", [128, 8, ns], F32)
            pc0 = A.ps("pc0", [128, 8, ns], F32)
            dma(lambda e: e.dma_start(out=c_sb[:], in_=c_in), [], [c_sb], c_sb)
            op("act", lambda e: e.activation(out=sg[:], in_=c_sb[:], func=AF.Sigmoid), [c_sb], [sg])
            op("dve", lambda e: e.tensor_tensor(out=c_sb[:], in0=c_sb[:], in1=sg[:], op=ALU.mult), [c_sb, sg], [c_sb])
            for kc in range(8):
                op("pe", lambda e, kc=kc: e.transpose(out=pc0[:, kc, :], in_=c_sb[0:ns, kc * 128:(kc + 1) * 128], identity=ident_f[0:ns, 0:ns]), [c_sb, ident_f], [pc0])
            op("dve", lambda e: e.tensor_copy(out=scT[:], in_=pc0[:]), [pc0], [scT])
            wst = [A.sb("wst%d" % i, [128, 8, 512], F32) for i in range(2)]
            b_b = A.sb("b_b", [ns, 6 * DM], F32); mod_sb = A.sb("mod_sb", [ns, 6 * DM], F32)
            pm = [A.ps("pm%d" % i, [ns, 512], F32) for i in range(2)]
            dma(lambda e: e.dma_start(out=b_b[:], in_=b_ada.partition_broadcast(ns)), [], [b_b], b_b)
            w_ada_v = w_ada.rearrange("(kc p) n -> p kc n", p=128)
            for j in range(12):
                ws = wst[j % 2]; pmj = pm[j % 2]
                dma(lambda e, ws=ws, j=j: e.dma_start(out=ws[:], in_=w_ada_v[:, :, j * 512:(j + 1) * 512]), [], [ws], ws)
                for kc in range(8):
                    op("pe", lambda e, ws=ws, pmj=pmj, kc=kc: e.matmul(pmj[:], lhsT=scT[:, kc, :], rhs=ws[:, kc, :], start=(kc == 0), stop=(kc == 7)), [scT, ws], [pmj])
                op("dve", lambda e, pmj=pmj, j=j: e.tensor_tensor(out=mod_sb[:, j * 512:(j + 1) * 512], in0=pmj[:], in1=b_b[:, j * 512:(j + 1) * 512], op=ALU.add), [pmj, b_b], [mod_sb])
            rows_sb = A.sb("rows_sb", [ns, 6, DM], F32); ng_b = A.sb("ng_b", [ns, 2, DM], F32)
            dma(lambda e: e.dma_start(out=ng_b[:, 0, :], in_=n1g.partition_broadcast(ns)), [], [ng_b], ng_b)
            dma(lambda e: e.dma_start(out=ng_b[:, 1, :], in_=n2g.partition_broadcast(ns)), [], [ng_b], ng_b)
            for half in range(2):
                o = half * 3 * DM
                op("dve", lambda e, o=o, half=half: e.scalar_tensor_tensor(out=rows_sb[:, half * 3 + 0, :], in0=mod_sb[:, o + DM:o + 2 * DM], scalar=1.0, in1=ng_b[:, half, :], op0=ALU.add, op1=ALU.mult), [mod_sb, ng_b], [rows_sb])
                op("dve", lambda e, o=o, half=half: e.tensor_copy(out=rows_sb[:, half * 3 + 1, :], in_=mod_sb[:, o:o + DM]), [mod_sb], [rows_sb])
                op("dve", lambda e, o=o, half=half: e.tensor_copy(out=rows_sb[:, half * 3 + 2, :], in_=mod_sb[:, o + 2 * DM:o + 3 * DM]), [mod_sb], [rows_sb])
            dma(lambda e: e.dma_start(out=rows_d, in_=rows_sb[:]), [rows_sb], [], rows_sb)
            pc1 = A.ps("pc1", [128, 4, 8, ns], F32)
            for wi, ri in enumerate((0, 1, 3, 4)):
                for kc in range(8):
                    op("pe", lambda e, wi=wi, ri=ri, kc=kc: e.transpose(out=pc1[:, wi, kc, :], in_=rows_sb[0:ns, ri, kc * 128:(kc + 1) * 128], identity=ident_f[0:ns, 0:ns]), [rows_sb, ident_f], [pc1])
            op("dve", lambda e: e.tensor_copy(out=cols[:], in_=pc1[:]), [pc1], [cols])

            S.barrier()
            S.flush()

        def rope_alloc(A):
            return (A.sb("posi", [128, NT], I32), A.sb("posf", [128, NT], F32), A.sb("ang", [128, NT, 32], F32),
                    A.sb("a2", [128, NT, 32], F32), A.sb("ki", [128, NT, 32], I32), A.sb("kf", [128, NT, 32], F32))

        def rope_tables(rb, s, cos_t, sin_t):
            posi, posf, ang, a2, ki, kf = rb
            dma(lambda e: e.dma_start(out=posi[:], in_=pos[s]), [], [posi], posi)
            op("dve", lambda e: e.tensor_copy(out=posf[:], in_=posi[:]), [posi], [posf])
            op("dve", lambda e: e.tensor_tensor(out=ang[:], in0=posf[:, :].unsqueeze(2).to_broadcast([128, NT, 32]), in1=inv_b[:, :].unsqueeze(1).to_broadcast([128, NT, 32]), op=ALU.mult), [posf, inv_b], [ang])
            for (shift, dst) in ((0.0, sin_t), (0.5 * math.pi, cos_t)):
                op("dve", lambda e, shift=shift: e.tensor_scalar(out=a2[:], in0=ang[:], scalar1=shift, scalar2=None, op0=ALU.add), [ang], [a2])
                op("dve", lambda e: e.tensor_scalar(out=ki[:], in0=a2[:], scalar1=1.0 / TWO_PI, scalar2=None, op0=ALU.mult), [a2], [ki])
                op("dve", lambda e: e.tensor_copy(out=kf[:], in_=ki[:]), [ki], [kf])
                op("dve", lambda e: e.scalar_tensor_tensor(out=a2[:], in0=kf[:], scalar=-TWO_PI, in1=a2[:], op0=ALU.mult, op1=ALU.add), [kf, a2], [a2])
                op("dve", lambda e: e.tensor_scalar(out=kf[:], in0=a2[:], scalar1=math.pi, scalar2=TWO_PI, op0=ALU.is_gt, op1=ALU.mult), [a2], [kf])
                op("dve", lambda e: e.tensor_tensor(out=a2[:], in0=a2[:], in1=kf[:], op=ALU.subtract), [a2, kf], [a2])
                op("dve", lambda e: e.tensor_scalar(out=kf[:], in0=a2[:], scalar1=-math.pi, scalar2=TWO_PI, op0=ALU.is_lt, op1=ALU.mult), [a2], [kf])
                op("dve", lambda e: e.tensor_tensor(out=a2[:], in0=a2[:], in1=kf[:], op=ALU.add), [a2, kf], [a2])
                op("act", lambda e, dst=dst: e.activation(out=dst[:], in_=a2[:], func=AF.Sin), [a2], [dst])

        def rms_rstd(A, ssq_ap, n, res, nm):
            op("dve", lambda e: e.tensor_scalar(out=ssq_ap, in0=ssq_ap, scalar1=1.0 / n, scalar2=EPS, op0=ALU.mult, op1=ALU.add), [res], [res])
            op("act", lambda e: e.activation(out=ssq_ap, in_=ssq_ap, func=AF.Sqrt), [res], [res])
            op("dve", lambda e: e.reciprocal(out=ssq_ap, in_=ssq_ap), [res], [res])

        if 1 in phases:
          with ExitStack() as es1:
            A = Alloc(nc, es1, "p1")
            w_in_bf = A.sb("w_in_bf", [128, 8, IN_COLS], BF16)
            w_in_v = w_in.rearrange("(kc p) n -> p kc n", p=128)
            nch = (IN_COLS + 511) // 512
            for j in range(nch):
                lo = j * 512; hi = min(IN_COLS, lo + 512)
                dma(lambda e, lo=lo, hi=hi: e.dma_start(out=w_in_bf[:, :, lo:hi], in_=w_in_v[:, :, lo:hi]), [], [w_in_bf], w_in_bf, eng="pool")
            cos_tr = [A.sb("cos_t%d" % i, [128, NT, 32], F32) for i in range(2)]; sin_tr = [A.sb("sin_t%d" % i, [128, NT, 32], F32) for i in range(2)]
            xt = [A.sb("xt%d" % i, [128, DM], F32) for i in range(2)]
            junk = A.sb("junk", [128, DM], BF16); xnr = [A.sb("xn%d" % i, [128, DM], BF16) for i in range(2)]
            ssq = [A.sb("ssq%d" % i, [128, 1], F32) for i in range(2)]
            hT = [A.sb("hT%d" % i, [128, 8, 128], BF16) for i in range(2)]
            projr = [A.sb("proj%d" % i, [128, NPROJ], F32) for i in range(2)]
            sqbr = [A.sb("sqb%d" % i, [128, 1600], F32) for i in range(2)]; ssqgr = [A.sb("ssqg%d" % i, [128, 25], F32) for i in range(2)]
            rqr = [A.sb("rq%d" % i, [128, 34, 64], BF16) for i in range(2)]
            tv = [A.sb("tv%d" % i, [128, 16, 32], F32) for i in range(2)]
            tp = [A.sb("tp%d" % i, [128, 9, 32], F32) for i in range(2)]
            dvb = [A.sb("dvb%d" % i, [128, 576], BF16) for i in range(2)]
            iwb = [A.sb("iwb%d" % i, [128, 8], F32) for i in range(2)]
            gs = [A.sb("gs%d" % i, [128, 16, 128], BF16) for i in range(2)]
            qTs = [A.sb("qTs%d" % i, [64, 34, 128], BF16) for i in range(2)]
            pTr = [A.ps("pT%d" % i, [128, 8, 128], BF16) for i in range(2)]
            pp = [A.ps("pp%d" % i, [128, 512], F32) for i in range(2)]
            pg = [A.ps("pg%d" % i, [128, 4, 128], F32) for i in range(2)]
            pq = [A.ps("pq%d" % i, [64, 8, 128], BF16) for i in range(2)]
            bounds = [0, 512, 1024, 1536, 2048, 2560, NPROJ]
            rb = rope_alloc(A)
            def p1_tile(s, t, gi):
                i2 = gi % 2
                r0 = s * T_SEQ + t * 128
                xti = xt[i2]; ssi = ssq[i2]; hTi = hT[i2]
                xn = xnr[i2]; proj = projr[i2]; sqb = sqbr[i2]; ssqg = ssqgr[i2]; rq = rqr[i2]; pT = pTr[i2]
                cos_t = cos_tr[s % 2]; sin_t = sin_tr[s % 2]
                dma(lambda e, xti=xti, r0=r0: e.dma_start(out=xti[:], in_=x[r0:r0 + 128, :]), [], [xti], xti)
                op("act", lambda e, xti=xti, ssi=ssi: e.activation(out=junk[:], in_=xti[:], func=AF.Square, accum_out=ssi[:, 0:1]), [xti], [junk, ssi])
                rms_rstd(A, ssi[:, 0:1], float(DM), ssi, "x")
                op("act", lambda e, xti=xti, ssi=ssi: e.activation(out=xn[:], in_=xti[:], func=AF.Copy, scale=ssi[:, 0:1]), [xti, ssi], [xn])
                yield
                for kc in range(8):
                    op("pe", lambda e, kc=kc: e.transpose(out=pT[:, kc, :], in_=xn[:, kc * 128:(kc + 1) * 128], identity=ident_b[:]), [xn, ident_b], [pT])
                for kc in range(8):
                    op("dve", lambda e, kc=kc, hTi=hTi, s=s: e.tensor_scalar(out=hTi[:, kc, :], in0=pT[:, kc, :], scalar1=cols[:, 0, kc, s:s + 1], scalar2=cols[:, 1, kc, s:s + 1], op0=ALU.mult, op1=ALU.add), [pT, cols], [hTi])
                yield
                for j in range(6):
                    lo, hi = bounds[j], bounds[j + 1]; w = hi - lo
                    ppj = pp[j % 2]
                    for kc in range(8):
                        op("pe", lambda e, ppj=ppj, kc=kc, lo=lo, hi=hi, w=w, hTi=hTi: e.matmul(ppj[:, 0:w], lhsT=hTi[:, kc, :], rhs=w_in_bf[:, kc, lo:hi], start=(kc == 0), stop=(kc == 7)), [hTi, w_in_bf], [ppj])
                    op("act", lambda e, ppj=ppj, lo=lo, hi=hi, w=w: e.activation(out=proj[:, lo:hi], in_=ppj[:, 0:w], func=AF.Copy), [ppj], [proj])
                    if j == 2 or j == 5:
                        yield
                gsi = gs[i2]

                def gates_half(ra, rb_):
                    for r in range(ra, rb_):
                        pgr = pg[r % 2]
                        for j in range(4):
                            gc = r * 4 + j
                            for kc in range(8):
                                op("pe", lambda e, pgr=pgr, j=j, gc=gc, kc=kc: e.matmul(pgr[:, j, :], lhsT=w_in_bf[:, kc, GATE0 + gc * 128:GATE0 + (gc + 1) * 128], rhs=hTi[:, kc, :], start=(kc == 0), stop=(kc == 7)), [hTi, w_in_bf], [pgr])
                        op("act", lambda e, pgr=pgr, r=r: e.activation(out=gsi[:, r * 4:(r + 1) * 4, :], in_=pgr[:], func=AF.Sigmoid), [pgr], [gsi])
                gates_half(0, 2)
                yield
                dvi = dvb[i2]; iwi = iwb[i2]
                op("pool", lambda e, dvi=dvi: e.tensor_copy(out=dvi[:, 0:512], in_=proj[:, 1024:1536]), [proj], [dvi])
                op("pool", lambda e, dvi=dvi: e.tensor_copy(out=dvi[:, 512:576], in_=proj[:, 2112:2176]), [proj], [dvi])
                op("pool", lambda e, iwi=iwi: e.tensor_copy(out=iwi[:], in_=proj[:, 2752:2760]), [proj], [iwi])
                dma(lambda e, dvi=dvi, s=s, t=t: e.dma_start(out=dv_s[s][t * 128:(t + 1) * 128, :], in_=dvi[:]), [dvi], [], dvi)
                dma(lambda e, iwi=iwi, s=s, t=t: e.dma_start(out=iw_s[s][t * 128:(t + 1) * 128, :], in_=iwi[:]), [iwi], [], iwi)
                op("pool", lambda e: e.tensor_tensor(out=sqb[:, 0:1024], in0=proj[:, 0:1024], in1=proj[:, 0:1024], op=ALU.mult), [proj], [sqb])
                op("pool", lambda e: e.tensor_tensor(out=sqb[:, 1024:1600], in0=proj[:, 1536:2112], in1=proj[:, 1536:2112], op=ALU.mult), [proj], [sqb])
                op("dve", lambda e: e.tensor_reduce(out=ssqg[:, 0:25], in_=sqb[:, :].rearrange("p (g d) -> p g d", d=64), axis=AX.X, op=ALU.add), [sqb], [ssqg])
                rms_rstd(A, ssqg[:, :], 64.0, ssqg, "g")
                pA = proj[:, 0:1024].rearrange("p (g d) -> p g d", d=64)
                pB = proj[:, 1536:2112].rearrange("p (g d) -> p g d", d=64)
                op("dve", lambda e, pA=pA: e.tensor_tensor(out=pA, in0=pA, in1=ssqg[:, 0:16].unsqueeze(2).to_broadcast([128, 16, 64]), op=ALU.mult), [proj, ssqg], [proj])
                op("dve", lambda e, pB=pB: e.tensor_tensor(out=pB, in0=pB, in1=ssqg[:, 16:25].unsqueeze(2).to_broadcast([128, 9, 64]), op=ALU.mult), [proj, ssqg], [proj])
                op("dve", lambda e, pA=pA: e.tensor_tensor(out=pA[:, 0:8, :], in0=pA[:, 0:8, :], in1=gains_b[:, 0, :].unsqueeze(1).to_broadcast([128, 8, 64]), op=ALU.mult), [proj, gains_b], [proj])
                op("dve", lambda e, pA=pA: e.tensor_tensor(out=pA[:, 8:16, :], in0=pA[:, 8:16, :], in1=gains_b[:, 1, :].unsqueeze(1).to_broadcast([128, 8, 64]), op=ALU.mult), [proj, gains_b], [proj])
                op("dve", lambda e, pB=pB: e.tensor_tensor(out=pB[:, 0:8, :], in0=pB[:, 0:8, :], in1=gains_b[:, 2, :].unsqueeze(1).to_broadcast([128, 8, 64]), op=ALU.mult), [proj, gains_b], [proj])
                op("dve", lambda e, pB=pB: e.tensor_tensor(out=pB[:, 8:9, :], in0=pB[:, 8:9, :], in1=gains_b[:, 3, :].unsqueeze(1).to_broadcast([128, 1, 64]), op=ALU.mult), [proj, gains_b], [proj])
                yield
                gates_half(2, 4)
                dma(lambda e: e.dma_start(out=gT_s[s][:, :, t * 128:(t + 1) * 128], in_=gsi[:]), [gsi], [], gsi)
                yield
                for (lo, G, g0, eng, tmps) in ((0, 16, 0, "dve", tv), (1536, 9, 16, "dve", tv), (2176, 9, 25, "pool", tp)):
                    X = proj[:, lo:lo + G * 64].rearrange("p (g h d) -> p g h d", h=2, d=32)
                    O = rq[:, g0:g0 + G, :].rearrange("p g (h d) -> p g h d", h=2)
                    cb = cos_t[:, t, :].unsqueeze(1).to_broadcast([128, G, 32])
                    sb_ = sin_t[:, t, :].unsqueeze(1).to_broadcast([128, G, 32])
                    t1 = tmps[0][:, 0:G, :]; t2 = tmps[1][:, 0:G, :]
                    rr = [proj, cos_t, sin_t]
                    op(eng, lambda e, X=X, cb=cb, t1=t1: e.tensor_tensor(out=t1, in0=X[:, :, 0, :], in1=cb, op=ALU.mult), rr, [tmps[0]])
                    op(eng, lambda e, X=X, sb_=sb_, t2=t2: e.tensor_tensor(out=t2, in0=X[:, :, 1, :], in1=sb_, op=ALU.mult), rr, [tmps[1]])
                    op(eng, lambda e, O=O, t1=t1, t2=t2: e.tensor_tensor(out=O[:, :, 0, :], in0=t1, in1=t2, op=ALU.subtract), [tmps[0], tmps[1]], [rq])
                    op(eng, lambda e, X=X, cb=cb, t1=t1: e.tensor_tensor(out=t1, in0=X[:, :, 1, :], in1=cb, op=ALU.mult), rr, [tmps[0]])
                    op(eng, lambda e, X=X, sb_=sb_, t2=t2: e.tensor_tensor(out=t2, in0=X[:, :, 0, :], in1=sb_, op=ALU.mult), rr, [tmps[1]])
                    op(eng, lambda e, O=O, t1=t1, t2=t2: e.tensor_tensor(out=O[:, :, 1, :], in0=t1, in1=t2, op=ALU.add), [tmps[0], tmps[1]], [rq])
                yield
                qTi = qTs[i2]
                for bi, (g0, g1) in enumerate(((0, 8), (8, 16), (16, 24), (24, 32), (32, 34))):
                    pqb = pq[bi % 2]
                    for g in range(g0, g1):
                        op("pe", lambda e, pqb=pqb, g=g, g0=g0: e.transpose(out=pqb[:, g - g0, :], in_=rq[:, g, :], identity=ident_b[:]), [rq, ident_b], [pqb])
                    op("act", lambda e, pqb=pqb, g0=g0, g1=g1, qTi=qTi: e.activation(out=qTi[:, g0:g1, :], in_=pqb[:, 0:g1 - g0, :], func=AF.Copy), [pqb], [qTi])
                dma(lambda e, qTi=qTi, s=s, t=t: e.dma_start(out=qT_s[s][:, :, t * 128:(t + 1) * 128], in_=qTi[:]), [qTi], [], qTi)

            tl = [(s_, t_) for s_ in range(ns) for t_ in range(NT)]
            gens = {}

            def pull(i):
                if i < 0 or i >= len(tl):
                    return
                if i not in gens:
                    s_, t_ = tl[i]
                    if t_ == 0:
                        rope_tables(rb, s_, cos_tr[s_ % 2], sin_tr[s_ % 2])
                    gens[i] = p1_tile(s_, t_, i)
                next(gens[i], None)

            pull(0); pull(0); pull(1)
            for i in range(len(tl) + 1):
                pull(i)
                pull(i - 1)
                pull(i)
                pull(i + 1)
                pull(i)
                pull(i)
                pull(i)
                pull(i)
                pull(i + 2)
            S.barrier()
            S.flush()

        if 2 in phases:
          with ExitStack() as es2:
            P2 = Alloc(nc, es2, "p2")
            wba = P2.sb("wba", [128, 4, DM], BF16); wbb = P2.sb("wbb", [128, 4, DM], BF16); wo = P2.sb("wo", [128, 8, DM], BF16)
            oaT = P2.sb("oaT", [128, 4, T_SEQ], BF16); obT = P2.sb("obT", [128, 4, T_SEQ], BF16)
            NMT = NT * (NT + 1) // 2
            maskT_all = P2.sb("maskT_all", [128, NMT, 128], BF16)
            moff = [t * (t + 1) // 2 for t in range(NT)]
            for (src, dst) in ((w_ba, wba), (w_bb, wbb), (w_o, wo)):
                sv_ = src.rearrange("(j p) n -> p j n", p=128)
                for j0 in range(0, sv_.shape[1], 2):
                    dma(lambda e, sv_=sv_, dst=dst, j0=j0: e.dma_start(out=dst[:, j0:j0 + 2, :], in_=sv_[:, j0:j0 + 2, :]), [], [dst], dst, eng="pool")
            for s in range(ns):
                with ExitStack() as esa:
                  if 'A' in stages:
                      A = Alloc(nc, esa, "p2a%d" % s)
                      dvs = A.sb("dvs", [128, NT, 512], BF16)
                      dkq = [A.sb("dkq%d" % i, [64, 2, T_SEQ], BF16) for i in range(2)]
                      PT = [A.sb("PT%d" % i, [128, 512], BF16) for i in range(3)]
                      o0n = A.sb("o0n", [128, T_SEQ], F32)
                      rz = A.sb("rz", [128, 512], F32); o1n = A.sb("o1n", [128, 512], F32); dd = A.sb("dd", [128, 512], F32)
                      dsq = A.sb("dsq", [128, 512], BF16); rstd = A.sb("rstd", [128, 512], F32)
                      ps_s = [A.ps("ps_s%d" % i, [128, 512], F32) for i in range(2)]
                      ps_o = [A.ps("ps_o%d" % i, [128, 512], F32) for i in range(2)]; ps_z = [A.ps("ps_z%d" % i, [128, 512], F32) for i in range(2)]
                      ikT = A.sb("ikT", [64, T_SEQ], BF16); iws = A.sb("iws", [128, NT, 8], F32)
                      iqT = [A.sb("iqT%d" % i, [64, 8, 128], BF16) for i in range(2)]
                      score2 = [A.sb("score%d" % i, [128, T_SEQ], F32) for i in range(2)]
                      rl = [A.sb("rl%d" % i, [128, 512], F32) for i in range(2)]
                      blo = [A.sb("blo%d" % i, [128, 1], F32) for i in range(2)]; bd0 = [A.sb("bd0%d" % i, [128, 1], F32) for i in range(2)]; bmid = [A.sb("bmid%d" % i, [128, 1], F32) for i in range(2)]
                      bcnt = [A.sb("bcnt%d" % i, [128, 1], F32) for i in range(2)]; bg = [A.sb("bg%d" % i, [128, 1], F32) for i in range(2)]; bjunk = A.sb("bjunk", [128, T_SEQ], BF16)
                      maskb = A.sb("maskb", [128, T_SEQ], BF16)
                      ps_i = A.ps("ps_i", [128, 512], F32); ps_m = A.ps("ps_m", [128, 8, 128], BF16)
                      dma(lambda e: e.dma_start(out=dvs[:], in_=dv_s[s].rearrange("(t p) c -> p t c", p=128)[:, :, 0:512]), [], [dvs], dvs)
                      dma(lambda e: e.dma_start(out=ikT[:], in_=qT_s[s][:, 33, :]), [], [ikT], ikT)
                      dma(lambda e: e.dma_start(out=iws[:], in_=iw_s[s].rearrange("(t p) c -> p t c", p=128)), [], [iws], iws)

                      def b_part1_gen():
                          ii = 0
                          for t0 in range(0, NT, 2):
                              pair = (t0, t0 + 1)
                              for pi, t in enumerate(pair):
                                  iq_ = iqT[t % 2]; sc_ = score2[pi]
                                  tsl = slice(t * 128, (t + 1) * 128)
                                  dma(lambda e, iq_=iq_, tsl=tsl: e.dma_start(out=iq_[:], in_=qT_s[s][:, 25:33, tsl]), [], [iq_], iq_)
                                  nk = (t + 1) * 128
                                  for kc in range((nk + 511) // 512):
                                      w = min(512, nk - kc * 512)
                                      ksl = slice(kc * 512, kc * 512 + w)
                                      for hh in range(8):
                                          rli = rl[ii % 2]; ii += 1
                                          op("pe", lambda e, iq_=iq_, hh=hh, ksl=ksl, w=w: e.matmul(ps_i[:, 0:w], lhsT=iq_[:, hh, :], rhs=ikT[:, ksl], start=True, stop=True), [iq_, ikT], [ps_i])
                                          op("act", lambda e, rli=rli, w=w: e.activation(out=rli[:, 0:w], in_=ps_i[:, 0:w], func=AF.Relu), [ps_i], [rli])
                                          if hh == 0:
                                              op("dve", lambda e, rli=rli, ksl=ksl, w=w, hh=hh, t=t, sc_=sc_: e.tensor_scalar(out=sc_[:, ksl], in0=rli[:, 0:w], scalar1=iws[:, t, hh:hh + 1], scalar2=None, op0=ALU.mult), [rli, iws], [sc_])
                                          else:
                                              op("dve", lambda e, rli=rli, ksl=ksl, w=w, hh=hh, t=t, sc_=sc_: e.scalar_tensor_tensor(out=sc_[:, ksl], in0=rli[:, 0:w], scalar=iws[:, t, hh:hh + 1], in1=sc_[:, ksl], op0=ALU.mult, op1=ALU.add), [rli, iws, sc_], [sc_])
                                          yield (w / 960.0 + 0.15, 0.4)
                                  op("dve", lambda e, t=t, sc_=sc_: e.memset(sc_[0:64, t * 128 + 64:(t + 1) * 128], NEG), [], [sc_])
                              if t0 >= 2:
                                  for pi, t in enumerate(pair):
                                      nk = (t + 1) * 128; sc_ = score2[pi]; lo_ = blo[pi]; d0_ = bd0[pi]
                                      op("dve", lambda e, nk=nk, sc_=sc_, lo_=lo_: e.tensor_reduce(out=lo_[:], in_=sc_[:, 0:nk - 64], axis=AX.X, op=ALU.min), [sc_], [lo_])
                                      op("dve", lambda e, nk=nk, sc_=sc_, d0_=d0_: e.tensor_reduce(out=d0_[:], in_=sc_[:, 0:nk], axis=AX.X, op=ALU.max), [sc_], [d0_])
                                  for pi in range(2):
                                      lo_ = blo[pi]; d0_ = bd0[pi]
                                      op("dve", lambda e, lo_=lo_, d0_=d0_: e.tensor_tensor(out=d0_[:], in0=d0_[:], in1=lo_[:], op=ALU.subtract), [d0_, lo_], [d0_])
                                  for k in range(24):
                                      ck = 2.0 ** (-(k + 1))
                                      for pi in range(2):
                                          lo_ = blo[pi]; d0_ = bd0[pi]; mid_ = bmid[pi]
                                          op("dve", lambda e, ck=ck, lo_=lo_, d0_=d0_, mid_=mid_: e.scalar_tensor_tensor(out=mid_[:], in0=d0_[:], scalar=ck, in1=lo_[:], op0=ALU.mult, op1=ALU.add), [d0_, lo_], [mid_])
                                      for pi, t in enumerate(pair):
                                          nk = (t + 1) * 128; sc_ = score2[pi]; mid_ = bmid[pi]; cnt_ = bcnt[pi]
                                          op("dve", lambda e, nk=nk, sc_=sc_, mid_=mid_, cnt_=cnt_: e.tensor_scalar(out=bjunk[:, 0:nk], in0=sc_[:, 0:nk], scalar1=mid_[:, 0:1], scalar2=None, op0=ALU.is_ge, op1=ALU.add, accum_out=cnt_[:, 0:1]), [sc_, mid_], [cnt_])
                                      for pi in range(2):
                                          cnt_ = bcnt[pi]; g_ = bg[pi]
                                          op("dve", lambda e, ck=ck, cnt_=cnt_, g_=g_: e.tensor_scalar(out=g_[:], in0=cnt_[:], scalar1=255.5, scalar2=ck, op0=ALU.is_ge, op1=ALU.mult), [cnt_], [g_])
                                      for pi in range(2):
                                          lo_ = blo[pi]; d0_ = bd0[pi]; g_ = bg[pi]
                                          op("dve", lambda e, lo_=lo_, d0_=d0_, g_=g_: e.scalar_tensor_tensor(out=lo_[:], in0=g_[:], scalar=d0_[:, 0:1], in1=lo_[:], op0=ALU.mult, op1=ALU.add), [g_, d0_, lo_], [lo_])
                                      yield (((pair[0] + 1) * 128 + (pair[1] + 1) * 128) / 960.0 + 1.2, 0.0)
                              for pi, t in enumerate(pair):
                                  nk = (t + 1) * 128; sc_ = score2[pi]; lo_ = blo[pi]
                                  if t0 >= 2:
                                      op("dve", lambda e, nk=nk, sc_=sc_, lo_=lo_: e.tensor_scalar(out=maskb[:, 0:nk], in0=sc_[:, 0:nk], scalar1=lo_[:, 0:1], scalar2=None, op0=ALU.is_ge), [sc_, lo_], [maskb])
                                  else:
                                      op("dve", lambda e, nk=nk, sc_=sc_: e.tensor_scalar(out=maskb[:, 0:nk], in0=sc_[:, 0:nk], scalar1=-1.0e29, scalar2=None, op0=ALU.is_ge), [sc_], [maskb])
                                  yield (nk / 960.0 + 0.2, 0.0)
                                  for kb in range((t + 8) // 8):
                                      k1 = min(t + 1, kb * 8 + 8)
                                      for kt in range(kb * 8, k1):
                                          op("pe", lambda e, kt=kt, kb=kb: e.transpose(out=ps_m[:, kt - kb * 8, :], in_=maskb[:, kt * 128:(kt + 1) * 128], identity=ident_b[:]), [maskb, ident_b], [ps_m])
                                      op("act", lambda e, kb=kb, k1=k1, t=t: e.activation(out=maskT_all[:, moff[t] + kb * 8:moff[t] + k1, :], in_=ps_m[:, 0:k1 - kb * 8, :], func=AF.Copy), [ps_m], [maskT_all])
                                      yield (0.0, 0.1 * (k1 - kb * 8))

                      its = []
                      for h in range(4):
                          for m in range(2):
                              for G in range(4):
                                  for kt in range(4 * G + 4):
                                      its.append((h, m, G, kt, 4 * G + 4))

                      def a_loads(g):
                          dk_ = dkq[g % 2]
                          dma(lambda e, dk_=dk_, g=g: e.dma_start(out=dk_[:, 0, :], in_=qT_s[s][:, g, :]), [], [dk_], dk_)
                          dma(lambda e, dk_=dk_, g=g: e.dma_start(out=dk_[:, 1, :], in_=qT_s[s][:, 8 + g, :]), [], [dk_], dk_)

                      def a_stage1(i):
                          h, m, G, kt, nkt = its[i]
                          dk_ = dkq[(2 * h + m) % 2]; pss = ps_s[acnt["s"] % 2]; acnt["s"] += 1; pt = PT[i % 3]
                          op("pe", lambda e: e.matmul(pss[:], lhsT=dk_[:, 1, kt * 128:(kt + 1) * 128], rhs=dk_[:, 0, G * 512:(G + 1) * 512], start=True, stop=True), [dk_], [pss])
                          op("act", lambda e: e.activation(out=pt[:], in_=pss[:], func=AF.Exp, scale=0.125), [pss], [pt])
                          if kt >= 4 * G:
                              j = kt - 4 * G
                              if j > 0:
                                  op("pool", lambda e: e.memset(pt[:, 0:j * 128], 0.0), [], [pt])
                              op("pool", lambda e: e.memset(pt[64:128, j * 128:j * 128 + 64], 0.0), [], [pt])

                      def a_stage2(i):
                          h, m, G, kt, nkt = its[i]
                          pt = PT[i % 3]
                          grp = (h * 2 + m) * 4 + G
                          pso_ = ps_o[grp % 2]; psz_ = ps_z[grp % 2]
                          op("pe", lambda e: e.matmul(pso_[:], lhsT=dvs[:, kt, h * 128:(h + 1) * 128], rhs=pt[:], start=(kt == 0), stop=(kt == nkt - 1)), [dvs, pt], [pso_])
                          op("pe", lambda e: e.matmul(psz_[:], lhsT=ones_b[:], rhs=pt[:], start=(kt == 0), stop=(kt == nkt - 1)), [ones_b, pt], [psz_])
                          if kt != nkt - 1:
                              return
                          op("dve", lambda e: e.reciprocal(out=rz[:], in_=psz_[:]), [psz_], [rz])
                          gsl = slice(G * 512, (G + 1) * 512)
                          if m == 0:
                              op("dve", lambda e: e.tensor_tensor(out=o0n[:, gsl], in0=pso_[:], in1=rz[:], op=ALU.mult), [pso_, rz], [o0n])
                          else:
                              op("dve", lambda e: e.tensor_tensor(out=o1n[:], in0=pso_[:], in1=rz[:], op=ALU.mult), [pso_, rz], [o1n])
                              op("dve", lambda e: e.scalar_tensor_tensor(out=dd[:], in0=o1n[:], scalar=neglam[:, 0:1], in1=o0n[:, gsl], op0=ALU.mult, op1=ALU.add), [o1n, neglam, o0n], [dd])
                              op("act", lambda e: e.activation(out=dsq[:], in_=dd[:], func=AF.Square), [dd], [dsq])
                              ps_q = ps_s[acnt["s"] % 2]; acnt["s"] += 1
                              op("pe", lambda e: e.matmul(ps_q[:], lhsT=ones_b[:], rhs=dsq[:], start=True, stop=True), [ones_b, dsq], [ps_q])
                              op("dve", lambda e: e.tensor_scalar(out=rstd[:], in0=ps_q[:], scalar1=1.0 / 128, scalar2=EPS, op0=ALU.mult, op1=ALU.add), [ps_q], [rstd])
                              op("act", lambda e: e.activation(out=rstd[:], in_=rstd[:], func=AF.Sqrt), [rstd], [rstd])
                              op("dve", lambda e: e.reciprocal(out=rstd[:], in_=rstd[:]), [rstd], [rstd])
                              op("dve", lambda e: e.tensor_tensor(out=dd[:], in0=dd[:], in1=rstd[:], op=ALU.mult), [dd, rstd], [dd])
                              op("dve", lambda e: e.tensor_scalar(out=oaT[:, h, gsl], in0=dd[:], scalar1=ogcol[:, 0:1], scalar2=None, op0=ALU.mult), [dd, ogcol], [oaT])

                      acnt = {"s": 0}
                      clk = {"pe": 0.0, "dve": 0.0}
                      bgen = b_part1_gen()
                      a_loads(0)
                      a_stage1(0)
                      for i in range(len(its)):
                          h, m, G, kt, nkt = its[i]
                          if G == 0 and kt == 0 and 2 * h + m + 1 < 8:
                              a_loads(2 * h + m + 1)
                          if i + 1 < len(its):
                              a_stage1(i + 1)
                          a_stage2(i)
                          clk["pe"] += 1.25
                          while clk["dve"] < clk["pe"]:
                              c = next(bgen, None)
                              if c is None:
                                  break
                              clk["dve"] += c[0]; clk["pe"] += c[1]
                      for _ in bgen:
                          pass
                      S.barrier(); S.flush()

                with ExitStack() as esb:
                  if 'B' in stages:
                      A = Alloc(nc, esb, "p2b%d" % s)
                      skT = A.sb("skT", [64, T_SEQ], BF16)
                      svd = A.sb("svd", [128, NT, 128], BF16)
                      sqT = [A.sb("sqT%d" % i, [64, 8, 128], BF16) for i in range(3)]
                      PT = [A.sb("PTb%d" % i, [128, 4, 128], BF16) for i in range(3)]
                      oS = [A.sb("oS%d" % i, [128, 4, 128], F32) for i in range(2)]; zS = [A.sb("zS%d" % i, [128, 4, 128], F32) for i in range(2)]
                      ps_s = [A.ps("ps_sb%d" % i, [128, 4, 128], F32) for i in range(2)]
                      ps_o = [A.ps("ps_ob%d" % i, [128, 4, 128], F32) for i in range(2)]; ps_z = [A.ps("ps_zb%d" % i, [128, 4, 128], F32) for i in range(2)]
                      dma(lambda e: e.dma_start(out=skT[:], in_=qT_s[s][:, 24, :]), [], [skT], skT)
                      svv = dv_s[s].rearrange("(t p) c -> p t c", p=128)
                      dma(lambda e: e.dma_start(out=svd[:, :, 0:64], in_=svv[:, :, 512:576]), [], [svd], svd)
                      dma(lambda e: e.dma_start(out=svd[:, :, 64:128], in_=svv[:, :, 512:576]), [], [svd], svd)
                      its = [(t, half, kt) for t in range(NT) for half in range(2) for kt in range(t + 1)]

                      def sq_load(t):
                          sq_ = sqT[t % 3]
                          dma(lambda e: e.dma_start(out=sq_[:], in_=qT_s[s][:, 16:24, t * 128:(t + 1) * 128]), [], [sq_], sq_)

                      def st1(i):
                          t, half, kt = its[i]
                          sq_ = sqT[t % 3]
                          pss = ps_s[i % 2]; pt = PT[i % 3]
                          op("pe", lambda e: e.matmul(pss[:], lhsT=skT[:, kt * 128:(kt + 1) * 128], rhs=sq_[:, 4 * half:4 * half + 4, :], start=True, stop=True), [skT, sq_], [pss])
                          op("act", lambda e: e.activation(out=pt[:], in_=pss[:], func=AF.Exp, scale=0.125), [pss], [pt])
                          op("pool", lambda e: e.tensor_tensor(out=pt[:], in0=pt[:], in1=maskT_all[:, moff[t] + kt, :].unsqueeze(1).to_broadcast([128, 4, 128]), op=ALU.mult), [pt, maskT_all], [pt])

                      def st2(i):
                          t, half, kt = its[i]
                          pt = PT[i % 3]
                          grp = 2 * t + half
                          pso_ = ps_o[grp % 2]; psz_ = ps_z[grp % 2]
                          tsl = slice(t * 128, (t + 1) * 128)
                          op("pe", lambda e: e.matmul(pso_[:], lhsT=svd[:, kt, :], rhs=pt[:], start=(kt == 0), stop=(kt == t)), [svd, pt], [pso_])
                          op("pe", lambda e: e.matmul(psz_[:], lhsT=ones_b[:], rhs=pt[:], start=(kt == 0), stop=(kt == t)), [ones_b, pt], [psz_])
                          if kt != t:
                              return
                          oS_ = oS[grp % 2]; zS_ = zS[grp % 2]
                          op("act", lambda e: e.activation(out=oS_[:], in_=pso_[:], func=AF.Copy), [pso_], [oS_])
                          op("dve", lambda e: e.reciprocal(out=zS_[:], in_=psz_[:]), [psz_], [zS_])
                          for par in range(2):
                              psl = slice(par * 64, par * 64 + 64)
                              op("dve", lambda e, psl=psl, par=par: e.tensor_tensor(out=obT[psl, 2 * half:2 * half + 2, tsl], in0=oS_[psl, par::2, :], in1=zS_[psl, par::2, :], op=ALU.mult), [oS_, zS_], [obT])

                      sq_load(0); sq_load(1)
                      st1(0)
                      for i in range(len(its)):
                          t, half, kt = its[i]
                          if half == 0 and kt == 0 and t + 2 < NT:
                              sq_load(t + 2)
                          if i + 1 < len(its):
                              st1(i + 1)
                          st2(i)
                      S.barrier(); S.flush()

                with ExitStack() as esc:
                  if 'C' in stages:
                      A = Alloc(nc, esc, "p2c%d" % s)
                      gT = [A.sb("gT%d" % i, [128, 2, 512], BF16) for i in range(2)]
                      t1 = A.sb("t1", [128, 512], F32); t2 = A.sb("t2", [128, 512], F32)
                      mT = A.sb("mT", [128, 8, 512], BF16)
                      xt = [A.sb("xtc%d" % i, [128, DM], F32) for i in range(2)]
                      g1b = A.sb("g1b", [128, DM], F32); tmp3 = A.sb("tmp3", [128, DM], F32)
                      ps_a = A.ps("ps_a", [128, 512], F32); ps_b = A.ps("ps_b", [128, 512], F32)
                      ps_x = [A.ps("ps_x%d" % i, [128, 512], F32) for i in range(2)]
                      dma(lambda e: e.dma_start(out=g1b[:], in_=rows_d[s, 2:3, :].partition_broadcast(128)), [], [g1b], g1b)
                      if dbg:
                          dma(lambda e: e.dma_start(out=dbg_oa[s], in_=oaT[:]), [oaT], [], oaT)
                          dma(lambda e: e.dma_start(out=dbg_ob[s], in_=obT[:]), [obT], [], obT)
                          dma(lambda e: e.dma_start(out=dbg_mk[s], in_=maskT_all[:]), [maskT_all], [], maskT_all)
                      ic = 0
                      for G in range(4):
                          gsl = slice(G * 512, (G + 1) * 512)
                          for c in range(8):
                              gTi = gT[ic % 2]; ic += 1
                              dma(lambda e, gTi=gTi, c=c, gsl=gsl: e.dma_start(out=gTi[:], in_=gT_s[s][:, c::8, gsl]), [], [gTi], gTi)
                              for j in range(4):
                                  op("pe", lambda e, j=j, c=c, gsl=gsl: e.matmul(ps_a[:], lhsT=wba[:, j, c * 128:(c + 1) * 128], rhs=oaT[:, j, gsl], start=(j == 0), stop=(j == 3)), [wba, oaT], [ps_a])
                              for j in range(4):
                                  op("pe", lambda e, j=j, c=c, gsl=gsl: e.matmul(ps_b[:], lhsT=wbb[:, j, c * 128:(c + 1) * 128], rhs=obT[:, j, gsl], start=(j == 0), stop=(j == 3)), [wbb, obT], [ps_b])
                              op("dve", lambda e, gTi=gTi: e.tensor_tensor(out=t1[:], in0=ps_a[:], in1=gTi[:, 0, :], op=ALU.mult), [ps_a, gTi], [t1])
                              op("dve", lambda e, gTi=gTi: e.tensor_tensor(out=t2[:], in0=ps_b[:], in1=gTi[:, 1, :], op=ALU.mult), [ps_b, gTi], [t2])
                              op("pool", lambda e, c=c: e.tensor_tensor(out=mT[:, c, :], in0=t1[:], in1=t2[:], op=ALU.add), [t1, t2], [mT])
                          for q in range(4):
                              tt = 4 * G + q
                              r0 = s * T_SEQ + tt * 128
                              xti = xt[tt % 2]
                              dma(lambda e, xti=xti, r0=r0: e.dma_start(out=xti[:], in_=x[r0:r0 + 128, :]), [], [xti], xti)
                              for half in range(2):
                                  psx = ps_x[half]
                                  for c in range(8):
                                      op("pe", lambda e, psx=psx, c=c, q=q, half=half: e.matmul(psx[:], lhsT=mT[:, c, q * 128:(q + 1) * 128], rhs=wo[:, c, half * 512:(half + 1) * 512], start=(c == 0), stop=(c == 7)), [mT, wo], [psx])
                                  op("dve", lambda e, psx=psx, half=half: e.tensor_tensor(out=tmp3[:, half * 512:(half + 1) * 512], in0=psx[:], in1=g1b[:, half * 512:(half + 1) * 512], op=ALU.mult), [psx, g1b], [tmp3])
                              op("pool", lambda e, xti=xti: e.tensor_tensor(out=xti[:], in0=xti[:], in1=tmp3[:], op=ALU.add), [xti, tmp3], [xti])
                              dma(lambda e, xti=xti, r0=r0: e.dma_start(out=out[r0:r0 + 128, :], in_=xti[:]), [xti], [], xti)
                      S.barrier(); S.flush()

        if 3 in phases:
          with ExitStack() as es3:
            A = Alloc(nc, es3, "p3")
            wq_bf = A.sb("wq_bf", [128, 8, 2048], BF16); skb = A.sb("skb", [128, 16, 128], BF16)
            w_q_v = w_q.rearrange("(kc p) n -> p kc n", p=128)
            for j in range(4):
                dma(lambda e, j=j: e.dma_start(out=wq_bf[:, :, j * 512:(j + 1) * 512], in_=w_q_v[:, :, j * 512:(j + 1) * 512]), [], [wq_bf], wq_bf, eng="pool")
            dma(lambda e: e.dma_start(out=skb[:], in_=subkT), [], [skb], skb, eng="pool")
            xt = [A.sb("xt%d" % i, [128, DM], F32) for i in range(2)]
            junk = A.sb("junk", [128, DM], BF16); xn = A.sb("xn", [128, DM], BF16)
            ssq = [A.sb("ssq%d" % i, [128, 1], F32) for i in range(2)]
            h2T = A.sb("h2T", [128, 8, 128], BF16)
            h2 = [A.sb("h2_%d" % i, [128, 8, 128], BF16) for i in range(2)]
            tmpf = A.sb("tmpf", [128, DM], F32)
            G2b = [A.sb("G2b%d" % i, [128, DM], F32) for i in range(2)]
            qT = A.sb("qT", [128, 16, 128], BF16)
            sc_sb = A.sb("sc_sb", [128, 16, 128], F32); scw = A.sb("scw", [128, 16, 128], F32)
            v16 = A.sb("v16", [128, 16, 16], F32); i16 = A.sb("i16", [128, 16, 16], U32); i16f = A.sb("i16f", [128, 16, 16], F32)
            cand = A.sb("cand", [128, 8, 256], F32); candw = A.sb("candw", [128, 8, 256], F32)
            sc = A.sb("sc", [128, 8, 16], F32); posu = A.sb("posu", [128, 8, 16], U32)
            au = A.sb("au", [128, 8, 16], U32); bu = A.sb("bu", [128, 8, 16], U32)
            af = A.sb("af", [128, 8, 16], F32); bf_ = A.sb("bf_", [128, 8, 16], F32)
            eq = A.sb("eq", [128, 8, 16, 16], F32)
            e1 = A.sb("e1", [128, 8, 16], F32); e2 = A.sb("e2", [128, 8, 16], F32)
            eidx = [A.sb("eidx%d" % i, [128, 128], U32) for i in range(2)]
            ex = A.sb("ex", [128, 8, 16], F32); esum = A.sb("esum", [128, 8], F32)
            gg = [A.sb("gg%d" % i, [128, 128], F32) for i in range(2)]
            av = A.sb("av", [128, 128], F32)
            wg8 = [A.sb("wg8_%d" % i, [128, 8], F32) for i in range(2)]
            NR = 20
            uvg = [A.sb("uvg%d" % i, [128, 2 * DM], BF16) for i in range(NR)]
            djunk = A.sb("djunk", [128, DM], BF16)
            dg = [A.sb("dg%d" % i, [128, 128], BF16) for i in range(4)]
            pT = A.ps("pT3", [128, 8, 128], BF16)
            psq = [A.ps("psq%d" % i, [128, 4, 128], F32) for i in range(2)]
            pso = [[A.ps("pso%d_%d" % (i, j), [128, 512], F32) for j in range(2)] for i in range(2)]
            tiles = [(s, t) for s in range(ns) for t in range(NT)]

            def routing_pieces(gi):
                s, t = tiles[gi]
                i2 = gi % 2
                r0 = s * T_SEQ + t * 128
                xti = xt[i2]; ssi = ssq[i2]; eix = eidx[i2]; h2i = h2[i2]; ggi = gg[i2]
                P = []

                def p_load():
                    if t == 0:
                        gb = G2b[s % 2]
                        dma(lambda e: e.dma_start(out=gb[:], in_=rows_d[s, 5:6, :].partition_broadcast(128)), [], [gb], gb)
                    dma(lambda e: e.dma_start(out=xti[:], in_=out[r0:r0 + 128, :]), [], [xti], xti)
                    op("act", lambda e: e.activation(out=junk[:], in_=xti[:], func=AF.Square, accum_out=ssi[:, 0:1]), [xti], [junk, ssi])
                    rms_rstd(A, ssi[:, 0:1], float(DM), ssi, "x")
                    op("act", lambda e: e.activation(out=xn[:], in_=xti[:], func=AF.Copy, scale=ssi[:, 0:1]), [xti, ssi], [xn])
                    for kc in range(8):
                        op("pe", lambda e, kc=kc: e.transpose(out=pT[:, kc, :], in_=xn[:, kc * 128:(kc + 1) * 128], identity=ident_b[:]), [xn, ident_b], [pT])
                    for kc in range(8):
                        op("act", lambda e, kc=kc: e.activation(out=h2T[:, kc, :], in_=pT[:, kc, :], func=AF.Identity, scale=cols[:, 2, kc, s:s + 1], bias=cols[:, 3, kc, s:s + 1]), [pT, cols], [h2T])
                    for kc in range(8):
                        op("pe", lambda e, kc=kc: e.transpose(out=pT[:, kc, :], in_=h2T[:, kc, :], identity=ident_b[:]), [h2T, ident_b], [pT])
                    op("act", lambda e: e.activation(out=h2i[:], in_=pT[:], func=AF.Copy), [pT], [h2i])
                P.append(p_load)

                def p_q(r0_, r1_):
                    def f():
                        for r in range(r0_, r1_):
                            pq_ = psq[r % 2]
                            for j in range(4):
                                hp = r * 4 + j
                                for kc in range(8):
                                    op("pe", lambda e, pq_=pq_, j=j, hp=hp, kc=kc: e.matmul(pq_[:, j, :], lhsT=wq_bf[:, kc, hp * 128:(hp + 1) * 128], rhs=h2T[:, kc, :], start=(kc == 0), stop=(kc == 7)), [wq_bf, h2T], [pq_])
                            op("act", lambda e, pq_=pq_, r=r: e.activation(out=qT[:, r * 4:(r + 1) * 4, :], in_=pq_[:], func=AF.Copy), [pq_], [qT])
                    return f
                P.append(p_q(0, 2)); P.append(p_q(2, 4))

                def p_sc():
                    for r in range(4):
                        pq_ = psq[r % 2]
                        for j in range(4):
                            hp = r * 4 + j
                            op("pe", lambda e, pq_=pq_, j=j, hp=hp: e.matmul(pq_[:, j, :], lhsT=qT[:, hp, :], rhs=skb[:, hp, :], start=True, stop=True), [qT, skb], [pq_])
                        op("act", lambda e, pq_=pq_, r=r: e.activation(out=sc_sb[:, r * 4:(r + 1) * 4, :], in_=pq_[:], func=AF.Copy), [pq_], [sc_sb])
                P.append(p_sc)

                def p_top(h0, h1):
                    def f():
                        for hp in range(h0, h1):
                            op("dve", lambda e, hp=hp: e.max(out=v16[:, hp, 0:8], in_=sc_sb[:, hp, :]), [sc_sb], [v16])
                            op("dve", lambda e, hp=hp: e.max_index(out=i16[:, hp, 0:8], in_max=v16[:, hp, 0:8], in_values=sc_sb[:, hp, :]), [sc_sb, v16], [i16])
                            op("dve", lambda e, hp=hp: e.match_replace(out=scw[:, hp, :], in_to_replace=v16[:, hp, 0:8], in_values=sc_sb[:, hp, :], imm_value=NEG), [sc_sb, v16], [scw])
                            op("dve", lambda e, hp=hp: e.max(out=v16[:, hp, 8:16], in_=scw[:, hp, :]), [scw], [v16])
                            op("dve", lambda e, hp=hp: e.max_index(out=i16[:, hp, 8:16], in_max=v16[:, hp, 8:16], in_values=scw[:, hp, :]), [scw, v16], [i16])
                    return f
                for q in range(4):
                    P.append(p_top(q * 4, q * 4 + 4))

                def p_cand():
                    v4 = v16[:, :, :].rearrange("p (h a) k -> p h a k", a=2)
                    c4 = cand[:, :, :].rearrange("p h (a b) -> p h a b", b=16)
                    op("dve", lambda e: e.tensor_tensor(out=c4, in0=v4[:, :, 0, :].unsqueeze(3).to_broadcast([128, 8, 16, 16]), in1=v4[:, :, 1, :].unsqueeze(2).to_broadcast([128, 8, 16, 16]), op=ALU.add), [v16], [cand])
                P.append(p_cand)

                def p_top2(h0, h1):
                    def f():
                        for h in range(h0, h1):
                            op("dve", lambda e, h=h: e.max(out=sc[:, h, 0:8], in_=cand[:, h, :]), [cand], [sc])
                            op("dve", lambda e, h=h: e.max_index(out=posu[:, h, 0:8], in_max=sc[:, h, 0:8], in_values=cand[:, h, :]), [cand, sc], [posu])
                            op("dve", lambda e, h=h: e.match_replace(out=candw[:, h, :], in_to_replace=sc[:, h, 0:8], in_values=cand[:, h, :], imm_value=NEG), [cand, sc], [candw])
                            op("dve", lambda e, h=h: e.max(out=sc[:, h, 8:16], in_=candw[:, h, :]), [candw], [sc])
                            op("dve", lambda e, h=h: e.max_index(out=posu[:, h, 8:16], in_max=sc[:, h, 8:16], in_values=candw[:, h, :]), [candw, sc], [posu])
                    return f
                for q in range(4):
                    P.append(p_top2(q * 2, q * 2 + 2))

                def p_idx():
                    op("dve", lambda e: e.tensor_single_scalar(out=au[:], in_=posu[:], scalar=4, op=ALU.logical_shift_right), [posu], [au])
                    op("dve", lambda e: e.tensor_single_scalar(out=bu[:], in_=posu[:], scalar=15, op=ALU.bitwise_and), [posu], [bu])
                    op("dve", lambda e: e.tensor_copy(out=af[:], in_=au[:]), [au], [af])
                    op("dve", lambda e: e.tensor_copy(out=bf_[:], in_=bu[:]), [bu], [bf_])
                    op("dve", lambda e: e.tensor_copy(out=i16f[:], in_=i16[:]), [i16], [i16f])
                    i4 = i16f[:, :, :].rearrange("p (h a) k -> p h a k", a=2)
                    io4 = iota16[:, :].unsqueeze(1).unsqueeze(1).to_broadcast([128, 8, 16, 16])
                    for (sel, which, dst) in ((af, 0, e1), (bf_, 1, e2)):
                        op("dve", lambda e, sel=sel: e.tensor_tensor(out=eq[:], in0=io4, in1=sel[:, :, :].unsqueeze(3).to_broadcast([128, 8, 16, 16]), op=ALU.is_equal), [iota16, sel], [eq])
                        op("dve", lambda e, which=which: e.tensor_tensor(out=eq[:], in0=eq[:], in1=i4[:, :, which, :].unsqueeze(2).to_broadcast([128, 8, 16, 16]), op=ALU.mult), [eq, i16f], [eq])
                        op("dve", lambda e, dst=dst: e.tensor_reduce(out=dst[:], in_=eq[:], axis=AX.X, op=ALU.add), [eq], [dst])
                    op("dve", lambda e: e.scalar_tensor_tensor(out=e1[:], in0=e1[:], scalar=128.0, in1=e2[:], op0=ALU.mult, op1=ALU.add), [e1, e2], [e1])
                    op("dve", lambda e: e.tensor_copy(out=eix[:], in_=e1[:, :, :].rearrange("p h k -> p (h k)")), [e1], [eix])
                P.append(p_idx)

                def p_soft():
                    op("dve", lambda e: e.tensor_tensor(out=ex[:], in0=sc[:], in1=sc[:, :, 0:1].to_broadcast([128, 8, 16]), op=ALU.subtract), [sc], [ex])
                    op("act", lambda e: e.activation(out=ex[:], in_=ex[:], func=AF.Exp), [ex], [ex])
                    op("dve", lambda e: e.tensor_reduce(out=esum[:], in_=ex[:], axis=AX.X, op=ALU.add), [ex], [esum])
                    op("dve", lambda e: e.reciprocal(out=esum[:], in_=esum[:]), [esum], [esum])
                    op("dve", lambda e: e.tensor_tensor(out=ggi[:, :].rearrange("p (h k) -> p h k", k=16), in0=ex[:], in1=esum[:, :].unsqueeze(2).to_broadcast([128, 8, 16]), op=ALU.mult), [ex, esum], [ggi])
                P.append(p_soft)
                return P

            gcnt = {"g": 0}
            avres = [[Res("avr%d_%d" % (i, j)) for j in range(8)] for i in range(2)]

            def gather_group(gi, grp):
                s, t = tiles[gi]
                i2 = gi % 2
                eix = eidx[i2]; h2i = h2[i2]; ggi = gg[i2]
                pso_ = pso[gi % 2]
                wg = wg8[grp % 2]
                bufs = []
                for j in range(8):
                    hk = grp * 8 + j
                    u_ = uvg[gcnt["g"] % NR]; gcnt["g"] += 1
                    bufs.append(u_)
                    dma(lambda e, u_=u_, hk=hk: e.indirect_dma_start(out=u_[:], out_offset=None, in_=uv_bf, in_offset=bass.IndirectOffsetOnAxis(ap=eix[:, hk:hk + 1], axis=0)), [eix], [u_], u_, eng="pool")
                    op("dve", lambda e, u_=u_, hk=hk: e.scalar_tensor_tensor(out=djunk[:], in0=u_[:, 0:DM], scalar=1.0, in1=h2i[:, :, :].rearrange("p a b -> p (a b)"), op0=ALU.mult, op1=ALU.mult, accum_out=av[:, hk:hk + 1]), [u_, h2i], [avres[grp % 2][j]])
                gs_ = slice(grp * 8, grp * 8 + 8)
                op("act", lambda e: e.activation(out=wg[:], in_=av[:, gs_], func=AF.Gelu), avres[grp % 2], [wg])
                op("dve", lambda e: e.tensor_tensor(out=wg[:], in0=wg[:], in1=ggi[:, gs_], op=ALU.mult), [wg, ggi], [wg])
                for j in range(8):
                    hk = grp * 8 + j
                    u_ = bufs[j]; d_ = dg[hk % 4]
                    op("act", lambda e, d_=d_, j=j: e.activation(out=d_[:], in_=ident_b[:], func=AF.Copy, scale=wg[:, j:j + 1]), [ident_b, wg], [d_])
                    for half in range(2):
                        op("pe", lambda e, d_=d_, u_=u_, half=half, hk=hk: e.matmul(pso_[half][:], lhsT=d_[:], rhs=u_[:, DM + half * 512:DM + (half + 1) * 512], start=(hk == 0), stop=(hk == 127)), [d_, u_], [pso_[half]])

            def final(gi):
                s, t = tiles[gi]
                r0 = s * T_SEQ + t * 128
                xti = xt[gi % 2]; pso_ = pso[gi % 2]; gb = G2b[s % 2]
                for half in range(2):
                    hs = slice(half * 512, (half + 1) * 512)
                    op("dve", lambda e, half=half, hs=hs: e.tensor_tensor(out=tmpf[:, hs], in0=pso_[half][:], in1=gb[:, hs], op=ALU.mult), [pso_[half], gb], [tmpf])
                op("dve", lambda e: e.tensor_tensor(out=xti[:], in0=xti[:], in1=tmpf[:], op=ALU.add), [xti, tmpf], [xti])
                dma(lambda e: e.dma_start(out=out[r0:r0 + 128, :], in_=xti[:]), [xti], [], xti)

            for p in routing_pieces(0):
                p()
            for gi in range(len(tiles)):
                nxt = routing_pieces(gi + 1) if gi + 1 < len(tiles) else []
                for grp in range(16):
                    gather_group(gi, grp)
                    if grp < len(nxt):
                        nxt[grp]()
                for p in nxt[16:]:
                    p()
                final(gi)
            S.barrier(); S.flush()
    return nc


def _core_inputs(inp, b0, ns):
    m = {}
    m["x"] = np.ascontiguousarray(inp["x"][b0:b0 + ns].reshape(ns * T_SEQ, DM))
    m["c"] = np.ascontiguousarray(inp["c"][b0:b0 + ns])
    m["pos"] = np.ascontiguousarray(np.asarray(inp["positions"][b0:b0 + ns]).reshape(ns, NT, 128).transpose(0, 2, 1)).astype(np.int32)
    return m


def kernel(**inputs):
    inp = {k: np.asarray(v) for k, v in inputs.items()}
    n_cores = 8
    ns = inp["x"].shape[0] // n_cores
    shared = {}
    shared["inv"] = (np.float32(10000.0) ** (-(np.arange(0, 64, 2, dtype=np.float32) / np.float32(64)))).astype(np.float32).reshape(1, 32)
    for k in ("w_ada", "w_in", "w_branch_a", "w_branch_b", "w_out", "peer_w_q", "peer_u", "peer_v"):
        shared[k] = np.ascontiguousarray(inp[k][0], dtype=np.float32)
    for k in ("b_ada", "norm1_g", "norm2_g", "diff_q_g", "diff_k_g", "dsa_q_g", "dsa_k_g",
              "diff_lam_q1", "diff_lam_k1", "diff_lam_q2", "diff_lam_k2"):
        shared[k] = np.ascontiguousarray(inp[k][0].reshape(1, -1), dtype=np.float32)
    shared["diff_out_g"] = np.ascontiguousarray(inp["diff_out_g"][0].reshape(128, 1), dtype=np.float32)
    shared["subkT"] = np.ascontiguousarray(inp["peer_sub_keys"][0].reshape(16, 128, 128).transpose(2, 0, 1), dtype=np.float32)
    in_maps = []
    for ci in range(n_cores):
        m = dict(shared)
        m.update(_core_inputs(inp, ci * ns, ns))
        in_maps.append(m)
    nc = build(ns)
    res = run_bass_kernel_spmd(nc, in_maps, core_ids=list(range(n_cores)))
    outs = [np.asarray(r["out"]).reshape(ns, T_SEQ, DM) for r in res.results]
    return np.concatenate(outs, axis=0).astype(np.float32)
```

```python
import numpy as np, math
from contextlib import ExitStack
import concourse.bass as bass
import concourse.mybir as mybir
from concourse.bass_utils import run_bass_kernel_spmd

F32 = mybir.dt.float32
BF16 = mybir.dt.bfloat16
I32 = mybir.dt.int32
U32 = mybir.dt.uint32
ALU = mybir.AluOpType
AF = mybir.ActivationFunctionType
AX = mybir.AxisListType


class Res:
    __slots__ = ("name", "w", "r", "dsem", "dcnt")

    def __init__(self, name):
        self.name = name
        self.w = []
        self.r = []
        self.dsem = None
        self.dcnt = 0


class Sched:
    ENGS = ("pe", "act", "dve", "pool", "sp")

    def __init__(self, nc, es):
        self.nc = nc
        self.es = es
        self.items = {e: [] for e in self.ENGS}
        self.cnt = {e: 0 for e in self.ENGS}
        self.sems = {}
        for e in self.ENGS:
            if e != "sp":
                self.sems[e] = es.enter_context(nc.semaphore("sem_" + e))
        self.known = {e: {} for e in self.ENGS}
        self.dstate = {}
        self.ninst = 0

    def _dstate(self, res):
        st = self.dstate.get(res.name)
        if st is None:
            sem = self.es.enter_context(self.nc.semaphore("dsem_%d_%s" % (len(self.dstate), res.name)))
            st = [sem, 0]
            self.dstate[res.name] = st
            self.sems[("d", res.name)] = sem
        return st

    def _collect(self, eng, reads, writes):
        deps = []
        for r in reads:
            deps += r.w
        for w in writes:
            deps += w.w
            deps += w.r
        out = {}
        for (k, v) in deps:
            if eng == "pe" and k == "pe":
                continue
            if self.known[eng].get(k, 0) >= v:
                continue
            if out.get(k, 0) < v:
                out[k] = v
        for k, v in out.items():
            self.known[eng][k] = v
        return list(out.items())

    def _update(self, dep, reads, writes):
        for r in reads:
            if r not in writes:
                r.r.append(dep)
        for w in writes:
            w.w = [dep]
            w.r = []

    def op(self, eng, fn, reads=(), writes=()):
        reads = list(reads); writes = list(writes)
        waits = self._collect(eng, reads, writes)
        self.cnt[eng] += 1
        dep = (eng, self.cnt[eng])
        self.items[eng].append((waits, fn, (eng, 1)))
        self._update(dep, reads, writes)
        self.ninst += 1

    def dma(self, fn, reads=(), writes=(), primary=None, eng="sp"):
        reads = list(reads); writes = list(writes)
        waits = self._collect(eng, reads, writes)
        st = self._dstate(primary)
        st[1] += 16
        key = ("d", primary.name)
        dep = (key, st[1])
        self.items[eng].append((waits, fn, (key, 16)))
        self._update(dep, reads, writes)
        self.ninst += 1

    def barrier(self):
        allv = []
        for e in self.ENGS:
            if e != "sp" and self.cnt[e] > 0:
                allv.append((e, self.cnt[e]))
        for nm, st in self.dstate.items():
            allv.append((("d", nm), st[1]))
        for e in self.ENGS:
            waits = []
            for (k, v) in allv:
                if self.known[e].get(k, 0) < v:
                    waits.append((k, v))
                    self.known[e][k] = v
            if waits:
                self.items[e].append((waits, None, None))

    def flush(self, name=None):
        nc = self.nc
        items = self.items
        sems = self.sems

        def replay(engname):
            def run(eng):
                for (waits, fn, inc) in items[engname]:
                    for (k, v) in waits:
                        eng.wait_ge(sems[k], v)
                    if fn is not None:
                        inst = fn(eng)
                        inst.then_inc(sems[inc[0]], inc[1])
            return run

        with nc.Block() as block:
            if items["sp"]:
                block.sync(replay("sp"))
            if items["act"]:
                block.scalar(replay("act"))
            if items["dve"]:
                block.vector(replay("dve"))
            if items["pool"]:
                block.gpsimd(replay("pool"))
            if items["pe"]:
                block.tensor(replay("pe"))
        self.items = {e: [] for e in self.ENGS}
NS_FULL = 4
T_SEQ = 2048
NT = 16
DM = 1024
IN_COLS = 4808
NPROJ = 2760
GATE0 = 2760
EPS = 1e-6
LAMBDA_INIT = 0.8 - 0.6 * math.exp(-0.0)
NEG = -1.0e30
TWO_PI = 2.0 * math.pi


class T_:
    __slots__ = ("t", "r")

    def __init__(self, t, name):
        self.t = t
        self.r = Res(name)

    def __getitem__(self, k):
        return self.t[k]


class Alloc:
    def __init__(self, nc, es, prefix):
        self.nc, self.es, self.p = nc, es, prefix
        self.n = 0

    def sb(self, name, shape, dt):
        self.n += 1
        return T_(self.es.enter_context(self.nc.sbuf_tensor("%s_%s_%d" % (self.p, name, self.n), shape, dt)), name)

    def ps(self, name, shape, dt):
        self.n += 1
        return T_(self.es.enter_context(self.nc.psum_tensor("%s_%s_%d" % (self.p, name, self.n), shape, dt)), name)


def _rs(lst):
    return [x.r if isinstance(x, T_) else x for x in lst]


class K:
    def __init__(self, nc, S, ns):
        self.nc, self.S, self.ns = nc, S, ns

    def op(self, eng, fn, reads=(), writes=()):
        self.S.op(eng, fn, _rs(reads), _rs(writes))

    def dma(self, fn, reads=(), writes=(), primary=None, eng="sp"):
        self.S.dma(fn, _rs(reads), _rs(writes), primary.r if isinstance(primary, T_) else primary, eng)

def build(ns, phases=(0, 1, 2, 3), dbg=False, stages="ABC"):
    nc = bass.Bass("TRN2", target_bir_lowering=False)
    NTOK = ns * T_SEQ

    def din(name, shape, dt=F32):
        return nc.dram_tensor(name, shape, dt, kind="ExternalInput").ap()

    def dscr(name, shape, dt):
        return nc.dram_tensor(name, shape, dt, kind=("ExternalOutput" if dbg else "Internal")).ap()

    x = din("x", [NTOK, DM]); c_in = din("c", [ns, DM]); pos = din("pos", [ns, 128, NT], I32); inv = din("inv", [1, 32])
    w_ada = din("w_ada", [DM, 6 * DM]); b_ada = din("b_ada", [1, 6 * DM]); n1g = din("norm1_g", [1, DM]); w_in = din("w_in", [DM, IN_COLS])
    gains_in = [din(n, [1, 64]) for n in ("diff_q_g", "diff_k_g", "dsa_q_g", "dsa_k_g")]
    lam_in = [din(n, [1, 64]) for n in ("diff_lam_q1", "diff_lam_k1", "diff_lam_q2", "diff_lam_k2")]
    og_in = din("diff_out_g", [128, 1])
    w_ba = din("w_branch_a", [512, DM]); w_bb = din("w_branch_b", [512, DM]); w_o = din("w_out", [DM, DM]); n2g = din("norm2_g", [1, DM])
    w_q = din("peer_w_q", [DM, 2048]); subkT = din("subkT", [128, 16, 128]); pu = din("peer_u", [16384, DM]); pv = din("peer_v", [16384, DM])
    out = nc.dram_tensor("out", [NTOK, DM], F32, kind="ExternalOutput").ap()

    uv_bf = dscr("uv_bf", [16384, 2 * DM], BF16)
    rows_d = dscr("rows_d", [ns, 6, DM], F32)
    qT_s = dscr("qT_s", [ns, 64, 34, T_SEQ], BF16)
    dv_s = dscr("dv_s", [ns, T_SEQ, 576], BF16)
    iw_s = dscr("iw_s", [ns, T_SEQ, 8], F32)
    gT_s = dscr("gT_s", [ns, 128, 16, T_SEQ], BF16)
    if dbg:
        dbg_oa = dscr("dbg_oa", [ns, 128, 4, T_SEQ], BF16); dbg_ob = dscr("dbg_ob", [ns, 128, 4, T_SEQ], BF16)
        dbg_mk = dscr("dbg_mk", [ns, 128, 136, 128], BF16)

    ges = ExitStack()
    with ges:
        S = Sched(nc, ges)
        kk = K(nc, S, ns)
        op, dma = kk.op, kk.dma
        GA = Alloc(nc, ges, "g")
        ident_f = GA.sb("ident_f", [128, 128], F32); ident_b = GA.sb("ident_b", [128, 128], BF16)
        ones_b = GA.sb("ones_b", [128, 128], BF16)
        cols = GA.sb("cols", [128, 4, 8, ns], F32)
        neglam = GA.sb("neglam", [128, 1], F32); ogcol = GA.sb("ogcol", [128, 1], F32)
        gains_b = GA.sb("gains_b", [128, 4, 64], F32); inv_b = GA.sb("inv_b", [128, 32], F32)
        iota16 = GA.sb("iota16", [128, 16], F32)

        with ExitStack() as es0:
            A = Alloc(nc, es0, "p0")
            if 3 in phases:
                R_uv = Res("uvcast")
                for (src, c0) in ((pu, 0), (pv, DM)):
                    for i in range(16):
                        dma(lambda e, src=src, c0=c0, i=i: e.dma_start(out=uv_bf[i * 1024:(i + 1) * 1024, c0:c0 + DM], in_=src[i * 1024:(i + 1) * 1024, :]), [], [], R_uv, eng="pool")
            op("pool", lambda e: e.memset(ident_f[:], 0.0), [], [ident_f])
            op("pool", lambda e: e.affine_select(out=ident_f[:], in_=ident_f[:], compare_op=ALU.not_equal, fill=1.0, base=0, pattern=[[-1, 128]], channel_multiplier=1), [ident_f], [ident_f])
            op("dve", lambda e: e.tensor_copy(out=ident_b[:], in_=ident_f[:]), [ident_f], [ident_b])
            op("dve", lambda e: e.memset(ones_b[:], 1.0), [], [ones_b])
            op("pool", lambda e: e.iota(iota16[:], pattern=[[1, 16]], base=0, channel_multiplier=0, allow_small_or_imprecise_dtypes=True), [], [iota16])
            for i in range(4):
                dma(lambda e, i=i: e.dma_start(out=gains_b[:, i, :], in_=gains_in[i].partition_broadcast(128)), [], [gains_b], gains_b)
            dma(lambda e: e.dma_start(out=inv_b[:], in_=inv.partition_broadcast(128)), [], [inv_b], inv_b)
            dma(lambda e: e.dma_start(out=ogcol[:], in_=og_in), [], [ogcol], ogcol)
            op("dve", lambda e: e.tensor_scalar(out=ogcol[:], in0=ogcol[:], scalar1=1.0 - LAMBDA_INIT, scalar2=None, op0=ALU.mult), [ogcol], [ogcol])
            lamv = A.sb("lamv", [128, 4, 64], F32); lamp = A.sb("lamp", [128, 2, 64], F32); lams = A.sb("lams", [128, 2], F32)
            for i in range(4):
                dma(lambda e, i=i: e.dma_start(out=lamv[:, i, :], in_=lam_in[i].partition_broadcast(128)), [], [lamv], lamv)
            lv4 = lamv[:, :, :].rearrange("p (a b) d -> p a b d", b=2)
            op("dve", lambda e: e.tensor_tensor(out=lamp[:], in0=lv4[:, :, 0, :], in1=lv4[:, :, 1, :], op=ALU.mult), [lamv], [lamp])
            op("dve", lambda e: e.tensor_reduce(out=lams[:], in_=lamp[:], axis=AX.X, op=ALU.add), [lamp], [lams])
            op("act", lambda e: e.activation(out=lams[:], in_=lams[:], func=AF.Exp), [lams], [lams])
            op("dve", lambda e: e.tensor_tensor(out=neglam[:], in0=lams[:, 1:2], in1=lams[:, 0:1], op=ALU.subtract), [lams], [neglam])
            op("dve", lambda e: e.tensor_scalar(out=neglam[:], in0=neglam[:], scalar1=-LAMBDA_INIT, scalar2=None, op0=ALU.add), [neglam], [neglam])

            c_sb = A.sb("c_sb", [ns, DM], F32); sg = A.sb("sg", [ns, DM], F32); scT = A.sb("scT", [128, 8, ns], F32)
            pc0 = A.ps("pc0", [128, 8, ns], F32)
            dma(lambda e: e.dma_start(out=c_sb[:], in_=c_in), [], [c_sb], c_sb)
            op("act", lambda e: e.activation(out=sg[:], in_=c_sb[:], func=AF.Sigmoid), [c_sb], [sg])
            op("dve", lambda e: e.tensor_tensor(out=c_sb[:], in0=c_sb[:], in1=sg[:], op=ALU.mult), [c_sb, sg], [c_sb])
            for kc in range(8):
                op("pe", lambda e, kc=kc: e.transpose(out=pc0[:, kc, :], in_=c_sb[0:ns, kc * 128:(kc + 1) * 128], identity=ident_f[0:ns, 0:ns]), [c_sb, ident_f], [pc0])
            op("dve", lambda e: e.tensor_copy(out=scT[:], in_=pc0[:]), [pc0], [scT])
            wst = [A.sb("wst%d" % i, [128, 8, 512], F32) for i in range(2)]
            b_b = A.sb("b_b", [ns, 6 * DM], F32); mod_sb = A.sb("mod_sb", [ns, 6 * DM], F32)
            pm = [A.ps("pm%d" % i, [ns, 512], F32) for i in range(2)]
            dma(lambda e: e.dma_start(out=b_b[:], in_=b_ada.partition_broadcast(ns)), [], [b_b], b_b)
            w_ada_v = w_ada.rearrange("(kc p) n -> p kc n", p=128)
            for j in range(12):
                ws = wst[j % 2]; pmj = pm[j % 2]
                dma(lambda e, ws=ws, j=j: e.dma_start(out=ws[:], in_=w_ada_v[:, :, j * 512:(j + 1) * 512]), [], [ws], ws)
                for kc in range(8):
                    op("pe", lambda e, ws=ws, pmj=pmj, kc=kc: e.matmul(pmj[:], lhsT=scT[:, kc, :], rhs=ws[:, kc, :], start=(kc == 0), stop=(kc == 7)), [scT, ws], [pmj])
                op("dve", lambda e, pmj=pmj, j=j: e.tensor_tensor(out=mod_sb[:, j * 512:(j + 1) * 512], in0=pmj[:], in1=b_b[:, j * 512:(j + 1) * 512], op=ALU.add), [pmj, b_b], [mod_sb])
            rows_sb = A.sb("rows_sb", [ns, 6, DM], F32); ng_b = A.sb("ng_b", [ns, 2, DM], F32)
            dma(lambda e: e.dma_start(out=ng_b[:, 0, :], in_=n1g.partition_broadcast(ns)), [], [ng_b], ng_b)
            dma(lambda e: e.dma_start(out=ng_b[:, 1, :], in_=n2g.partition_broadcast(ns)), [], [ng_b], ng_b)
            for half in range(2):
                o = half * 3 * DM
                op("dve", lambda e, o=o, half=half: e.scalar_tensor_tensor(out=rows_sb[:, half * 3 + 0, :], in0=mod_sb[:, o + DM:o + 2 * DM], scalar=1.0, in1=ng_b[:, half, :], op0=ALU.add, op1=ALU.mult), [mod_sb, ng_b], [rows_sb])
                op("dve", lambda e, o=o, half=half: e.tensor_copy(out=rows_sb[:, half * 3 + 1, :], in_=mod_sb[:, o:o + DM]), [mod_sb], [rows_sb])
                op("dve", lambda e, o=o, half=half: e.tensor_copy(out=rows_sb[:, half * 3 + 2, :], in_=mod_sb[:, o + 2 * DM:o + 3 * DM]), [mod_sb], [rows_sb])
            dma(lambda e: e.dma_start(out=rows_d, in_=rows_sb[:]), [rows_sb], [], rows_sb)
            pc1 = A.ps("pc1", [128, 4, 8, ns], F32)
            for wi, ri in enumerate((0, 1, 3, 4)):
                for kc in range(8):
                    op("pe", lambda e, wi=wi, ri=ri, kc=kc: e.transpose(out=pc1[:, wi, kc, :], in_=rows_sb[0:ns, ri, kc * 128:(kc + 1) * 128], identity=ident_f[0:ns, 0:ns]), [rows_sb, ident_f], [pc1])
            op("dve", lambda e: e.tensor_copy(out=cols[:], in_=pc1[:]), [pc1], [cols])

            S.barrier()
            S.flush()

        def rope_alloc(A):
            return (A.sb("posi", [128, NT], I32), A.sb("posf", [128, NT], F32), A.sb("ang", [128, NT, 32], F32),
                    A.sb("a2", [128, NT, 32], F32), A.sb("ki", [128, NT, 32], I32), A.sb("kf", [128, NT, 32], F32))

        def rope_tables(rb, s, cos_t, sin_t):
            posi, posf, ang, a2, ki, kf = rb
            dma(lambda e: e.dma_start(out=posi[:], in_=pos[s]), [], [posi], posi)
            op("dve", lambda e: e.tensor_copy(out=posf[:], in_=posi[:]), [posi], [posf])
            op("dve", lambda e: e.tensor_tensor(out=ang[:], in0=posf[:, :].unsqueeze(2).to_broadcast([128, NT, 32]), in1=inv_b[:, :].unsqueeze(1).to_broadcast([128, NT, 32]), op=ALU.mult), [posf, inv_b], [ang])
            for (shift, dst) in ((0.0, sin_t), (0.5 * math.pi, cos_t)):
                op("dve", lambda e, shift=shift: e.tensor_scalar(out=a2[:], in0=ang[:], scalar1=shift, scalar2=None, op0=ALU.add), [ang], [a2])
                op("dve", lambda e: e.tensor_scalar(out=ki[:], in0=a2[:], scalar1=1.0 / TWO_PI, scalar2=None, op0=ALU.mult), [a2], [ki])
                op("dve", lambda e: e.tensor_copy(out=kf[:], in_=ki[:]), [ki], [kf])
                op("dve", lambda e: e.scalar_tensor_tensor(out=a2[:], in0=kf[:], scalar=-TWO_PI, in1=a2[:], op0=ALU.mult, op1=ALU.add), [kf, a2], [a2])
                op("dve", lambda e: e.tensor_scalar(out=kf[:], in0=a2[:], scalar1=math.pi, scalar2=TWO_PI, op0=ALU.is_gt, op1=ALU.mult), [a2], [kf])
                op("dve", lambda e: e.tensor_tensor(out=a2[:], in0=a2[:], in1=kf[:], op=ALU.subtract), [a2, kf], [a2])
                op("dve", lambda e: e.tensor_scalar(out=kf[:], in0=a2[:], scalar1=-math.pi, scalar2=TWO_PI, op0=ALU.is_lt, op1=ALU.mult), [a2], [kf])
                op("dve", lambda e: e.tensor_tensor(out=a2[:], in0=a2[:], in1=kf[:], op=ALU.add), [a2, kf], [a2])
                op("act", lambda e, dst=dst: e.activation(out=dst[:], in_=a2[:], func=AF.Sin), [a2], [dst])

        def rms_rstd(A, ssq_ap, n, res, nm):
            op("dve", lambda e: e.tensor_scalar(out=ssq_ap, in0=ssq_ap, scalar1=1.0 / n, scalar2=EPS, op0=ALU.mult, op1=ALU.add), [res], [res])
            op("act", lambda e: e.activation(out=ssq_ap, in_=ssq_ap, func=AF.Sqrt), [res], [res])
            op("dve", lambda e: e.reciprocal(out=ssq_ap, in_=ssq_ap), [res], [res])

        if 1 in phases:
          with ExitStack() as es1:
            A = Alloc(nc, es1, "p1")
            w_in_bf = A.sb("w_in_bf", [128, 8, IN_COLS], BF16)
            w_in_v = w_in.rearrange("(kc p) n -> p kc n", p=128)
            nch = (IN_COLS + 511) // 512
            for j in range(nch):
                lo = j * 512; hi = min(IN_COLS, lo + 512)
                dma(lambda e, lo=lo, hi=hi: e.dma_start(out=w_in_bf[:, :, lo:hi], in_=w_in_v[:, :, lo:hi]), [], [w_in_bf], w_in_bf, eng="pool")
            cos_tr = [A.sb("cos_t%d" % i, [128, NT, 32], F32) for i in range(2)]; sin_tr = [A.sb("sin_t%d" % i, [128, NT, 32], F32) for i in range(2)]
            xt = [A.sb("xt%d" % i, [128, DM], F32) for i in range(2)]
            junk = A.sb("junk", [128, DM], BF16); xnr = [A.sb("xn%d" % i, [128, DM], BF16) for i in range(2)]
            ssq = [A.sb("ssq%d" % i, [128, 1], F32) for i in range(2)]
            hT = [A.sb("hT%d" % i, [128, 8, 128], BF16) for i in range(2)]
            projr = [A.sb("proj%d" % i, [128, NPROJ], F32) for i in range(2)]
            sqbr = [A.sb("sqb%d" % i, [128, 1600], F32) for i in range(2)]; ssqgr = [A.sb("ssqg%d" % i, [128, 25], F32) for i in range(2)]
            rqr = [A.sb("rq%d" % i, [128, 34, 64], BF16) for i in range(2)]
            tv = [A.sb("tv%d" % i, [128, 16, 32], F32) for i in range(2)]
            tp = [A.sb("tp%d" % i, [128, 9, 32], F32) for i in range(2)]
            dvb = [A.sb("dvb%d" % i, [128, 576], BF16) for i in range(2)]
            iwb = [A.sb("iwb%d" % i, [128, 8], F32) for i in range(2)]
            gs = [A.sb("gs%d" % i, [128, 16, 128], BF16) for i in range(2)]
            qTs = [A.sb("qTs%d" % i, [64, 34, 128], BF16) for i in range(2)]
            pTr = [A.ps("pT%d" % i, [128, 8, 128], BF16) for i in range(2)]
            pp = [A.ps("pp%d" % i, [128, 512], F32) for i in range(2)]
            pg = [A.ps("pg%d" % i, [128, 4, 128], F32) for i in range(2)]
            pq = [A.ps("pq%d" % i, [64, 8, 128], BF16) for i in range(2)]
            bounds = [0, 512, 1024, 1536, 2048, 2560, NPROJ]
            rb = rope_alloc(A)
            def p1_tile(s, t, gi):
                i2 = gi % 2
                r0 = s * T_SEQ + t * 128
                xti = xt[i2]; ssi = ssq[i2]; hTi = hT[i2]
                xn = xnr[i2]; proj = projr[i2]; sqb = sqbr[i2]; ssqg = ssqgr[i2]; rq = rqr[i2]; pT = pTr[i2]
                cos_t = cos_tr[s % 2]; sin_t = sin_tr[s % 2]
                dma(lambda e, xti=xti, r0=r0: e.dma_start(out=xti[:], in_=x[r0:r0 + 128, :]), [], [xti], xti)
                op("act", lambda e, xti=xti, ssi=ssi: e.activation(out=junk[:], in_=xti[:], func=AF.Square, accum_out=ssi[:, 0:1]), [xti], [junk, ssi])
                rms_rstd(A, ssi[:, 0:1], float(DM), ssi, "x")
                op("act", lambda e, xti=xti, ssi=ssi: e.activation(out=xn[:], in_=xti[:], func=AF.Copy, scale=ssi[:, 0:1]), [xti, ssi], [xn])
                yield
                for kc in range(8):
                    op("pe", lambda e, kc=kc: e.transpose(out=pT[:, kc, :], in_=xn[:, kc * 128:(kc + 1) * 128], identity=ident_b[:]), [xn, ident_b], [pT])
                for kc in range(8):
                    op("dve", lambda e, kc=kc, hTi=hTi, s=s: e.tensor_scalar(out=hTi[:, kc, :], in0=pT[:, kc, :], scalar1=cols[:, 0, kc, s:s + 1], scalar2=cols[:, 1, kc, s:s + 1], op0=ALU.mult, op1=ALU.add), [pT, cols], [hTi])
                yield
                for j in range(6):
                    lo, hi = bounds[j], bounds[j + 1]; w = hi - lo
                    ppj = pp[j % 2]
                    for kc in range(8):
                        op("pe", lambda e, ppj=ppj, kc=kc, lo=lo, hi=hi, w=w, hTi=hTi: e.matmul(ppj[:, 0:w], lhsT=hTi[:, kc, :], rhs=w_in_bf[:, kc, lo:hi], start=(kc == 0), stop=(kc == 7)), [hTi, w_in_bf], [ppj])
                    op("act", lambda e, ppj=ppj, lo=lo, hi=hi, w=w: e.activation(out=proj[:, lo:hi], in_=ppj[:, 0:w], func=AF.Copy), [ppj], [proj])
                    if j == 2 or j == 5:
                        yield
                gsi = gs[i2]

                def gates_half(ra, rb_):
                    for r in range(ra, rb_):
                        pgr = pg[r % 2]
                        for j in range(4):
                            gc = r * 4 + j
                            for kc in range(8):
                                op("pe", lambda e, pgr=pgr, j=j, gc=gc, kc=kc: e.matmul(pgr[:, j, :], lhsT=w_in_bf[:, kc, GATE0 + gc * 128:GATE0 + (gc + 1) * 128], rhs=hTi[:, kc, :], start=(kc == 0), stop=(kc == 7)), [hTi, w_in_bf], [pgr])
                        op("act", lambda e, pgr=pgr, r=r: e.activation(out=gsi[:, r * 4:(r + 1) * 4, :], in_=pgr[:], func=AF.Sigmoid), [pgr], [gsi])
                gates_half(0, 2)
                yield
                dvi = dvb[i2]; iwi = iwb[i2]
                op("pool", lambda e, dvi=dvi: e.tensor_copy(out=dvi[:, 0:512], in_=proj[:, 1024:1536]), [proj], [dvi])
                op("pool", lambda e, dvi=dvi: e.tensor_copy(out=dvi[:, 512:576], in_=proj[:, 2112:2176]), [proj], [dvi])
                op("pool", lambda e, iwi=iwi: e.tensor_copy(out=iwi[:], in_=proj[:, 2752:2760]), [proj], [iwi])
                dma(lambda e, dvi=dvi, s=s, t=t: e.dma_start(out=dv_s[s][t * 128:(t + 1) * 128, :], in_=dvi[:]), [dvi], [], dvi)
                dma(lambda e, iwi=iwi, s=s, t=t: e.dma_start(out=iw_s[s][t * 128:(t + 1) * 128, :], in_=iwi[:]), [iwi], [], iwi)
                op("pool", lambda e: e.tensor_tensor(out=sqb[:, 0:1024], in0=proj[:, 0:1024], in1=proj[:, 0:1024], op=ALU.mult), [proj], [sqb])
                op("pool", lambda e: e.tensor_tensor(out=sqb[:, 1024:1600], in0=proj[:, 1536:2112], in1=proj[:, 1536:2112], op=ALU.mult), [proj], [sqb])
                op("dve", lambda e: e.tensor_reduce(out=ssqg[:, 0:25], in_=sqb[:, :].rearrange("p (g d) -> p g d", d=64), axis=AX.X, op=ALU.add), [sqb], [ssqg])
                rms_rstd(A, ssqg[:, :], 64.0, ssqg, "g")
                pA = proj[:, 0:1024].rearrange("p (g d) -> p g d", d=64)
                pB = proj[:, 1536:2112].rearrange("p (g d) -> p g d", d=64)
                op("dve", lambda e, pA=pA: e.tensor_tensor(out=pA, in0=pA, in1=ssqg[:, 0:16].unsqueeze(2).to_broadcast([128, 16, 64]), op=ALU.mult), [proj, ssqg], [proj])
                op("dve", lambda e, pB=pB: e.tensor_tensor(out=pB, in0=pB, in1=ssqg[:, 16:25].unsqueeze(2).to_broadcast([128, 9, 64]), op=ALU.mult), [proj, ssqg], [proj])
                op("dve", lambda e, pA=pA: e.tensor_tensor(out=pA[:, 0:8, :], in0=pA[:, 0:8, :], in1=gains_b[:, 0, :].unsqueeze(1).to_broadcast([128, 8, 64]), op=ALU.mult), [proj, gains_b], [proj])
                op("dve", lambda e, pA=pA: e.tensor_tensor(out=pA[:, 8:16, :], in0=pA[:, 8:16, :], in1=gains_b[:, 1, :].unsqueeze(1).to_broadcast([128, 8, 64]), op=ALU.mult), [proj, gains_b], [proj])
                op("dve", lambda e, pB=pB: e.tensor_tensor(out=pB[:, 0:8, :], in0=pB[:, 0:8, :], in1=gains_b[:, 2, :].unsqueeze(1).to_broadcast([128, 8, 64]), op=ALU.mult), [proj, gains_b], [proj])
                op("dve", lambda e, pB=pB: e.tensor_tensor(out=pB[:, 8:9, :], in0=pB[:, 8:9, :], in1=gains_b[:, 3, :].unsqueeze(1).to_broadcast([128, 1, 64]), op=ALU.mult), [proj, gains_b], [proj])
                yield
                gates_half(2, 4)
                dma(lambda e: e.dma_start(out=gT_s[s][:, :, t * 128:(t + 1) * 128], in_=gsi[:]), [gsi], [], gsi)
                yield
                for (lo, G, g0, eng, tmps) in ((0, 16, 0, "dve", tv), (1536, 9, 16, "dve", tv), (2176, 9, 25, "pool", tp)):
                    X = proj[:, lo:lo + G * 64].rearrange("p (g h d) -> p g h d", h=2, d=32)
                    O = rq[:, g0:g0 + G, :].rearrange("p g (h d) -> p g h d", h=2)
                    cb = cos_t[:, t, :].unsqueeze(1).to_broadcast([128, G, 32])
                    sb_ = sin_t[:, t, :].unsqueeze(1).to_broadcast([128, G, 32])
                    t1 = tmps[0][:, 0:G, :]; t2 = tmps[1][:, 0:G, :]
                    rr = [proj, cos_t, sin_t]
                    op(eng, lambda e, X=X, cb=cb, t1=t1: e.tensor_tensor(out=t1, in0=X[:, :, 0, :], in1=cb, op=ALU.mult), rr, [tmps[0]])
                    op(eng, lambda e, X=X, sb_=sb_, t2=t2: e.tensor_tensor(out=t2, in0=X[:, :, 1, :], in1=sb_, op=ALU.mult), rr, [tmps[1]])
                    op(eng, lambda e, O=O, t1=t1, t2=t2: e.tensor_tensor(out=O[:, :, 0, :], in0=t1, in1=t2, op=ALU.subtract), [tmps[0], tmps[1]], [rq])
                    op(eng, lambda e, X=X, cb=cb, t1=t1: e.tensor_tensor(out=t1, in0=X[:, :, 1, :], in1=cb, op=ALU.mult), rr, [tmps[0]])
                    op(eng, lambda e, X=X, sb_=sb_, t2=t2: e.tensor_tensor(out=t2, in0=X[:, :, 0, :], in1=sb_, op=ALU.mult), rr, [tmps[1]])
                    op(eng, lambda e, O=O, t1=t1, t2=t2: e.tensor_tensor(out=O[:, :, 1, :], in0=t1, in1=t2, op=ALU.add), [tmps[0], tmps[1]], [rq])
                yield
                qTi = qTs[i2]
                for bi, (g0, g1) in enumerate(((0, 8), (8, 16), (16, 24), (24, 32), (32, 34))):
                    pqb = pq[bi % 2]
                    for g in range(g0, g1):
                        op("pe", lambda e, pqb=pqb, g=g, g0=g0: e.transpose(out=pqb[:, g - g0, :], in_=rq[:, g, :], identity=ident_b[:]), [rq, ident_b], [pqb])
                    op("act", lambda e, pqb=pqb, g0=g0, g1=g1, qTi=qTi: e.activation(out=qTi[:, g0:g1, :], in_=pqb[:, 0:g1 - g0, :], func=AF.Copy), [pqb], [qTi])
                dma(lambda e, qTi=qTi, s=s, t=t: e.dma_start(out=qT_s[s][:, :, t * 128:(t + 1) * 128], in_=qTi[:]), [qTi], [], qTi)

            tl = [(s_, t_) for s_ in range(ns) for t_ in range(NT)]
            gens = {}

            def pull(i):
                if i < 0 or i >= len(tl):
                    return
                if i not in gens:
                    s_, t_ = tl[i]
                    if t_ == 0:
                        rope_tables(rb, s_, cos_tr[s_ % 2], sin_tr[s_ % 2])
                    gens[i] = p1_tile(s_, t_, i)
                next(gens[i], None)

            pull(0); pull(0); pull(1)
            for i in range(len(tl) + 1):
                pull(i)
                pull(i - 1)
                pull(i)
                pull(i + 1)
                pull(i)
                pull(i)
                pull(i)
                pull(i)
                pull(i + 2)
            S.barrier()
            S.flush()

        if 2 in phases:
          with ExitStack() as es2:
            P2 = Alloc(nc, es2, "p2")
            wba = P2.sb("wba", [128, 4, DM], BF16); wbb = P2.sb("wbb", [128, 4, DM], BF16); wo = P2.sb("wo", [128, 8, DM], BF16)
            oaT = P2.sb("oaT", [128, 4, T_SEQ], BF16); obT = P2.sb("obT", [128, 4, T_SEQ], BF16)
            NMT = NT * (NT + 1) // 2
            maskT_all = P2.sb("maskT_all", [128, NMT, 128], BF16)
            moff = [t * (t + 1) // 2 for t in range(NT)]
            for (src, dst) in ((w_ba, wba), (w_bb, wbb), (w_o, wo)):
                sv_ = src.rearrange("(j p) n -> p j n", p=128)
                for j0 in range(0, sv_.shape[1], 2):
                    dma(lambda e, sv_=sv_, dst=dst, j0=j0: e.dma_start(out=dst[:, j0:j0 + 2, :], in_=sv_[:, j0:j0 + 2, :]), [], [dst], dst, eng="pool")
            for s in range(ns):
                with ExitStack() as esa:
                  if 'A' in stages:
                      A = Alloc(nc, esa, "p2a%d" % s)
                      dvs = A.sb("dvs", [128, NT, 512], BF16)
                      dkq = [A.sb("dkq%d" % i, [64, 2, T_SEQ], BF16) for i in range(2)]
                      PT = [A.sb("PT%d" % i, [128, 512], BF16) for i in range(3)]
                      o0n = A.sb("o0n", [128, T_SEQ], F32)
                      rz = A.sb("rz", [128, 512], F32); o1n = A.sb("o1n", [128, 512], F32); dd = A.sb("dd", [128, 512], F32)
                      dsq = A.sb("dsq", [128, 512], BF16); rstd = A.sb("rstd", [128, 512], F32)
                      ps_s = [A.ps("ps_s%d" % i, [128, 512], F32) for i in range(2)]
                      ps_o = [A.ps("ps_o%d" % i, [128, 512], F32) for i in range(2)]; ps_z = [A.ps("ps_z%d" % i, [128, 512], F32) for i in range(2)]
                      ikT = A.sb("ikT", [64, T_SEQ], BF16); iws = A.sb("iws", [128, NT, 8], F32)
                      iqT = [A.sb("iqT%d" % i, [64, 8, 128], BF16) for i in range(2)]
                      score2 = [A.sb("score%d" % i, [128, T_SEQ], F32) for i in range(2)]
                      rl = [A.sb("rl%d" % i, [128, 512], F32) for i in range(2)]
                      blo = [A.sb("blo%d" % i, [128, 1], F32) for i in range(2)]; bd0 = [A.sb("bd0%d" % i, [128, 1], F32) for i in range(2)]; bmid = [A.sb("bmid%d" % i, [128, 1], F32) for i in range(2)]
                      bcnt = [A.sb("bcnt%d" % i, [128, 1], F32) for i in range(2)]; bg = [A.sb("bg%d" % i, [128, 1], F32) for i in range(2)]; bjunk = A.sb("bjunk", [128, T_SEQ], BF16)
                      maskb = A.sb("maskb", [128, T_SEQ], BF16)
                      ps_i = A.ps("ps_i", [128, 512], F32); ps_m = A.ps("ps_m", [128, 8, 128], BF16)
                      dma(lambda e: e.dma_start(out=dvs[:], in_=dv_s[s].rearrange("(t p) c -> p t c", p=128)[:, :, 0:512]), [], [dvs], dvs)
                      dma(lambda e: e.dma_start(out=ikT[:], in_=qT_s[s][:, 33, :]), [], [ikT], ikT)
                      dma(lambda e: e.dma_start(out=iws[:], in_=iw_s[s].rearrange("(t p) c -> p t c", p=128)), [], [iws], iws)

                      def b_part1_gen():
                          ii = 0
                          for t0 in range(0, NT, 2):
                              pair = (t0, t0 + 1)
                              for pi, t in enumerate(pair):
                                  iq_ = iqT[t % 2]; sc_ = score2[pi]
                                  tsl = slice(t * 128, (t + 1) * 128)
                                  dma(lambda e, iq_=iq_, tsl=tsl: e.dma_start(out=iq_[:], in_=qT_s[s][:, 25:33, tsl]), [], [iq_], iq_)
                                  nk = (t + 1) * 128
                                  for kc in range((nk + 511) // 512):
                                      w = min(512, nk - kc * 512)
                                      ksl = slice(kc * 512, kc * 512 + w)
                                      for hh in range(8):
                                          rli = rl[ii % 2]; ii += 1
                                          op("pe", lambda e, iq_=iq_, hh=hh, ksl=ksl, w=w: e.matmul(ps_i[:, 0:w], lhsT=iq_[:, hh, :], rhs=ikT[:, ksl], start=True, stop=True), [iq_, ikT], [ps_i])
                                          op("act", lambda e, rli=rli, w=w: e.activation(out=rli[:, 0:w], in_=ps_i[:, 0:w], func=AF.Relu), [ps_i], [rli])
                                          if hh == 0:
                                              op("dve", lambda e, rli=rli, ksl=ksl, w=w, hh=hh, t=t, sc_=sc_: e.tensor_scalar(out=sc_[:, ksl], in0=rli[:, 0:w], scalar1=iws[:, t, hh:hh + 1], scalar2=None, op0=ALU.mult), [rli, iws], [sc_])
                                          else:
                                              op("dve", lambda e, rli=rli, ksl=ksl, w=w, hh=hh, t=t, sc_=sc_: e.scalar_tensor_tensor(out=sc_[:, ksl], in0=rli[:, 0:w], scalar=iws[:, t, hh:hh + 1], in1=sc_[:, ksl], op0=ALU.mult, op1=ALU.add), [rli, iws, sc_], [sc_])
                                          yield (w / 960.0 + 0.15, 0.4)
                                  op("dve", lambda e, t=t, sc_=sc_: e.memset(sc_[0:64, t * 128 + 64:(t + 1) * 128], NEG), [], [sc_])
                              if t0 >= 2:
                                  for pi, t in enumerate(pair):
                                      nk = (t + 1) * 128; sc_ = score2[pi]; lo_ = blo[pi]; d0_ = bd0[pi]
                                      op("dve", lambda e, nk=nk, sc_=sc_, lo_=lo_: e.tensor_reduce(out=lo_[:], in_=sc_[:, 0:nk - 64], axis=AX.X, op=ALU.min), [sc_], [lo_])
                                      op("dve", lambda e, nk=nk, sc_=sc_, d0_=d0_: e.tensor_reduce(out=d0_[:], in_=sc_[:, 0:nk], axis=AX.X, op=ALU.max), [sc_], [d0_])
                                  for pi in range(2):
                                      lo_ = blo[pi]; d0_ = bd0[pi]
                                      op("dve", lambda e, lo_=lo_, d0_=d0_: e.tensor_tensor(out=d0_[:], in0=d0_[:], in1=lo_[:], op=ALU.subtract), [d0_, lo_], [d0_])
                                  for k in range(24):
                                      ck = 2.0 ** (-(k + 1))
                                      for pi in range(2):
                                          lo_ = blo[pi]; d0_ = bd0[pi]; mid_ = bmid[pi]
                                          op("dve", lambda e, ck=ck, lo_=lo_, d0_=d0_, mid_=mid_: e.scalar_tensor_tensor(out=mid_[:], in0=d0_[:], scalar=ck, in1=lo_[:], op0=ALU.mult, op1=ALU.add), [d0_, lo_], [mid_])
                                      for pi, t in enumerate(pair):
                                          nk = (t + 1) * 128; sc_ = score2[pi]; mid_ = bmid[pi]; cnt_ = bcnt[pi]
                                          op("dve", lambda e, nk=nk, sc_=sc_, mid_=mid_, cnt_=cnt_: e.tensor_scalar(out=bjunk[:, 0:nk], in0=sc_[:, 0:nk], scalar1=mid_[:, 0:1], scalar2=None, op0=ALU.is_ge, op1=ALU.add, accum_out=cnt_[:, 0:1]), [sc_, mid_], [cnt_])
                                      for pi in range(2):
                                          cnt_ = bcnt[pi]; g_ = bg[pi]
                                          op("dve", lambda e, ck=ck, cnt_=cnt_, g_=g_: e.tensor_scalar(out=g_[:], in0=cnt_[:], scalar1=255.5, scalar2=ck, op0=ALU.is_ge, op1=ALU.mult), [cnt_], [g_])
                                      for pi in range(2):
                                          lo_ = blo[pi]; d0_ = bd0[pi]; g_ = bg[pi]
                                          op("dve", lambda e, lo_=lo_, d0_=d0_, g_=g_: e.scalar_tensor_tensor(out=lo_[:], in0=g_[:], scalar=d0_[:, 0:1], in1=lo_[:], op0=ALU.mult, op1=ALU.add), [g_, d0_, lo_], [lo_])
                                      yield (((pair[0] + 1) * 128 + (pair[1] + 1) * 128) / 960.0 + 1.2, 0.0)
                              for pi, t in enumerate(pair):
                                  nk = (t + 1) * 128; sc_ = score2[pi]; lo_ = blo[pi]
                                  if t0 >= 2:
                                      op("dve", lambda e, nk=nk, sc_=sc_, lo_=lo_: e.tensor_scalar(out=maskb[:, 0:nk], in0=sc_[:, 0:nk], scalar1=lo_[:, 0:1], scalar2=None, op0=ALU.is_ge), [sc_, lo_], [maskb])
                                  else:
                                      op("dve", lambda e, nk=nk, sc_=sc_: e.tensor_scalar(out=maskb[:, 0:nk], in0=sc_[:, 0:nk], scalar1=-1.0e29, scalar2=None, op0=ALU.is_ge), [sc_], [maskb])
                                  yield (nk / 960.0 + 0.2, 0.0)
                                  for kb in range((t + 8) // 8):
                                      k1 = min(t + 1, kb * 8 + 8)
                                      for kt in range(kb * 8, k1):
                                          op("pe", lambda e, kt=kt, kb=kb: e.transpose(out=ps_m[:, kt - kb * 8, :], in_=maskb[:, kt * 128:(kt + 1) * 128], identity=ident_b[:]), [maskb, ident_b], [ps_m])
                                      op("act", lambda e, kb=kb, k1=k1, t=t: e.activation(out=maskT_all[:, moff[t] + kb * 8:moff[t] + k1, :], in_=ps_m[:, 0:k1 - kb * 8, :], func=AF.Copy), [ps_m], [maskT_all])
                                      yield (0.0, 0.1 * (k1 - kb * 8))

                      its = []
                      for h in range(4):
                          for m in range(2):
                              for G in range(4):
                                  for kt in range(4 * G + 4):
                                      its.append((h, m, G, kt, 4 * G + 4))

                      def a_loads(g):
                          dk_ = dkq[g % 2]
                          dma(lambda e, dk_=dk_, g=g: e.dma_start(out=dk_[:, 0, :], in_=qT_s[s][:, g, :]), [], [dk_], dk_)
                          dma(lambda e, dk_=dk_, g=g: e.dma_start(out=dk_[:, 1, :], in_=qT_s[s][:, 8 + g, :]), [], [dk_], dk_)

                      def a_stage1(i):
                          h, m, G, kt, nkt = its[i]
                          dk_ = dkq[(2 * h + m) % 2]; pss = ps_s[acnt["s"] % 2]; acnt["s"] += 1; pt = PT[i % 3]
                          op("pe", lambda e: e.matmul(pss[:], lhsT=dk_[:, 1, kt * 128:(kt + 1) * 128], rhs=dk_[:, 0, G * 512:(G + 1) * 512], start=True, stop=True), [dk_], [pss])
                          op("act", lambda e: e.activation(out=pt[:], in_=pss[:], func=AF.Exp, scale=0.125), [pss], [pt])
                          if kt >= 4 * G:
                              j = kt - 4 * G
                              if j > 0:
                                  op("pool", lambda e: e.memset(pt[:, 0:j * 128], 0.0), [], [pt])
                              op("pool", lambda e: e.memset(pt[64:128, j * 128:j * 128 + 64], 0.0), [], [pt])

                      def a_stage2(i):
                          h, m, G, kt, nkt = its[i]
                          pt = PT[i % 3]
                          grp = (h * 2 + m) * 4 + G
                          pso_ = ps_o[grp % 2]; psz_ = ps_z[grp % 2]
                          op("pe", lambda e: e.matmul(pso_[:], lhsT=dvs[:, kt, h * 128:(h + 1) * 128], rhs=pt[:], start=(kt == 0), stop=(kt == nkt - 1)), [dvs, pt], [pso_])
                          op("pe", lambda e: e.matmul(psz_[:], lhsT=ones_b[:], rhs=pt[:], start=(kt == 0), stop=(kt == nkt - 1)), [ones_b, pt], [psz_])
                          if kt != nkt - 1:
                              return
                          op("dve", lambda e: e.reciprocal(out=rz[:], in_=psz_[:]), [psz_], [rz])
                          gsl = slice(G * 512, (G + 1) * 512)
                          if m == 0:
                              op("dve", lambda e: e.tensor_tensor(out=o0n[:, gsl], in0=pso_[:], in1=rz[:], op=ALU.mult), [pso_, rz], [o0n])
                          else:
                              op("dve", lambda e: e.tensor_tensor(out=o1n[:], in0=pso_[:], in1=rz[:], op=ALU.mult), [pso_, rz], [o1n])
                              op("dve", lambda e: e.scalar_tensor_tensor(out=dd[:], in0=o1n[:], scalar=neglam[:, 0:1], in1=o0n[:, gsl], op0=ALU.mult, op1=ALU.add), [o1n, neglam, o0n], [dd])
                              op("act", lambda e: e.activation(out=dsq[:], in_=dd[:], func=AF.Square), [dd], [dsq])
                              ps_q = ps_s[acnt["s"] % 2]; acnt["s"] += 1
                              op("pe", lambda e: e.matmul(ps_q[:], lhsT=ones_b[:], rhs=dsq[:], start=True, stop=True), [ones_b, dsq], [ps_q])
                              op("dve", lambda e: e.tensor_scalar(out=rstd[:], in0=ps_q[:], scalar1=1.0 / 128, scalar2=EPS, op0=ALU.mult, op1=ALU.add), [ps_q], [rstd])
                              op("act", lambda e: e.activation(out=rstd[:], in_=rstd[:], func=AF.Sqrt), [rstd], [rstd])
                              op("dve", lambda e: e.reciprocal(out=rstd[:], in_=rstd[:]), [rstd], [rstd])
                              op("dve", lambda e: e.tensor_tensor(out=dd[:], in0=dd[:], in1=rstd[:], op=ALU.mult), [dd, rstd], [dd])
                              op("dve", lambda e: e.tensor_scalar(out=oaT[:, h, gsl], in0=dd[:], scalar1=ogcol[:, 0:1], scalar2=None, op0=ALU.mult), [dd, ogcol], [oaT])

                      acnt = {"s": 0}
                      clk = {"pe": 0.0, "dve": 0.0}
                      bgen = b_part1_gen()
                      a_loads(0)
                      a_stage1(0)
                      for i in range(len(its)):
                          h, m, G, kt, nkt = its[i]
                          if G == 0 and kt == 0 and 2 * h + m + 1 < 8:
                              a_loads(2 * h + m + 1)
                          if i + 1 < len(its):
                              a_stage1(i + 1)
                          a_stage2(i)
                          clk["pe"] += 1.25
                          while clk["dve"] < clk["pe"]:
                              c = next(bgen, None)
                              if c is None:
                                  break
                              clk["dve"] += c[0]; clk["pe"] += c[1]
                      for _ in bgen:
                          pass
                      S.barrier(); S.flush()

                with ExitStack() as esb:
                  if 'B' in stages:
                      A = Alloc(nc, esb, "p2b%d" % s)
                      skT = A.sb("skT", [64, T_SEQ], BF16)
                      svd = A.sb("svd", [128, NT, 128], BF16)
                      sqT = [A.sb("sqT%d" % i, [64, 8, 128], BF16) for i in range(3)]
                      PT = [A.sb("PTb%d" % i, [128, 4, 128], BF16) for i in range(3)]
                      oS = [A.sb("oS%d" % i, [128, 4, 128], F32) for i in range(2)]; zS = [A.sb("zS%d" % i, [128, 4, 128], F32) for i in range(2)]
                      ps_s = [A.ps("ps_sb%d" % i, [128, 4, 128], F32) for i in range(2)]
                      ps_o = [A.ps("ps_ob%d" % i, [128, 4, 128], F32) for i in range(2)]; ps_z = [A.ps("ps_zb%d" % i, [128, 4, 128], F32) for i in range(2)]
                      dma(lambda e: e.dma_start(out=skT[:], in_=qT_s[s][:, 24, :]), [], [skT], skT)
                      svv = dv_s[s].rearrange("(t p) c -> p t c", p=128)
                      dma(lambda e: e.dma_start(out=svd[:, :, 0:64], in_=svv[:, :, 512:576]), [], [svd], svd)
                      dma(lambda e: e.dma_start(out=svd[:, :, 64:128], in_=svv[:, :, 512:576]), [], [svd], svd)
                      its = [(t, half, kt) for t in range(NT) for half in range(2) for kt in range(t + 1)]

                      def sq_load(t):
                          sq_ = sqT[t % 3]
                          dma(lambda e: e.dma_start(out=sq_[:], in_=qT_s[s][:, 16:24, t * 128:(t + 1) * 128]), [], [sq_], sq_)

                      def st1(i):
                          t, half, kt = its[i]
                          sq_ = sqT[t % 3]
                          pss = ps_s[i % 2]; pt = PT[i % 3]
                          op("pe", lambda e: e.matmul(pss[:], lhsT=skT[:, kt * 128:(kt + 1) * 128], rhs=sq_[:, 4 * half:4 * half + 4, :], start=True, stop=True), [skT, sq_], [pss])
                          op("act", lambda e: e.activation(out=pt[:], in_=pss[:], func=AF.Exp, scale=0.125), [pss], [pt])
                          op("pool", lambda e: e.tensor_tensor(out=pt[:], in0=pt[:], in1=maskT_all[:, moff[t] + kt, :].unsqueeze(1).to_broadcast([128, 4, 128]), op=ALU.mult), [pt, maskT_all], [pt])

                      def st2(i):
                          t, half, kt = its[i]
                          pt = PT[i % 3]
                          grp = 2 * t + half
                          pso_ = ps_o[grp % 2]; psz_ = ps_z[grp % 2]
                          tsl = slice(t * 128, (t + 1) * 128)
                          op("pe", lambda e: e.matmul(pso_[:], lhsT=svd[:, kt, :], rhs=pt[:], start=(kt == 0), stop=(kt == t)), [svd, pt], [pso_])
                          op("pe", lambda e: e.matmul(psz_[:], lhsT=ones_b[:], rhs=pt[:], start=(kt == 0), stop=(kt == t)), [ones_b, pt], [psz_])
                          if kt != t:
                              return
                          oS_ = oS[grp % 2]; zS_ = zS[grp % 2]
                          op("act", lambda e: e.activation(out=oS_[:], in_=pso_[:], func=AF.Copy), [pso_], [oS_])
                          op("dve", lambda e: e.reciprocal(out=zS_[:], in_=psz_[:]), [psz_], [zS_])
                          for par in range(2):
                              psl = slice(par * 64, par * 64 + 64)
                              op("dve", lambda e, psl=psl, par=par: e.tensor_tensor(out=obT[psl, 2 * half:2 * half + 2, tsl], in0=oS_[psl, par::2, :], in1=zS_[psl, par::2, :], op=ALU.mult), [oS_, zS_], [obT])

                      sq_load(0); sq_load(1)
                      st1(0)
                      for i in range(len(its)):
                          t, half, kt = its[i]
                          if half == 0 and kt == 0 and t + 2 < NT:
                              sq_load(t + 2)
                          if i + 1 < len(its):
                              st1(i + 1)
                          st2(i)
                      S.barrier(); S.flush()

                with ExitStack() as esc:
                  if 'C' in stages:
                      A = Alloc(nc, esc, "p2c%d" % s)
                      gT = [A.sb("gT%d" % i, [128, 2, 512], BF16) for i in range(2)]
                      t1 = A.sb("t1", [128, 512], F32); t2 = A.sb("t2", [128, 512], F32)
                      mT = A.sb("mT", [128, 8, 512], BF16)
                      xt = [A.sb("xtc%d" % i, [128, DM], F32) for i in range(2)]
                      g1b = A.sb("g1b", [128, DM], F32); tmp3 = A.sb("tmp3", [128, DM], F32)
                      ps_a = A.ps("ps_a", [128, 512], F32); ps_b = A.ps("ps_b", [128, 512], F32)
                      ps_x = [A.ps("ps_x%d" % i, [128, 512], F32) for i in range(2)]
                      dma(lambda e: e.dma_start(out=g1b[:], in_=rows_d[s, 2:3, :].partition_broadcast(128)), [], [g1b], g1b)
                      if dbg:
                          dma(lambda e: e.dma_start(out=dbg_oa[s], in_=oaT[:]), [oaT], [], oaT)
                          dma(lambda e: e.dma_start(out=dbg_ob[s], in_=obT[:]), [obT], [], obT)
                          dma(lambda e: e.dma_start(out=dbg_mk[s], in_=maskT_all[:]), [maskT_all], [], maskT_all)
                      ic = 0
                      for G in range(4):
                          gsl = slice(G * 512, (G + 1) * 512)
                          for c in range(8):
                              gTi = gT[ic % 2]; ic += 1
                              dma(lambda e, gTi=gTi, c=c, gsl=gsl: e.dma_start(out=gTi[:], in_=gT_s[s][:, c::8, gsl]), [], [gTi], gTi)
                              for j in range(4):
                                  op("pe", lambda e, j=j, c=c, gsl=gsl: e.matmul(ps_a[:], lhsT=wba[:, j, c * 128:(c + 1) * 128], rhs=oaT[:, j, gsl], start=(j == 0), stop=(j == 3)), [wba, oaT], [ps_a])
                              for j in range(4):
                                  op("pe", lambda e, j=j, c=c, gsl=gsl: e.matmul(ps_b[:], lhsT=wbb[:, j, c * 128:(c + 1) * 128], rhs=obT[:, j, gsl], start=(j == 0), stop=(j == 3)), [wbb, obT], [ps_b])
                              op("dve", lambda e, gTi=gTi: e.tensor_tensor(out=t1[:], in0=ps_a[:], in1=gTi[:, 0, :], op=ALU.mult), [ps_a, gTi], [t1])
                              op("dve", lambda e, gTi=gTi: e.tensor_tensor(out=t2[:], in0=ps_b[:], in1=gTi[:, 1, :], op=ALU.mult), [ps_b, gTi], [t2])
                              op("pool", lambda e, c=c: e.tensor_tensor(out=mT[:, c, :], in0=t1[:], in1=t2[:], op=ALU.add), [t1, t2], [mT])
                          for q in range(4):
                              tt = 4 * G + q
                              r0 = s * T_SEQ + tt * 128
                              xti = xt[tt % 2]
                              dma(lambda e, xti=xti, r0=r0: e.dma_start(out=xti[:], in_=x[r0:r0 + 128, :]), [], [xti], xti)
                              for half in range(2):
                                  psx = ps_x[half]
                                  for c in range(8):
                                      op("pe", lambda e, psx=psx, c=c, q=q, half=half: e.matmul(psx[:], lhsT=mT[:, c, q * 128:(q + 1) * 128], rhs=wo[:, c, half * 512:(half + 1) * 512], start=(c == 0), stop=(c == 7)), [mT, wo], [psx])
                                  op("dve", lambda e, psx=psx, half=half: e.tensor_tensor(out=tmp3[:, half * 512:(half + 1) * 512], in0=psx[:], in1=g1b[:, half * 512:(half + 1) * 512], op=ALU.mult), [psx, g1b], [tmp3])
                              op("pool", lambda e, xti=xti: e.tensor_tensor(out=xti[:], in0=xti[:], in1=tmp3[:], op=ALU.add), [xti, tmp3], [xti])
                              dma(lambda e, xti=xti, r0=r0: e.dma_start(out=out[r0:r0 + 128, :], in_=xti[:]), [xti], [], xti)
                      S.barrier(); S.flush()

        if 3 in phases:
          with ExitStack() as es3:
            A = Alloc(nc, es3, "p3")
            wq_bf = A.sb("wq_bf", [128, 8, 2048], BF16); skb = A.sb("skb", [128, 16, 128], BF16)
            w_q_v = w_q.rearrange("(kc p) n -> p kc n", p=128)
            for j in range(4):
                dma(lambda e, j=j: e.dma_start(out=wq_bf[:, :, j * 512:(j + 1) * 512], in_=w_q_v[:, :, j * 512:(j + 1) * 512]), [], [wq_bf], wq_bf, eng="pool")
            dma(lambda e: e.dma_start(out=skb[:], in_=subkT), [], [skb], skb, eng="pool")
            xt = [A.sb("xt%d" % i, [128, DM], F32) for i in range(2)]
            junk = A.sb("junk", [128, DM], BF16); xn = A.sb("xn", [128, DM], BF16)
            ssq = [A.sb("ssq%d" % i, [128, 1], F32) for i in range(2)]
            h2T = A.sb("h2T", [128, 8, 128], BF16)
            h2 = [A.sb("h2_%d" % i, [128, 8, 128], BF16) for i in range(2)]
            tmpf = A.sb("tmpf", [128, DM], F32)
            G2b = [A.sb("G2b%d" % i, [128, DM], F32) for i in range(2)]
            qT = A.sb("qT", [128, 16, 128], BF16)
            sc_sb = A.sb("sc_sb", [128, 16, 128], F32)
            v16 = A.sb("v16", [128, 16, 16], F32); i16 = A.sb("i16", [128, 16, 16], U32); i16f = A.sb("i16f", [128, 16, 16], F32)
            cand = A.sb("cand", [128, 8, 256], F32)
            sc = A.sb("sc", [128, 8, 16], F32); posu = A.sb("posu", [128, 8, 16], U32)
            au = A.sb("au", [128, 8, 16], U32); bu = A.sb("bu", [128, 8, 16], U32)
            af = A.sb("af", [128, 8, 16], F32); bf_ = A.sb("bf_", [128, 8, 16], F32)
            eq = A.sb("eq", [128, 8, 16, 16], F32)
            e1 = A.sb("e1", [128, 8, 16], F32); e2 = A.sb("e2", [128, 8, 16], F32)
            eidx = [A.sb("eidx%d" % i, [128, 128], U32) for i in range(2)]
            ex = A.sb("ex", [128, 8, 16], F32); esum = A.sb("esum", [128, 8], F32)
            gg = [A.sb("gg%d" % i, [128, 128], F32) for i in range(2)]
            av = A.sb("av", [128, 128], F32)
            wg8 = [A.sb("wg8_%d" % i, [128, 8], F32) for i in range(2)]
            NR = 24
            uvg = [A.sb("uvg%d" % i, [128, 2 * DM], BF16) for i in range(NR)]
            djunk = A.sb("djunk", [128, DM], BF16)
            dg = [A.sb("dg%d" % i, [128, 128], BF16) for i in range(4)]
            pT = A.ps("pT3", [128, 8, 128], BF16)
            psq = [A.ps("psq%d" % i, [128, 4, 128], F32) for i in range(2)]
            pso = [[A.ps("pso%d_%d" % (i, j), [128, 512], F32) for j in range(2)] for i in range(2)]
            tiles = [(s, t) for s in range(ns) for t in range(NT)]

            def routing_pieces(gi):
                s, t = tiles[gi]
                i2 = gi % 2
                r0 = s * T_SEQ + t * 128
                xti = xt[i2]; ssi = ssq[i2]; eix = eidx[i2]; h2i = h2[i2]; ggi = gg[i2]
                P = []
                M = []

                def R(eng, fn, reads=(), writes=()):
                    M.append((eng, fn, reads, writes, None))

                def RD(fn, reads=(), writes=(), primary=None):
                    M.append(("dma", fn, reads, writes, primary))

                def rms_R(ssq_ap, n, res, nm):
                    R("dve", lambda e: e.tensor_scalar(out=ssq_ap, in0=ssq_ap, scalar1=1.0 / n, scalar2=EPS, op0=ALU.mult, op1=ALU.add), [res], [res])
                    R("act", lambda e: e.activation(out=ssq_ap, in_=ssq_ap, func=AF.Sqrt), [res], [res])
                    R("dve", lambda e: e.reciprocal(out=ssq_ap, in_=ssq_ap), [res], [res])

                def p_load():
                    if t == 0:
                        gb = G2b[s % 2]
                        RD(lambda e: e.dma_start(out=gb[:], in_=rows_d[s, 5:6, :].partition_broadcast(128)), [], [gb], gb)
                    RD(lambda e: e.dma_start(out=xti[:], in_=out[r0:r0 + 128, :]), [], [xti], xti)
                    R("act", lambda e: e.activation(out=junk[:], in_=xti[:], func=AF.Square, accum_out=ssi[:, 0:1]), [xti], [junk, ssi])
                    rms_R(ssi[:, 0:1], float(DM), ssi, "x")
                    R("act", lambda e: e.activation(out=xn[:], in_=xti[:], func=AF.Copy, scale=ssi[:, 0:1]), [xti, ssi], [xn])
                    for kc in range(8):
                        R("pe", lambda e, kc=kc: e.transpose(out=pT[:, kc, :], in_=xn[:, kc * 128:(kc + 1) * 128], identity=ident_b[:]), [xn, ident_b], [pT])
                    for kc in range(8):
                        R("act", lambda e, kc=kc: e.activation(out=h2T[:, kc, :], in_=pT[:, kc, :], func=AF.Identity, scale=cols[:, 2, kc, s:s + 1], bias=cols[:, 3, kc, s:s + 1]), [pT, cols], [h2T])
                    for kc in range(8):
                        R("pe", lambda e, kc=kc: e.transpose(out=pT[:, kc, :], in_=h2T[:, kc, :], identity=ident_b[:]), [h2T, ident_b], [pT])
                    R("act", lambda e: e.activation(out=h2i[:], in_=pT[:], func=AF.Copy), [pT], [h2i])
                P.append(p_load)

                def p_q(r0_, r1_):
                    def f():
                        for r in range(r0_, r1_):
                            pq_ = psq[r % 2]
                            for j in range(4):
                                hp = r * 4 + j
                                for kc in range(8):
                                    R("pe", lambda e, pq_=pq_, j=j, hp=hp, kc=kc: e.matmul(pq_[:, j, :], lhsT=wq_bf[:, kc, hp * 128:(hp + 1) * 128], rhs=h2T[:, kc, :], start=(kc == 0), stop=(kc == 7)), [wq_bf, h2T], [pq_])
                            R("act", lambda e, pq_=pq_, r=r: e.activation(out=qT[:, r * 4:(r + 1) * 4, :], in_=pq_[:], func=AF.Copy), [pq_], [qT])
                    return f
                P.append(p_q(0, 2)); P.append(p_q(2, 4))

                def p_sc():
                    for r in range(4):
                        pq_ = psq[r % 2]
                        for j in range(4):
                            hp = r * 4 + j
                            R("pe", lambda e, pq_=pq_, j=j, hp=hp: e.matmul(pq_[:, j, :], lhsT=qT[:, hp, :], rhs=skb[:, hp, :], start=True, stop=True), [qT, skb], [pq_])
                        R("act", lambda e, pq_=pq_, r=r: e.activation(out=sc_sb[:, r * 4:(r + 1) * 4, :], in_=pq_[:], func=AF.Copy), [pq_], [sc_sb])
                P.append(p_sc)

                def p_top(h0, h1):
                    def f():
                        for hp in range(h0, h1):
                            R("dve", lambda e, hp=hp: e.max(out=v16[:, hp, 0:8], in_=sc_sb[:, hp, :]), [sc_sb], [v16])
                            R("dve", lambda e, hp=hp: e.max_index(out=i16[:, hp, 0:8], in_max=v16[:, hp, 0:8], in_values=sc_sb[:, hp, :]), [sc_sb, v16], [i16])
                            R("dve", lambda e, hp=hp: e.match_replace(out=sc_sb[:, hp, :], in_to_replace=v16[:, hp, 0:8], in_values=sc_sb[:, hp, :], imm_value=NEG), [sc_sb, v16], [sc_sb])
                            R("dve", lambda e, hp=hp: e.max(out=v16[:, hp, 8:16], in_=sc_sb[:, hp, :]), [sc_sb], [v16])
                            R("dve", lambda e, hp=hp: e.max_index(out=i16[:, hp, 8:16], in_max=v16[:, hp, 8:16], in_values=sc_sb[:, hp, :]), [sc_sb, v16], [i16])
                    return f
                for q in range(4):
                    P.append(p_top(q * 4, q * 4 + 4))

                def p_cand():
                    v4 = v16[:, :, :].rearrange("p (h a) k -> p h a k", a=2)
                    c4 = cand[:, :, :].rearrange("p h (a b) -> p h a b", b=16)
                    R("dve", lambda e: e.tensor_tensor(out=c4, in0=v4[:, :, 0, :].unsqueeze(3).to_broadcast([128, 8, 16, 16]), in1=v4[:, :, 1, :].unsqueeze(2).to_broadcast([128, 8, 16, 16]), op=ALU.add), [v16], [cand])
                P.append(p_cand)

                def p_top2(h0, h1):
                    def f():
                        for h in range(h0, h1):
                            R("dve", lambda e, h=h: e.max(out=sc[:, h, 0:8], in_=cand[:, h, :]), [cand], [sc])
                            R("dve", lambda e, h=h: e.max_index(out=posu[:, h, 0:8], in_max=sc[:, h, 0:8], in_values=cand[:, h, :]), [cand, sc], [posu])
                            R("dve", lambda e, h=h: e.match_replace(out=cand[:, h, :], in_to_replace=sc[:, h, 0:8], in_values=cand[:, h, :], imm_value=NEG), [cand, sc], [cand])
                            R("dve", lambda e, h=h: e.max(out=sc[:, h, 8:16], in_=cand[:, h, :]), [cand], [sc])
                            R("dve", lambda e, h=h: e.max_index(out=posu[:, h, 8:16], in_max=sc[:, h, 8:16], in_values=cand[:, h, :]), [cand, sc], [posu])
                    return f
                for q in range(4):
                    P.append(p_top2(q * 2, q * 2 + 2))

                def p_idx():
                    R("dve", lambda e: e.tensor_single_scalar(out=au[:], in_=posu[:], scalar=4, op=ALU.logical_shift_right), [posu], [au])
                    R("dve", lambda e: e.tensor_single_scalar(out=bu[:], in_=posu[:], scalar=15, op=ALU.bitwise_and), [posu], [bu])
                    R("dve", lambda e: e.tensor_copy(out=af[:], in_=au[:]), [au], [af])
                    R("dve", lambda e: e.tensor_copy(out=bf_[:], in_=bu[:]), [bu], [bf_])
                    R("dve", lambda e: e.tensor_copy(out=i16f[:], in_=i16[:]), [i16], [i16f])
                    i4 = i16f[:, :, :].rearrange("p (h a) k -> p h a k", a=2)
                    io4 = iota16[:, :].unsqueeze(1).unsqueeze(1).to_broadcast([128, 8, 16, 16])
                    for (sel, which, dst) in ((af, 0, e1), (bf_, 1, e2)):
                        R("dve", lambda e, sel=sel: e.tensor_tensor(out=eq[:], in0=io4, in1=sel[:, :, :].unsqueeze(3).to_broadcast([128, 8, 16, 16]), op=ALU.is_equal), [iota16, sel], [eq])
                        R("dve", lambda e, which=which: e.tensor_tensor(out=eq[:], in0=eq[:], in1=i4[:, :, which, :].unsqueeze(2).to_broadcast([128, 8, 16, 16]), op=ALU.mult), [eq, i16f], [eq])
                        R("dve", lambda e, dst=dst: e.tensor_reduce(out=dst[:], in_=eq[:], axis=AX.X, op=ALU.add), [eq], [dst])
                    R("dve", lambda e: e.scalar_tensor_tensor(out=e1[:], in0=e1[:], scalar=128.0, in1=e2[:], op0=ALU.mult, op1=ALU.add), [e1, e2], [e1])
                    R("dve", lambda e: e.tensor_copy(out=eix[:], in_=e1[:, :, :].rearrange("p h k -> p (h k)")), [e1], [eix])
                P.append(p_idx)

                def p_soft():
                    R("dve", lambda e: e.tensor_tensor(out=ex[:], in0=sc[:], in1=sc[:, :, 0:1].to_broadcast([128, 8, 16]), op=ALU.subtract), [sc], [ex])
                    R("act", lambda e: e.activation(out=ex[:], in_=ex[:], func=AF.Exp), [ex], [ex])
                    R("dve", lambda e: e.tensor_reduce(out=esum[:], in_=ex[:], axis=AX.X, op=ALU.add), [ex], [esum])
                    R("dve", lambda e: e.reciprocal(out=esum[:], in_=esum[:]), [esum], [esum])
                    R("dve", lambda e: e.tensor_tensor(out=ggi[:, :].rearrange("p (h k) -> p h k", k=16), in0=ex[:], in1=esum[:, :].unsqueeze(2).to_broadcast([128, 8, 16]), op=ALU.mult), [ex, esum], [ggi])
                P.append(p_soft)
                for p in P:
                    p()
                return M

            gcnt = {"g": 0}
            avres = [[Res("avr%d_%d" % (i, j)) for j in range(8)] for i in range(2)]

            def gather_group(gi, grp, nxt):
                s, t = tiles[gi]
                i2 = gi % 2
                eix = eidx[i2]; h2i = h2[i2]; ggi = gg[i2]
                pso_ = pso[gi % 2]
                wg = wg8[grp % 2]
                bufs = []
                for j in range(8):
                    hk = grp * 8 + j
                    u_ = uvg[gcnt["g"] % NR]; gcnt["g"] += 1
                    bufs.append(u_)
                    dma(lambda e, u_=u_, hk=hk: e.indirect_dma_start(out=u_[:], out_offset=None, in_=uv_bf, in_offset=bass.IndirectOffsetOnAxis(ap=eix[:, hk:hk + 1], axis=0)), [eix], [u_], u_, eng="pool")
                    op("dve", lambda e, u_=u_, hk=hk: e.scalar_tensor_tensor(out=djunk[:], in0=u_[:, 0:DM], scalar=1.0, in1=h2i[:, :, :].rearrange("p a b -> p (a b)"), op0=ALU.mult, op1=ALU.mult, accum_out=av[:, hk:hk + 1]), [u_, h2i], [avres[grp % 2][j]])
                    if nxt is not None and nxt.ndve > 0:
                        dots_left = 128 - hk
                        nxt.pull_dve((nxt.ndve + dots_left - 1) // dots_left)
                gs_ = slice(grp * 8, grp * 8 + 8)
                op("act", lambda e: e.activation(out=wg[:], in_=av[:, gs_], func=AF.Gelu), avres[grp % 2], [wg])
                op("dve", lambda e: e.tensor_tensor(out=wg[:], in0=wg[:], in1=ggi[:, gs_], op=ALU.mult), [wg, ggi], [wg])
                for j in range(8):
                    hk = grp * 8 + j
                    u_ = bufs[j]; d_ = dg[hk % 4]
                    op("act", lambda e, d_=d_, j=j: e.activation(out=d_[:], in_=ident_b[:], func=AF.Copy, scale=wg[:, j:j + 1]), [ident_b, wg], [d_])
                    for half in range(2):
                        op("pe", lambda e, d_=d_, u_=u_, half=half, hk=hk: e.matmul(pso_[half][:], lhsT=d_[:], rhs=u_[:, DM + half * 512:DM + (half + 1) * 512], start=(hk == 0), stop=(hk == 127)), [d_, u_], [pso_[half]])

            def final(gi):
                s, t = tiles[gi]
                r0 = s * T_SEQ + t * 128
                xti = xt[gi % 2]; pso_ = pso[gi % 2]; gb = G2b[s % 2]
                for half in range(2):
                    hs = slice(half * 512, (half + 1) * 512)
                    op("dve", lambda e, half=half, hs=hs: e.tensor_tensor(out=tmpf[:, hs], in0=pso_[half][:], in1=gb[:, hs], op=ALU.mult), [pso_[half], gb], [tmpf])
                op("dve", lambda e: e.tensor_tensor(out=xti[:], in0=xti[:], in1=tmpf[:], op=ALU.add), [xti, tmpf], [xti])
                dma(lambda e: e.dma_start(out=out[r0:r0 + 128, :], in_=xti[:]), [xti], [], xti)

            def emit_micro(mo):
                eng, fn, reads, writes, primary = mo
                if eng == "dma":
                    dma(fn, reads, writes, primary)
                else:
                    op(eng, fn, reads, writes)

            class RQ:
                def __init__(self, M):
                    self.M = M; self.i = 0
                    self.ndve = sum(1 for m in M if m[0] == "dve")

                def pull_dve(self, k):
                    while k > 0 and self.i < len(self.M):
                        mo = self.M[self.i]; self.i += 1
                        emit_micro(mo)
                        if mo[0] == "dve":
                            k -= 1; self.ndve -= 1

                def drain(self):
                    while self.i < len(self.M):
                        emit_micro(self.M[self.i]); self.i += 1

            RQ(routing_pieces(0)).drain()
            for gi in range(len(tiles)):
                nxt = RQ(routing_pieces(gi + 1)) if gi + 1 < len(tiles) else None
                for grp in range(16):
                    gather_group(gi, grp, nxt)
                if nxt is not None:
                    nxt.drain()
                final(gi)
            S.barrier(); S.flush()
    return nc


def _core_inputs(inp, b0, ns):
    m = {}
    m["x"] = np.ascontiguousarray(inp["x"][b0:b0 + ns].reshape(ns * T_SEQ, DM))
    m["c"] = np.ascontiguousarray(inp["c"][b0:b0 + ns])
    m["pos"] = np.ascontiguousarray(np.asarray(inp["positions"][b0:b0 + ns]).reshape(ns, NT, 128).transpose(0, 2, 1)).astype(np.int32)
    return m


def kernel(**inputs):
    inp = {k: np.asarray(v) for k, v in inputs.items()}
    n_cores = 8
    ns = inp["x"].shape[0] // n_cores
    shared = {}
    shared["inv"] = (np.float32(10000.0) ** (-(np.arange(0, 64, 2, dtype=np.float32) / np.float32(64)))).astype(np.float32).reshape(1, 32)
    for k in ("w_ada", "w_in", "w_branch_a", "w_branch_b", "w_out", "peer_w_q", "peer_u", "peer_v"):
        shared[k] = np.ascontiguousarray(inp[k][0], dtype=np.float32)
    for k in ("b_ada", "norm1_g", "norm2_g", "diff_q_g", "diff_k_g", "dsa_q_g", "dsa_k_g",
              "diff_lam_q1", "diff_lam_k1", "diff_lam_q2", "diff_lam_k2"):
        shared[k] = np.ascontiguousarray(inp[k][0].reshape(1, -1), dtype=np.float32)
    shared["diff_out_g"] = np.ascontiguousarray(inp["diff_out_g"][0].reshape(128, 1), dtype=np.float32)
    shared["subkT"] = np.ascontiguousarray(inp["peer_sub_keys"][0].reshape(16, 128, 128).transpose(2, 0, 1), dtype=np.float32)
    in_maps = []
    for ci in range(n_cores):
        m = dict(shared)
        m.update(_core_inputs(inp, ci * ns, ns))
        in_maps.append(m)
    nc = build(ns)
    res = run_bass_kernel_spmd(nc, in_maps, core_ids=list(range(n_cores)))
    outs = [np.asarray(r["out"]).reshape(ns, T_SEQ, DM) for r in res.results]
    return np.concatenate(outs, axis=0).astype(np.float32)
```

```python
import numpy as np, math
from contextlib import ExitStack
import concourse.bass as bass
import concourse.mybir as mybir
from concourse.bass_utils import run_bass_kernel_spmd

F32 = mybir.dt.float32
BF16 = mybir.dt.bfloat16
I32 = mybir.dt.int32
U32 = mybir.dt.uint32
ALU = mybir.AluOpType
AF = mybir.ActivationFunctionType
AX = mybir.AxisListType


class Res:
    __slots__ = ("name", "w", "r", "dsem", "dcnt")

    def __init__(self, name):
        self.name = name
        self.w = []
        self.r = []
        self.dsem = None
        self.dcnt = 0


class Sched:
    ENGS = ("pe", "act", "dve", "pool", "sp")

    def __init__(self, nc, es):
        self.nc = nc
        self.es = es
        self.items = {e: [] for e in self.ENGS}
        self.cnt = {e: 0 for e in self.ENGS}
        self.sems = {}
        for e in self.ENGS:
            if e != "sp":
                self.sems[e] = es.enter_context(nc.semaphore("sem_" + e))
        self.known = {e: {} for e in self.ENGS}
        self.dstate = {}
        self.ninst = 0

    def _dstate(self, res):
        st = self.dstate.get(res.name)
        if st is None:
            sem = self.es.enter_context(self.nc.semaphore("dsem_%d_%s" % (len(self.dstate), res.name)))
            st = [sem, 0]
            self.dstate[res.name] = st
            self.sems[("d", res.name)] = sem
        return st

    def _collect(self, eng, reads, writes):
        deps = []
        for r in reads:
            deps += r.w
        for w in writes:
            deps += w.w
            deps += w.r
        out = {}
        for (k, v) in deps:
            if eng == "pe" and k == "pe":
                continue
            if self.known[eng].get(k, 0) >= v:
                continue
            if out.get(k, 0) < v:
                out[k] = v
        for k, v in out.items():
            self.known[eng][k] = v
        return list(out.items())

    def _update(self, dep, reads, writes):
        for r in reads:
            if r not in writes:
                r.r.append(dep)
        for w in writes:
            w.w = [dep]
            w.r = []

    def op(self, eng, fn, reads=(), writes=()):
        reads = list(reads); writes = list(writes)
        waits = self._collect(eng, reads, writes)
        self.cnt[eng] += 1
        dep = (eng, self.cnt[eng])
        self.items[eng].append((waits, fn, (eng, 1)))
        self._update(dep, reads, writes)
        self.ninst += 1

    def dma(self, fn, reads=(), writes=(), primary=None, eng="sp"):
        reads = list(reads); writes = list(writes)
        waits = self._collect(eng, reads, writes)
        st = self._dstate(primary)
        st[1] += 16
        key = ("d", primary.name)
        dep = (key, st[1])
        self.items[eng].append((waits, fn, (key, 16)))
        self._update(dep, reads, writes)
        self.ninst += 1

    def barrier(self):
        allv = []
        for e in self.ENGS:
            if e != "sp" and self.cnt[e] > 0:
                allv.append((e, self.cnt[e]))
        for nm, st in self.dstate.items():
            allv.append((("d", nm), st[1]))
        for e in self.ENGS:
            waits = []
            for (k, v) in allv:
                if self.known[e].get(k, 0) < v:
                    waits.append((k, v))
                    self.known[e][k] = v
            if waits:
                self.items[e].append((waits, None, None))

    def flush(self, name=None):
        nc = self.nc
        items = self.items
        sems = self.sems

        def replay(engname):
            def run(eng):
                for (waits, fn, inc) in items[engname]:
                    for (k, v) in waits:
                        eng.wait_ge(sems[k], v)
                    if fn is not None:
                        inst = fn(eng)
                        inst.then_inc(sems[inc[0]], inc[1])
            return run

        with nc.Block() as block:
            if items["sp"]:
                block.sync(replay("sp"))
            if items["act"]:
                block.scalar(replay("act"))
            if items["dve"]:
                block.vector(replay("dve"))
            if items["pool"]:
                block.gpsimd(replay("pool"))
            if items["pe"]:
                block.tensor(replay("pe"))
        self.items = {e: [] for e in self.ENGS}
NS_FULL = 4
T_SEQ = 2048
NT = 16
DM = 1024
IN_COLS = 4808
NPROJ = 2760
GATE0 = 2760
EPS = 1e-6
LAMBDA_INIT = 0.8 - 0.6 * math.exp(-0.0)
NEG = -1.0e30
TWO_PI = 2.0 * math.pi


class T_:
    __slots__ = ("t", "r")

    def __init__(self, t, name):
        self.t = t
        self.r = Res(name)

    def __getitem__(self, k):
        return self.t[k]


class Alloc:
    def __init__(self, nc, es, prefix):
        self.nc, self.es, self.p = nc, es, prefix
        self.n = 0

    def sb(self, name, shape, dt):
        self.n += 1
        return T_(self.es.enter_context(self.nc.sbuf_tensor("%s_%s_%d" % (self.p, name, self.n), shape, dt)), name)

    def ps(self, name, shape, dt):
        self.n += 1
        return T_(self.es.enter_context(self.nc.psum_tensor("%s_%s_%d" % (self.p, name, self.n), shape, dt)), name)


def _rs(lst):
    return [x.r if isinstance(x, T_) else x for x in lst]


class K:
    def __init__(self, nc, S, ns):
        self.nc, self.S, self.ns = nc, S, ns

    def op(self, eng, fn, reads=(), writes=()):
        self.S.op(eng, fn, _rs(reads), _rs(writes))

    def dma(self, fn, reads=(), writes=(), primary=None, eng="sp"):
        self.S.dma(fn, _rs(reads), _rs(writes), primary.r if isinstance(primary, T_) else primary, eng)

def build(ns, phases=(0, 1, 2, 3), dbg=False, stages="ABC"):
    nc = bass.Bass("TRN2", target_bir_lowering=False)
    NTOK = ns * T_SEQ

    def din(name, shape, dt=F32):
        return nc.dram_tensor(name, shape, dt, kind="ExternalInput").ap()

    def dscr(name, shape, dt):
        return nc.dram_tensor(name, shape, dt, kind=("ExternalOutput" if dbg else "Internal")).ap()

    x = din("x", [NTOK, DM]); c_in = din("c", [ns, DM]); pos = din("pos", [ns, 128, NT], I32); inv = din("inv", [1, 32])
    w_ada = din("w_ada", [DM, 6 * DM]); b_ada = din("b_ada", [1, 6 * DM]); n1g = din("norm1_g", [1, DM]); w_in = din("w_in", [DM, IN_COLS])
    gains_in = [din(n, [1, 64]) for n in ("diff_q_g", "diff_k_g", "dsa_q_g", "dsa_k_g")]
    lam_in = [din(n, [1, 64]) for n in ("diff_lam_q1", "diff_lam_k1", "diff_lam_q2", "diff_lam_k2")]
    og_in = din("diff_out_g", [128, 1])
    w_ba = din("w_branch_a", [512, DM]); w_bb = din("w_branch_b", [512, DM]); w_o = din("w_out", [DM, DM]); n2g = din("norm2_g", [1, DM])
    w_q = din("peer_w_q", [DM, 2048]); subkT = din("subkT", [128, 16, 128]); pu = din("peer_u", [16384, DM]); pv = din("peer_v", [16384, DM])
    out = nc.dram_tensor("out", [NTOK, DM], F32, kind="ExternalOutput").ap()

    uv_bf = dscr("uv_bf", [16384, 2 * DM], BF16)
    rows_d = dscr("rows_d", [ns, 6, DM], F32)
    qT_s = dscr("qT_s", [ns, 64, 34, T_SEQ], BF16)
    dv_s = dscr("dv_s", [ns, T_SEQ, 576], BF16)
    iw_s = dscr("iw_s", [ns, T_SEQ, 8], F32)
    gT_s = dscr("gT_s", [ns, 128, 16, T_SEQ], BF16)
    if dbg:
        dbg_oa = dscr("dbg_oa", [ns, 128, 4, T_SEQ], BF16); dbg_ob = dscr("dbg_ob", [ns, 128, 4, T_SEQ], BF16)
        dbg_mk = dscr("dbg_mk", [ns, 128, 136, 128], BF16)

    ges = ExitStack()
    with ges:
        S = Sched(nc, ges)
        kk = K(nc, S, ns)
        op, dma = kk.op, kk.dma
        GA = Alloc(nc, ges, "g")
        ident_f = GA.sb("ident_f", [128, 128], F32); ident_b = GA.sb("ident_b", [128, 128], BF16)
        ones_b = GA.sb("ones_b", [128, 128], BF16)
        cols = GA.sb("cols", [128, 4, 8, ns], F32)
        neglam = GA.sb("neglam", [128, 1], F32); ogcol = GA.sb("ogcol", [128, 1], F32)
        gains_b = GA.sb("gains_b", [128, 4, 64], F32); inv_b = GA.sb("inv_b", [128, 32], F32)
        iota16 = GA.sb("iota16", [128, 16], F32)

        with ExitStack() as es0:
            A = Alloc(nc, es0, "p0")
            if 3 in phases:
                R_uv = Res("uvcast")
                for (src, c0) in ((pu, 0), (pv, DM)):
                    for i in range(16):
                        dma(lambda e, src=src, c0=c0, i=i: e.dma_start(out=uv_bf[i * 1024:(i + 1) * 1024, c0:c0 + DM], in_=src[i * 1024:(i + 1) * 1024, :]), [], [], R_uv, eng="pool")
            op("pool", lambda e: e.memset(ident_f[:], 0.0), [], [ident_f])
            op("pool", lambda e: e.affine_select(out=ident_f[:], in_=ident_f[:], compare_op=ALU.not_equal, fill=1.0, base=0, pattern=[[-1, 128]], channel_multiplier=1), [ident_f], [ident_f])
            op("dve", lambda e: e.tensor_copy(out=ident_b[:], in_=ident_f[:]), [ident_f], [ident_b])
            op("dve", lambda e: e.memset(ones_b[:], 1.0), [], [ones_b])
            op("pool", lambda e: e.iota(iota16[:], pattern=[[1, 16]], base=0, channel_multiplier=0, allow_small_or_imprecise_dtypes=True), [], [iota16])
            for i in range(4):
                dma(lambda e, i=i: e.dma_start(out=gains_b[:, i, :], in_=gains_in[i].partition_broadcast(128)), [], [gains_b], gains_b)
            dma(lambda e: e.dma_start(out=inv_b[:], in_=inv.partition_broadcast(128)), [], [inv_b], inv_b)
            dma(lambda e: e.dma_start(out=ogcol[:], in_=og_in), [], [ogcol], ogcol)
            op("dve", lambda e: e.tensor_scalar(out=ogcol[:], in0=ogcol[:], scalar1=1.0 - LAMBDA_INIT, scalar2=None, op0=ALU.mult), [ogcol], [ogcol])
            lamv = A.sb("lamv", [128, 4, 64], F32); lamp = A.sb("lamp", [128, 2, 64], F32); lams = A.sb("lams", [128, 2], F32)
            for i in range(4):
                dma(lambda e, i=i: e.dma_start(out=lamv[:, i, :], in_=lam_in[i].partition_broadcast(128)), [], [lamv], lamv)
            lv4 = lamv[:, :, :].rearrange("p (a b) d -> p a b d", b=2)
            op("dve", lambda e: e.tensor_tensor(out=lamp[:], in0=lv4[:, :, 0, :], in1=lv4[:, :, 1, :], op=ALU.mult), [lamv], [lamp])
            op("dve", lambda e: e.tensor_reduce(out=lams[:], in_=lamp[:], axis=AX.X, op=ALU.add), [lamp], [lams])
            op("act", lambda e: e.activation(out=lams[:], in_=lams[:], func=AF.Exp), [lams], [lams])
            op("dve", lambda e: e.tensor_tensor(out=neglam[:], in0=lams[:, 1:2], in1=lams[:, 0:1], op=ALU.subtract), [lams], [neglam])
            op("dve", lambda e: e.tensor_scalar(out=neglam[:], in0=neglam[:], scalar1=-LAMBDA_INIT, scalar2=None, op0=ALU.add), [neglam], [neglam])

            c_sb = A.sb("c_sb", [ns, DM], F32); sg = A.sb("sg", [ns, DM], F32); scT = A.sb("scT", [128, 8, ns], F32)
            pc0 = A.ps("pc0", [128, 8, ns], F32)
            dma(lambda e: e.dma_start(out=c_sb[:], in_=c_in), [], [c_sb], c_sb)
            op("act", lambda e: e.activation(out=sg[:], in_=c_sb[:], func=AF.Sigmoid), [c_sb], [sg])
            op("dve", lambda e: e.tensor_tensor(out=c_sb[:], in0=c_sb[:], in1=sg[:], op=ALU.mult), [c_sb, sg], [c_sb])
            for kc in range(8):
                op("pe", lambda e, kc=kc: e.transpose(out=pc0[:, kc, :], in_=c_sb[0:ns, kc * 128:(kc + 1) * 128], identity=ident_f[0:ns, 0:ns]), [c_sb, ident_f], [pc0])
            op("dve", lambda e: e.tensor_copy(out=scT[:], in_=pc0[:]), [pc0], [scT])
            wst = [A.sb("wst%d" % i, [128, 8, 512], F32) for i in range(2)]
            b_b = A.sb("b_b", [ns, 6 * DM], F32); mod_sb = A.sb("mod_sb", [ns, 6 * DM], F32)
            pm = [A.ps("pm%d" % i, [ns, 512], F32) for i in range(2)]
            dma(lambda e: e.dma_start(out=b_b[:], in_=b_ada.partition_broadcast(ns)), [], [b_b], b_b)
            w_ada_v = w_ada.rearrange("(kc p) n -> p kc n", p=128)
            for j in range(12):
                ws = wst[j % 2]; pmj = pm[j % 2]
                dma(lambda e, ws=ws, j=j: e.dma_start(out=ws[:], in_=w_ada_v[:, :, j * 512:(j + 1) * 512]), [], [ws], ws)
                for kc in range(8):
                    op("pe", lambda e, ws=ws, pmj=pmj, kc=kc: e.matmul(pmj[:], lhsT=scT[:, kc, :], rhs=ws[:, kc, :], start=(kc == 0), stop=(kc == 7)), [scT, ws], [pmj])
                op("dve", lambda e, pmj=pmj, j=j: e.tensor_tensor(out=mod_sb[:, j * 512:(j + 1) * 512], in0=pmj[:], in1=b_b[:, j * 512:(j + 1) * 512], op=ALU.add), [pmj, b_b], [mod_sb])
            rows_sb = A.sb("rows_sb", [ns, 6, DM], F32); ng_b = A.sb("ng_b", [ns, 2, DM], F32)
            dma(lambda e: e.dma_start(out=ng_b[:, 0, :], in_=n1g.partition_broadcast(ns)), [], [ng_b], ng_b)
            dma(lambda e: e.dma_start(out=ng_b[:, 1, :], in_=n2g.partition_broadcast(ns)), [], [ng_b], ng_b)
            for half in range(2):
                o = half * 3 * DM
                op("dve", lambda e, o=o, half=half: e.scalar_tensor_tensor(out=rows_sb[:, half * 3 + 0, :], in0=mod_sb[:, o + DM:o + 2 * DM], scalar=1.0, in1=ng_b[:, half, :], op0=ALU.add, op1=ALU.mult), [mod_sb, ng_b], [rows_sb])
                op("dve", lambda e, o=o, half=half: e.tensor_copy(out=rows_sb[:, half * 3 + 1, :], in_=mod_sb[:, o:o + DM]), [mod_sb], [rows_sb])
                op("dve", lambda e, o=o, half=half: e.tensor_copy(out=rows_sb[:, half * 3 + 2, :], in_=mod_sb[:, o + 2 * DM:o + 3 * DM]), [mod_sb], [rows_sb])
            dma(lambda e: e.dma_start(out=rows_d, in_=rows_sb[:]), [rows_sb], [], rows_sb)
            pc1 = A.ps("pc1", [128, 4, 8, ns], F32)
            for wi, ri in enumerate((0, 1, 3, 4)):
                for kc in range(8):
                    op("pe", lambda e, wi=wi, ri=ri, kc=kc: e.transpose(out=pc1[:, wi, kc, :], in_=rows_sb[0:ns, ri, kc * 128:(kc + 1) * 128], identity=ident_f[0:ns, 0:ns]), [rows_sb, ident_f], [pc1])
            op("dve", lambda e: e.tensor_copy(out=cols[:], in_=pc1[:]), [pc1], [cols])

            S.barrier()
            S.flush()

        def rope_alloc(A):
            return (A.sb("posi", [128, NT], I32), A.sb("posf", [128, NT], F32), A.sb("ang", [128, NT, 32], F32),
                    A.sb("a2", [128, NT, 32], F32), A.sb("ki", [128, NT, 32], I32), A.sb("kf", [128, NT, 32], F32))

        def rope_tables(rb, s, cos_t, sin_t):
            posi, posf, ang, a2, ki, kf = rb
            dma(lambda e: e.dma_start(out=posi[:], in_=pos[s]), [], [posi], posi)
            op("dve", lambda e: e.tensor_copy(out=posf[:], in_=posi[:]), [posi], [posf])
            op("dve", lambda e: e.tensor_tensor(out=ang[:], in0=posf[:, :].unsqueeze(2).to_broadcast([128, NT, 32]), in1=inv_b[:, :].unsqueeze(1).to_broadcast([128, NT, 32]), op=ALU.mult), [posf, inv_b], [ang])
            for (shift, dst) in ((0.0, sin_t), (0.5 * math.pi, cos_t)):
                op("dve", lambda e, shift=shift: e.tensor_scalar(out=a2[:], in0=ang[:], scalar1=shift, scalar2=None, op0=ALU.add), [ang], [a2])
                op("dve", lambda e: e.tensor_scalar(out=ki[:], in0=a2[:], scalar1=1.0 / TWO_PI, scalar2=None, op0=ALU.mult), [a2], [ki])
                op("dve", lambda e: e.tensor_copy(out=kf[:], in_=ki[:]), [ki], [kf])
                op("dve", lambda e: e.scalar_tensor_tensor(out=a2[:], in0=kf[:], scalar=-TWO_PI, in1=a2[:], op0=ALU.mult, op1=ALU.add), [kf, a2], [a2])
                op("dve", lambda e: e.tensor_scalar(out=kf[:], in0=a2[:], scalar1=math.pi, scalar2=TWO_PI, op0=ALU.is_gt, op1=ALU.mult), [a2], [kf])
                op("dve", lambda e: e.tensor_tensor(out=a2[:], in0=a2[:], in1=kf[:], op=ALU.subtract), [a2, kf], [a2])
                op("dve", lambda e: e.tensor_scalar(out=kf[:], in0=a2[:], scalar1=-math.pi, scalar2=TWO_PI, op0=ALU.is_lt, op1=ALU.mult), [a2], [kf])
                op("dve", lambda e: e.tensor_tensor(out=a2[:], in0=a2[:], in1=kf[:], op=ALU.add), [a2, kf], [a2])
                op("act", lambda e, dst=dst: e.activation(out=dst[:], in_=a2[:], func=AF.Sin), [a2], [dst])

        def rms_rstd(A, ssq_ap, n, res, nm):
            op("dve", lambda e: e.tensor_scalar(out=ssq_ap, in0=ssq_ap, scalar1=1.0 / n, scalar2=EPS, op0=ALU.mult, op1=ALU.add), [res], [res])
            op("act", lambda e: e.activation(out=ssq_ap, in_=ssq_ap, func=AF.Sqrt), [res], [res])
            op("dve", lambda e: e.reciprocal(out=ssq_ap, in_=ssq_ap), [res], [res])

        if 1 in phases:
          with ExitStack() as es1:
            A = Alloc(nc, es1, "p1")
            w_in_bf = A.sb("w_in_bf", [128, 8, IN_COLS], BF16)
            w_in_v = w_in.rearrange("(kc p) n -> p kc n", p=128)
            nch = (IN_COLS + 511) // 512
            for j in range(nch):
                lo = j * 512; hi = min(IN_COLS, lo + 512)
                dma(lambda e, lo=lo, hi=hi: e.dma_start(out=w_in_bf[:, :, lo:hi], in_=w_in_v[:, :, lo:hi]), [], [w_in_bf], w_in_bf, eng="pool")
            cos_tr = [A.sb("cos_t%d" % i, [128, NT, 32], F32) for i in range(2)]; sin_tr = [A.sb("sin_t%d" % i, [128, NT, 32], F32) for i in range(2)]
            xt = [A.sb("xt%d" % i, [128, DM], F32) for i in range(2)]
            junk = A.sb("junk", [128, DM], BF16); xnr = [A.sb("xn%d" % i, [128, DM], BF16) for i in range(2)]
            ssq = [A.sb("ssq%d" % i, [128, 1], F32) for i in range(2)]
            hT = [A.sb("hT%d" % i, [128, 8, 128], BF16) for i in range(2)]
            projr = [A.sb("proj%d" % i, [128, NPROJ], F32) for i in range(2)]
            sqbr = [A.sb("sqb%d" % i, [128, 1600], F32) for i in range(2)]; ssqgr = [A.sb("ssqg%d" % i, [128, 25], F32) for i in range(2)]
            rqr = [A.sb("rq%d" % i, [128, 34, 64], BF16) for i in range(2)]
            tv = [A.sb("tv%d" % i, [128, 16, 32], F32) for i in range(2)]
            tp = [A.sb("tp%d" % i, [128, 9, 32], F32) for i in range(2)]
            dvb = [A.sb("dvb%d" % i, [128, 576], BF16) for i in range(2)]
            iwb = [A.sb("iwb%d" % i, [128, 8], F32) for i in range(2)]
            gs = [A.sb("gs%d" % i, [128, 16, 128], BF16) for i in range(2)]
            qTs = [A.sb("qTs%d" % i, [64, 34, 128], BF16) for i in range(2)]
            pTr = [A.ps("pT%d" % i, [128, 8, 128], BF16) for i in range(2)]
            pp = [A.ps("pp%d" % i, [128, 512], F32) for i in range(2)]
            pg = [A.ps("pg%d" % i, [128, 4, 128], F32) for i in range(2)]
            pq = [A.ps("pq%d" % i, [64, 8, 128], BF16) for i in range(2)]
            bounds = [0, 512, 1024, 1536, 2048, 2560, NPROJ]
            rb = rope_alloc(A)
            def p1_tile(s, t, gi):
                i2 = gi % 2
                r0 = s * T_SEQ + t * 128
                xti = xt[i2]; ssi = ssq[i2]; hTi = hT[i2]
                xn = xnr[i2]; proj = projr[i2]; sqb = sqbr[i2]; ssqg = ssqgr[i2]; rq = rqr[i2]; pT = pTr[i2]
                cos_t = cos_tr[s % 2]; sin_t = sin_tr[s % 2]
                dma(lambda e, xti=xti, r0=r0: e.dma_start(out=xti[:], in_=x[r0:r0 + 128, :]), [], [xti], xti)
                op("act", lambda e, xti=xti, ssi=ssi: e.activation(out=junk[:], in_=xti[:], func=AF.Square, accum_out=ssi[:, 0:1]), [xti], [junk, ssi])
                rms_rstd(A, ssi[:, 0:1], float(DM), ssi, "x")
                op("act", lambda e, xti=xti, ssi=ssi: e.activation(out=xn[:], in_=xti[:], func=AF.Copy, scale=ssi[:, 0:1]), [xti, ssi], [xn])
                yield
                for kc in range(8):
                    op("pe", lambda e, kc=kc: e.transpose(out=pT[:, kc, :], in_=xn[:, kc * 128:(kc + 1) * 128], identity=ident_b[:]), [xn, ident_b], [pT])
                for kc in range(8):
                    op("dve", lambda e, kc=kc, hTi=hTi, s=s: e.tensor_scalar(out=hTi[:, kc, :], in0=pT[:, kc, :], scalar1=cols[:, 0, kc, s:s + 1], scalar2=cols[:, 1, kc, s:s + 1], op0=ALU.mult, op1=ALU.add), [pT, cols], [hTi])
                yield
                for j in range(6):
                    lo, hi = bounds[j], bounds[j + 1]; w = hi - lo
                    ppj = pp[j % 2]
                    for kc in range(8):
                        op("pe", lambda e, ppj=ppj, kc=kc, lo=lo, hi=hi, w=w, hTi=hTi: e.matmul(ppj[:, 0:w], lhsT=hTi[:, kc, :], rhs=w_in_bf[:, kc, lo:hi], start=(kc == 0), stop=(kc == 7)), [hTi, w_in_bf], [ppj])
                    op("act", lambda e, ppj=ppj, lo=lo, hi=hi, w=w: e.activation(out=proj[:, lo:hi], in_=ppj[:, 0:w], func=AF.Copy), [ppj], [proj])
                    if j == 2 or j == 5:
                        yield
                gsi = gs[i2]

                def gates_half(ra, rb_):
                    for r in range(ra, rb_):
                        pgr = pg[r % 2]
                        for j in range(4):
                            gc = r * 4 + j
                            for kc in range(8):
                                op("pe", lambda e, pgr=pgr, j=j, gc=gc, kc=kc: e.matmul(pgr[:, j, :], lhsT=w_in_bf[:, kc, GATE0 + gc * 128:GATE0 + (gc + 1) * 128], rhs=hTi[:, kc, :], start=(kc == 0), stop=(kc == 7)), [hTi, w_in_bf], [pgr])
                        op("act", lambda e, pgr=pgr, r=r: e.activation(out=gsi[:, r * 4:(r + 1) * 4, :], in_=pgr[:], func=AF.Sigmoid), [pgr], [gsi])
                gates_half(0, 2)
                yield
                dvi = dvb[i2]; iwi = iwb[i2]
                op("pool", lambda e, dvi=dvi: e.tensor_copy(out=dvi[:, 0:512], in_=proj[:, 1024:1536]), [proj], [dvi])
                op("pool", lambda e, dvi=dvi: e.tensor_copy(out=dvi[:, 512:576], in_=proj[:, 2112:2176]), [proj], [dvi])
                op("pool", lambda e, iwi=iwi: e.tensor_copy(out=iwi[:], in_=proj[:, 2752:2760]), [proj], [iwi])
                dma(lambda e, dvi=dvi, s=s, t=t: e.dma_start(out=dv_s[s][t * 128:(t + 1) * 128, :], in_=dvi[:]), [dvi], [], dvi)
                dma(lambda e, iwi=iwi, s=s, t=t: e.dma_start(out=iw_s[s][t * 128:(t + 1) * 128, :], in_=iwi[:]), [iwi], [], iwi)
                op("pool", lambda e: e.tensor_tensor(out=sqb[:, 0:1024], in0=proj[:, 0:1024], in1=proj[:, 0:1024], op=ALU.mult), [proj], [sqb])
                op("pool", lambda e: e.tensor_tensor(out=sqb[:, 1024:1600], in0=proj[:, 1536:2112], in1=proj[:, 1536:2112], op=ALU.mult), [proj], [sqb])
                op("dve", lambda e: e.tensor_reduce(out=ssqg[:, 0:25], in_=sqb[:, :].rearrange("p (g d) -> p g d", d=64), axis=AX.X, op=ALU.add), [sqb], [ssqg])
                rms_rstd(A, ssqg[:, :], 64.0, ssqg, "g")
                pA = proj[:, 0:1024].rearrange("p (g d) -> p g d", d=64)
                pB = proj[:, 1536:2112].rearrange("p (g d) -> p g d", d=64)
                op("dve", lambda e, pA=pA: e.tensor_tensor(out=pA, in0=pA, in1=ssqg[:, 0:16].unsqueeze(2).to_broadcast([128, 16, 64]), op=ALU.mult), [proj, ssqg], [proj])
                op("dve", lambda e, pB=pB: e.tensor_tensor(out=pB, in0=pB, in1=ssqg[:, 16:25].unsqueeze(2).to_broadcast([128, 9, 64]), op=ALU.mult), [proj, ssqg], [proj])
                op("dve", lambda e, pA=pA: e.tensor_tensor(out=pA[:, 0:8, :], in0=pA[:, 0:8, :], in1=gains_b[:, 0, :].unsqueeze(1).to_broadcast([128, 8, 64]), op=ALU.mult), [proj, gains_b], [proj])
                op("dve", lambda e, pA=pA: e.tensor_tensor(out=pA[:, 8:16, :], in0=pA[:, 8:16, :], in1=gains_b[:, 1, :].unsqueeze(1).to_broadcast([128, 8, 64]), op=ALU.mult), [proj, gains_b], [proj])
                op("dve", lambda e, pB=pB: e.tensor_tensor(out=pB[:, 0:8, :], in0=pB[:, 0:8, :], in1=gains_b[:, 2, :].unsqueeze(1).to_broadcast([128, 8, 64]), op=ALU.mult), [proj, gains_b], [proj])
                op("dve", lambda e, pB=pB: e.tensor_tensor(out=pB[:, 8:9, :], in0=pB[:, 8:9, :], in1=gains_b[:, 3, :].unsqueeze(1).to_broadcast([128, 1, 64]), op=ALU.mult), [proj, gains_b], [proj])
                yield
                gates_half(2, 4)
                dma(lambda e: e.dma_start(out=gT_s[s][:, :, t * 128:(t + 1) * 128], in_=gsi[:]), [gsi], [], gsi)
                yield
                for (lo, G, g0, eng, tmps) in ((0, 16, 0, "dve", tv), (1536, 9, 16, "dve", tv), (2176, 9, 25, "pool", tp)):
                    X = proj[:, lo:lo + G * 64].rearrange("p (g h d) -> p g h d", h=2, d=32)
                    O = rq[:, g0:g0 + G, :].rearrange("p g (h d) -> p g h d", h=2)
                    cb = cos_t[:, t, :].unsqueeze(1).to_broadcast([128, G, 32])
                    sb_ = sin_t[:, t, :].unsqueeze(1).to_broadcast([128, G, 32])
                    t1 = tmps[0][:, 0:G, :]; t2 = tmps[1][:, 0:G, :]
                    rr = [proj, cos_t, sin_t]
                    op(eng, lambda e, X=X, cb=cb, t1=t1: e.tensor_tensor(out=t1, in0=X[:, :, 0, :], in1=cb, op=ALU.mult), rr, [tmps[0]])
                    op(eng, lambda e, X=X, sb_=sb_, t2=t2: e.tensor_tensor(out=t2, in0=X[:, :, 1, :], in1=sb_, op=ALU.mult), rr, [tmps[1]])
                    op(eng, lambda e, O=O, t1=t1, t2=t2: e.tensor_tensor(out=O[:, :, 0, :], in0=t1, in1=t2, op=ALU.subtract), [tmps[0], tmps[1]], [rq])
                    op(eng, lambda e, X=X, cb=cb, t1=t1: e.tensor_tensor(out=t1, in0=X[:, :, 1, :], in1=cb, op=ALU.mult), rr, [tmps[0]])
                    op(eng, lambda e, X=X, sb_=sb_, t2=t2: e.tensor_tensor(out=t2, in0=X[:, :, 0, :], in1=sb_, op=ALU.mult), rr, [tmps[1]])
                    op(eng, lambda e, O=O, t1=t1, t2=t2: e.tensor_tensor(out=O[:, :, 1, :], in0=t1, in1=t2, op=ALU.add), [tmps[0], tmps[1]], [rq])
                yield
                qTi = qTs[i2]
                for bi, (g0, g1) in enumerate(((0, 8), (8, 16), (16, 24), (24, 32), (32, 34))):
                    pqb = pq[bi % 2]
                    for g in range(g0, g1):
                        op("pe", lambda e, pqb=pqb, g=g, g0=g0: e.transpose(out=pqb[:, g - g0, :], in_=rq[:, g, :], identity=ident_b[:]), [rq, ident_b], [pqb])
                    op("act", lambda e, pqb=pqb, g0=g0, g1=g1, qTi=qTi: e.activation(out=qTi[:, g0:g1, :], in_=pqb[:, 0:g1 - g0, :], func=AF.Copy), [pqb], [qTi])
                dma(lambda e, qTi=qTi, s=s, t=t: e.dma_start(out=qT_s[s][:, :, t * 128:(t + 1) * 128], in_=qTi[:]), [qTi], [], qTi)

            tl = [(s_, t_) for s_ in range(ns) for t_ in range(NT)]
            gens = {}

            def pull(i):
                if i < 0 or i >= len(tl):
                    return
                if i not in gens:
                    s_, t_ = tl[i]
                    if t_ == 0:
                        rope_tables(rb, s_, cos_tr[s_ % 2], sin_tr[s_ % 2])
                    gens[i] = p1_tile(s_, t_, i)
                next(gens[i], None)

            pull(0); pull(0); pull(1)
            for i in range(len(tl) + 1):
                pull(i)
                pull(i - 1)
                pull(i)
                pull(i + 1)
                pull(i)
                pull(i)
                pull(i)
                pull(i)
                pull(i + 2)
            S.barrier()
            S.flush()

        if 2 in phases:
          with ExitStack() as es2:
            P2 = Alloc(nc, es2, "p2")
            wba = P2.sb("wba", [128, 4, DM], BF16); wbb = P2.sb("wbb", [128, 4, DM], BF16); wo = P2.sb("wo", [128, 8, DM], BF16)
            oaT = P2.sb("oaT", [128, 4, T_SEQ], BF16); obT = P2.sb("obT", [128, 4, T_SEQ], BF16)
            NMT = NT * (NT + 1) // 2
            maskT_all = P2.sb("maskT_all", [128, NMT, 128], BF16)
            moff = [t * (t + 1) // 2 for t in range(NT)]
            for (src, dst) in ((w_ba, wba), (w_bb, wbb), (w_o, wo)):
                sv_ = src.rearrange("(j p) n -> p j n", p=128)
                for j0 in range(0, sv_.shape[1], 2):
                    dma(lambda e, sv_=sv_, dst=dst, j0=j0: e.dma_start(out=dst[:, j0:j0 + 2, :], in_=sv_[:, j0:j0 + 2, :]), [], [dst], dst, eng="pool")
            for s in range(ns):
                with ExitStack() as esa:
                  if 'A' in stages:
                      A = Alloc(nc, esa, "p2a%d" % s)
                      dvs = A.sb("dvs", [128, NT, 512], BF16)
                      dkq = [A.sb("dkq%d" % i, [64, 2, T_SEQ], BF16) for i in range(2)]
                      PT = [A.sb("PT%d" % i, [128, 512], BF16) for i in range(3)]
                      o0n = A.sb("o0n", [128, T_SEQ], F32)
                      rz = A.sb("rz", [128, 512], F32); o1n = A.sb("o1n", [128, 512], F32); dd = A.sb("dd", [128, 512], F32)
                      dsq = A.sb("dsq", [128, 512], BF16); rstd = A.sb("rstd", [128, 512], F32)
                      ps_s = [A.ps("ps_s%d" % i, [128, 512], F32) for i in range(2)]
                      ps_o = [A.ps("ps_o%d" % i, [128, 512], F32) for i in range(2)]; ps_z = [A.ps("ps_z%d" % i, [128, 512], F32) for i in range(2)]
                      ikT = A.sb("ikT", [64, T_SEQ], BF16); iws = A.sb("iws", [128, NT, 8], F32)
                      iqT = [A.sb("iqT%d" % i, [64, 8, 128], BF16) for i in range(2)]
                      score2 = [A.sb("score%d" % i, [128, T_SEQ], F32) for i in range(2)]
                      rl = [A.sb("rl%d" % i, [128, 512], F32) for i in range(2)]
                      blo = [A.sb("blo%d" % i, [128, 1], F32) for i in range(2)]; bd0 = [A.sb("bd0%d" % i, [128, 1], F32) for i in range(2)]; bmid = [A.sb("bmid%d" % i, [128, 1], F32) for i in range(2)]
                      bcnt = [A.sb("bcnt%d" % i, [128, 1], F32) for i in range(2)]; bg = [A.sb("bg%d" % i, [128, 1], F32) for i in range(2)]; bjunk = A.sb("bjunk", [128, T_SEQ], BF16)
                      maskb = A.sb("maskb", [128, T_SEQ], BF16)
                      ps_i = A.ps("ps_i", [128, 512], F32); ps_m = A.ps("ps_m", [128, 8, 128], BF16)
                      dma(lambda e: e.dma_start(out=dvs[:], in_=dv_s[s].rearrange("(t p) c -> p t c", p=128)[:, :, 0:512]), [], [dvs], dvs)
                      dma(lambda e: e.dma_start(out=ikT[:], in_=qT_s[s][:, 33, :]), [], [ikT], ikT)
                      dma(lambda e: e.dma_start(out=iws[:], in_=iw_s[s].rearrange("(t p) c -> p t c", p=128)), [], [iws], iws)

                      def b_part1_gen():
                          ii = 0
                          for t0 in range(0, NT, 2):
                              pair = (t0, t0 + 1)
                              for pi, t in enumerate(pair):
                                  iq_ = iqT[t % 2]; sc_ = score2[pi]
                                  tsl = slice(t * 128, (t + 1) * 128)
                                  dma(lambda e, iq_=iq_, tsl=tsl: e.dma_start(out=iq_[:], in_=qT_s[s][:, 25:33, tsl]), [], [iq_], iq_)
                                  nk = (t + 1) * 128
                                  for kc in range((nk + 511) // 512):
                                      w = min(512, nk - kc * 512)
                                      ksl = slice(kc * 512, kc * 512 + w)
                                      for hh in range(8):
                                          rli = rl[ii % 2]; ii += 1
                                          op("pe", lambda e, iq_=iq_, hh=hh, ksl=ksl, w=w: e.matmul(ps_i[:, 0:w], lhsT=iq_[:, hh, :], rhs=ikT[:, ksl], start=True, stop=True), [iq_, ikT], [ps_i])
                                          op("act", lambda e, rli=rli, w=w: e.activation(out=rli[:, 0:w], in_=ps_i[:, 0:w], func=AF.Relu), [ps_i], [rli])
                                          if hh == 0:
                                              op("dve", lambda e, rli=rli, ksl=ksl, w=w, hh=hh, t=t, sc_=sc_: e.tensor_scalar(out=sc_[:, ksl], in0=rli[:, 0:w], scalar1=iws[:, t, hh:hh + 1], scalar2=None, op0=ALU.mult), [rli, iws], [sc_])
                                          else:
                                              op("dve", lambda e, rli=rli, ksl=ksl, w=w, hh=hh, t=t, sc_=sc_: e.scalar_tensor_tensor(out=sc_[:, ksl], in0=rli[:, 0:w], scalar=iws[:, t, hh:hh + 1], in1=sc_[:, ksl], op0=ALU.mult, op1=ALU.add), [rli, iws, sc_], [sc_])
                                          yield (w / 960.0 + 0.15, 0.4)
                                  op("dve", lambda e, t=t, sc_=sc_: e.memset(sc_[0:64, t * 128 + 64:(t + 1) * 128], NEG), [], [sc_])
                              if t0 >= 2:
                                  for pi, t in enumerate(pair):
                                      nk = (t + 1) * 128; sc_ = score2[pi]; lo_ = blo[pi]; d0_ = bd0[pi]
                                      op("dve", lambda e, nk=nk, sc_=sc_, lo_=lo_: e.tensor_reduce(out=lo_[:], in_=sc_[:, 0:nk - 64], axis=AX.X, op=ALU.min), [sc_], [lo_])
                                      op("dve", lambda e, nk=nk, sc_=sc_, d0_=d0_: e.tensor_reduce(out=d0_[:], in_=sc_[:, 0:nk], axis=AX.X, op=ALU.max), [sc_], [d0_])
                                  for pi in range(2):
                                      lo_ = blo[pi]; d0_ = bd0[pi]
                                      op("dve", lambda e, lo_=lo_, d0_=d0_: e.tensor_tensor(out=d0_[:], in0=d0_[:], in1=lo_[:], op=ALU.subtract), [d0_, lo_], [d0_])
                                  for k in range(24):
                                      ck = 2.0 ** (-(k + 1))
                                      for pi in range(2):
                                          lo_ = blo[pi]; d0_ = bd0[pi]; mid_ = bmid[pi]
                                          op("dve", lambda e, ck=ck, lo_=lo_, d0_=d0_, mid_=mid_: e.scalar_tensor_tensor(out=mid_[:], in0=d0_[:], scalar=ck, in1=lo_[:], op0=ALU.mult, op1=ALU.add), [d0_, lo_], [mid_])
                                      for pi, t in enumerate(pair):
                                          nk = (t + 1) * 128; sc_ = score2[pi]; mid_ = bmid[pi]; cnt_ = bcnt[pi]
                                          op("dve", lambda e, nk=nk, sc_=sc_, mid_=mid_, cnt_=cnt_: e.tensor_scalar(out=bjunk[:, 0:nk], in0=sc_[:, 0:nk], scalar1=mid_[:, 0:1], scalar2=None, op0=ALU.is_ge, op1=ALU.add, accum_out=cnt_[:, 0:1]), [sc_, mid_], [cnt_])
                                      for pi in range(2):
                                          cnt_ = bcnt[pi]; g_ = bg[pi]
                                          op("dve", lambda e, ck=ck, cnt_=cnt_, g_=g_: e.tensor_scalar(out=g_[:], in0=cnt_[:], scalar1=255.5, scalar2=ck, op0=ALU.is_ge, op1=ALU.mult), [cnt_], [g_])
                                      for pi in range(2):
                                          lo_ = blo[pi]; d0_ = bd0[pi]; g_ = bg[pi]
                                          op("dve", lambda e, lo_=lo_, d0_=d0_, g_=g_: e.scalar_tensor_tensor(out=lo_[:], in0=g_[:], scalar=d0_[:, 0:1], in1=lo_[:], op0=ALU.mult, op1=ALU.add), [g_, d0_, lo_], [lo_])
                                      yield (((pair[0] + 1) * 128 + (pair[1] + 1) * 128) / 960.0 + 1.2, 0.0)
                              for pi, t in enumerate(pair):
                                  nk = (t + 1) * 128; sc_ = score2[pi]; lo_ = blo[pi]
                                  if t0 >= 2:
                                      op("dve", lambda e, nk=nk, sc_=sc_, lo_=lo_: e.tensor_scalar(out=maskb[:, 0:nk], in0=sc_[:, 0:nk], scalar1=lo_[:, 0:1], scalar2=None, op0=ALU.is_ge), [sc_, lo_], [maskb])
                                  else:
                                      op("dve", lambda e, nk=nk, sc_=sc_: e.tensor_scalar(out=maskb[:, 0:nk], in0=sc_[:, 0:nk], scalar1=-1.0e29, scalar2=None, op0=ALU.is_ge), [sc_], [maskb])
                                  yield (nk / 960.0 + 0.2, 0.0)
                                  for kb in range((t + 8) // 8):
                                      k1 = min(t + 1, kb * 8 + 8)
                                      for kt in range(kb * 8, k1):
                                          op("pe", lambda e, kt=kt, kb=kb: e.transpose(out=ps_m[:, kt - kb * 8, :], in_=maskb[:, kt * 128:(kt + 1) * 128], identity=ident_b[:]), [maskb, ident_b], [ps_m])
                                      op("act", lambda e, kb=kb, k1=k1, t=t: e.activation(out=maskT_all[:, moff[t] + kb * 8:moff[t] + k1, :], in_=ps_m[:, 0:k1 - kb * 8, :], func=AF.Copy), [ps_m], [maskT_all])
                                      yield (0.0, 0.1 * (k1 - kb * 8))

                      its = []
                      for h in range(4):
                          for m in range(2):
                              for G in range(4):
                                  for kt in range(4 * G + 4):
                                      its.append((h, m, G, kt, 4 * G + 4))

                      def a_loads(g):
                          dk_ = dkq[g % 2]
                          dma(lambda e, dk_=dk_, g=g: e.dma_start(out=dk_[:, 0, :], in_=qT_s[s][:, g, :]), [], [dk_], dk_)
                          dma(lambda e, dk_=dk_, g=g: e.dma_start(out=dk_[:, 1, :], in_=qT_s[s][:, 8 + g, :]), [], [dk_], dk_)

                      def a_stage1(i):
                          h, m, G, kt, nkt = its[i]
                          dk_ = dkq[(2 * h + m) % 2]; pss = ps_s[acnt["s"] % 2]; acnt["s"] += 1; pt = PT[i % 3]
                          op("pe", lambda e: e.matmul(pss[:], lhsT=dk_[:, 1, kt * 128:(kt + 1) * 128], rhs=dk_[:, 0, G * 512:(G + 1) * 512], start=True, stop=True), [dk_], [pss])
                          op("act", lambda e: e.activation(out=pt[:], in_=pss[:], func=AF.Exp, scale=0.125), [pss], [pt])
                          if kt >= 4 * G:
                              j = kt - 4 * G
                              if j > 0:
                                  op("pool", lambda e: e.memset(pt[:, 0:j * 128], 0.0), [], [pt])
                              op("pool", lambda e: e.memset(pt[64:128, j * 128:j * 128 + 64], 0.0), [], [pt])

                      def a_stage2(i):
                          h, m, G, kt, nkt = its[i]
                          pt = PT[i % 3]
                          grp = (h * 2 + m) * 4 + G
                          pso_ = ps_o[grp % 2]; psz_ = ps_z[grp % 2]
                          op("pe", lambda e: e.matmul(pso_[:], lhsT=dvs[:, kt, h * 128:(h + 1) * 128], rhs=pt[:], start=(kt == 0), stop=(kt == nkt - 1)), [dvs, pt], [pso_])
                          op("pe", lambda e: e.matmul(psz_[:], lhsT=ones_b[:], rhs=pt[:], start=(kt == 0), stop=(kt == nkt - 1)), [ones_b, pt], [psz_])
                          if kt != nkt - 1:
                              return
                          op("dve", lambda e: e.reciprocal(out=rz[:], in_=psz_[:]), [psz_], [rz])
                          gsl = slice(G * 512, (G + 1) * 512)
                          if m == 0:
                              op("dve", lambda e: e.tensor_tensor(out=o0n[:, gsl], in0=pso_[:], in1=rz[:], op=ALU.mult), [pso_, rz], [o0n])
                          else:
                              op("dve", lambda e: e.tensor_tensor(out=o1n[:], in0=pso_[:], in1=rz[:], op=ALU.mult), [pso_, rz], [o1n])
                              op("dve", lambda e: e.scalar_tensor_tensor(out=dd[:], in0=o1n[:], scalar=neglam[:, 0:1], in1=o0n[:, gsl], op0=ALU.mult, op1=ALU.add), [o1n, neglam, o0n], [dd])
                              op("act", lambda e: e.activation(out=dsq[:], in_=dd[:], func=AF.Square), [dd], [dsq])
                              ps_q = ps_s[acnt["s"] % 2]; acnt["s"] += 1
                              op("pe", lambda e: e.matmul(ps_q[:], lhsT=ones_b[:], rhs=dsq[:], start=True, stop=True), [ones_b, dsq], [ps_q])
                              op("dve", lambda e: e.tensor_scalar(out=rstd[:], in0=ps_q[:], scalar1=1.0 / 128, scalar2=EPS, op0=ALU.mult, op1=ALU.add), [ps_q], [rstd])
                              op("act", lambda e: e.activation(out=rstd[:], in_=rstd[:], func=AF.Sqrt), [rstd], [rstd])
                              op("dve", lambda e: e.reciprocal(out=rstd[:], in_=rstd[:]), [rstd], [rstd])
                              op("dve", lambda e: e.tensor_tensor(out=dd[:], in0=dd[:], in1=rstd[:], op=ALU.mult), [dd, rstd], [dd])
                              op("dve", lambda e: e.tensor_scalar(out=oaT[:, h, gsl], in0=dd[:], scalar1=ogcol[:, 0:1], scalar2=None, op0=ALU.mult), [dd, ogcol], [oaT])

                      acnt = {"s": 0}
                      clk = {"pe": 0.0, "dve": 0.0}
                      bgen = b_part1_gen()
                      a_loads(0)
                      a_stage1(0)
                      for i in range(len(its)):
                          h, m, G, kt, nkt = its[i]
                          if G == 0 and kt == 0 and 2 * h + m + 1 < 8:
                              a_loads(2 * h + m + 1)
                          if i + 1 < len(its):
                              a_stage1(i + 1)
                          a_stage2(i)
                          clk["pe"] += 1.25
                          while clk["dve"] < clk["pe"]:
                              c = next(bgen, None)
                              if c is None:
                                  break
                              clk["dve"] += c[0]; clk["pe"] += c[1]
                      for _ in bgen:
                          pass
                      S.barrier(); S.flush()

                with ExitStack() as esb:
                  if 'B' in stages:
                      A = Alloc(nc, esb, "p2b%d" % s)
                      skT = A.sb("skT", [64, T_SEQ], BF16)
                      svd = A.sb("svd", [128, NT, 128], BF16)
                      sqT = [A.sb("sqT%d" % i, [64, 8, 128], BF16) for i in range(3)]
                      PT = [A.sb("PTb%d" % i, [128, 4, 128], BF16) for i in range(4)]
                      oS = [A.sb("oS%d" % i, [128, 4, 128], F32) for i in range(2)]; zS = [A.sb("zS%d" % i, [128, 4, 128], F32) for i in range(2)]
                      ps_s = [A.ps("ps_sb%d" % i, [128, 4, 128], F32) for i in range(3)]
                      ps_o = [A.ps("ps_ob%d" % i, [128, 4, 128], F32) for i in range(2)]; ps_z = [A.ps("ps_zb%d" % i, [128, 4, 128], F32) for i in range(2)]
                      dma(lambda e: e.dma_start(out=skT[:], in_=qT_s[s][:, 24, :]), [], [skT], skT)
                      svv = dv_s[s].rearrange("(t p) c -> p t c", p=128)
                      dma(lambda e: e.dma_start(out=svd[:, :, 0:64], in_=svv[:, :, 512:576]), [], [svd], svd)
                      dma(lambda e: e.dma_start(out=svd[:, :, 64:128], in_=svv[:, :, 512:576]), [], [svd], svd)
                      its = [(t, half, kt) for t in range(NT) for half in range(2) for kt in range(t + 1)]

                      def sq_load(t):
                          sq_ = sqT[t % 3]
                          dma(lambda e: e.dma_start(out=sq_[:], in_=qT_s[s][:, 16:24, t * 128:(t + 1) * 128]), [], [sq_], sq_)

                      def st1(i):
                          t, half, kt = its[i]
                          sq_ = sqT[t % 3]
                          pss = ps_s[i % 3]; pt = PT[i % 4]
                          op("pe", lambda e: e.matmul(pss[:], lhsT=skT[:, kt * 128:(kt + 1) * 128], rhs=sq_[:, 4 * half:4 * half + 4, :], start=True, stop=True), [skT, sq_], [pss])
                          op("act", lambda e: e.activation(out=pt[:], in_=pss[:], func=AF.Exp, scale=0.125), [pss], [pt])
                          op("dve", lambda e: e.tensor_tensor(out=pt[:], in0=pt[:], in1=maskT_all[:, moff[t] + kt, :].unsqueeze(1).to_broadcast([128, 4, 128]), op=ALU.mult), [pt, maskT_all], [pt])

                      def st2(i):
                          t, half, kt = its[i]
                          pt = PT[i % 4]
                          grp = 2 * t + half
                          pso_ = ps_o[grp % 2]; psz_ = ps_z[grp % 2]
                          tsl = slice(t * 128, (t + 1) * 128)
                          op("pe", lambda e: e.matmul(pso_[:], lhsT=svd[:, kt, :], rhs=pt[:], start=(kt == 0), stop=(kt == t)), [svd, pt], [pso_])
                          op("pe", lambda e: e.matmul(psz_[:], lhsT=ones_b[:], rhs=pt[:], start=(kt == 0), stop=(kt == t)), [ones_b, pt], [psz_])
                          if kt != t:
                              return
                          oS_ = oS[grp % 2]; zS_ = zS[grp % 2]
                          op("act", lambda e: e.activation(out=oS_[:], in_=pso_[:], func=AF.Copy), [pso_], [oS_])
                          op("dve", lambda e: e.reciprocal(out=zS_[:], in_=psz_[:]), [psz_], [zS_])
                          for par in range(2):
                              psl = slice(par * 64, par * 64 + 64)
                              op("dve", lambda e, psl=psl, par=par: e.tensor_tensor(out=obT[psl, 2 * half:2 * half + 2, tsl], in0=oS_[psl, par::2, :], in1=zS_[psl, par::2, :], op=ALU.mult), [oS_, zS_], [obT])

                      sq_load(0); sq_load(1)
                      st1(0); st1(1)
                      for i in range(len(its)):
                          t, half, kt = its[i]
                          if half == 0 and kt == 0 and t + 2 < NT:
                              sq_load(t + 2)
                          if i + 2 < len(its):
                              st1(i + 2)
                          st2(i)
                      S.barrier(); S.flush()

                with ExitStack() as esc:
                  if 'C' in stages:
                      A = Alloc(nc, esc, "p2c%d" % s)
                      gT = [A.sb("gT%d" % i, [128, 2, 512], BF16) for i in range(2)]
                      t1 = A.sb("t1", [128, 512], F32); t2 = A.sb("t2", [128, 512], F32)
                      mT = A.sb("mT", [128, 8, 512], BF16)
                      xt = [A.sb("xtc%d" % i, [128, DM], F32) for i in range(2)]
                      g1b = A.sb("g1b", [128, DM], F32); tmp3 = A.sb("tmp3", [128, DM], F32)
                      ps_a = A.ps("ps_a", [128, 512], F32); ps_b = A.ps("ps_b", [128, 512], F32)
                      ps_x = [A.ps("ps_x%d" % i, [128, 512], F32) for i in range(2)]
                      dma(lambda e: e.dma_start(out=g1b[:], in_=rows_d[s, 2:3, :].partition_broadcast(128)), [], [g1b], g1b)
                      if dbg:
                          dma(lambda e: e.dma_start(out=dbg_oa[s], in_=oaT[:]), [oaT], [], oaT)
                          dma(lambda e: e.dma_start(out=dbg_ob[s], in_=obT[:]), [obT], [], obT)
                          dma(lambda e: e.dma_start(out=dbg_mk[s], in_=maskT_all[:]), [maskT_all], [], maskT_all)
                      ic = 0
                      for G in range(4):
                          gsl = slice(G * 512, (G + 1) * 512)
                          for c in range(8):
                              gTi = gT[ic % 2]; ic += 1
                              dma(lambda e, gTi=gTi, c=c, gsl=gsl: e.dma_start(out=gTi[:], in_=gT_s[s][:, c::8, gsl]), [], [gTi], gTi)
                              for j in range(4):
                                  op("pe", lambda e, j=j, c=c, gsl=gsl: e.matmul(ps_a[:], lhsT=wba[:, j, c * 128:(c + 1) * 128], rhs=oaT[:, j, gsl], start=(j == 0), stop=(j == 3)), [wba, oaT], [ps_a])
                              for j in range(4):
                                  op("pe", lambda e, j=j, c=c, gsl=gsl: e.matmul(ps_b[:], lhsT=wbb[:, j, c * 128:(c + 1) * 128], rhs=obT[:, j, gsl], start=(j == 0), stop=(j == 3)), [wbb, obT], [ps_b])
                              op("dve", lambda e, gTi=gTi: e.tensor_tensor(out=t1[:], in0=ps_a[:], in1=gTi[:, 0, :], op=ALU.mult), [ps_a, gTi], [t1])
                              op("dve", lambda e, gTi=gTi: e.tensor_tensor(out=t2[:], in0=ps_b[:], in1=gTi[:, 1, :], op=ALU.mult), [ps_b, gTi], [t2])
                              op("pool", lambda e, c=c: e.tensor_tensor(out=mT[:, c, :], in0=t1[:], in1=t2[:], op=ALU.add), [t1, t2], [mT])
                          for q in range(4):
                              tt = 4 * G + q
                              r0 = s * T_SEQ + tt * 128
                              xti = xt[tt % 2]
                              dma(lambda e, xti=xti, r0=r0: e.dma_start(out=xti[:], in_=x[r0:r0 + 128, :]), [], [xti], xti)
                              for half in range(2):
                                  psx = ps_x[half]
                                  for c in range(8):
                                      op("pe", lambda e, psx=psx, c=c, q=q, half=half: e.matmul(psx[:], lhsT=mT[:, c, q * 128:(q + 1) * 128], rhs=wo[:, c, half * 512:(half + 1) * 512], start=(c == 0), stop=(c == 7)), [mT, wo], [psx])
                                  op("dve", lambda e, psx=psx, half=half: e.tensor_tensor(out=tmp3[:, half * 512:(half + 1) * 512], in0=psx[:], in1=g1b[:, half * 512:(half + 1) * 512], op=ALU.mult), [psx, g1b], [tmp3])
                              op("pool", lambda e, xti=xti: e.tensor_tensor(out=xti[:], in0=xti[:], in1=tmp3[:], op=ALU.add), [xti, tmp3], [xti])
                              dma(lambda e, xti=xti, r0=r0: e.dma_start(out=out[r0:r0 + 128, :], in_=xti[:]), [xti], [], xti)
                      S.barrier(); S.flush()

        if 3 in phases:
          with ExitStack() as es3:
            A = Alloc(nc, es3, "p3")
            wq_bf = A.sb("wq_bf", [128, 8, 2048], BF16); skb = A.sb("skb", [128, 16, 128], BF16)
            w_q_v = w_q.rearrange("(kc p) n -> p kc n", p=128)
            for j in range(4):
                dma(lambda e, j=j: e.dma_start(out=wq_bf[:, :, j * 512:(j + 1) * 512], in_=w_q_v[:, :, j * 512:(j + 1) * 512]), [], [wq_bf], wq_bf, eng="pool")
            dma(lambda e: e.dma_start(out=skb[:], in_=subkT), [], [skb], skb, eng="pool")
            xt = [A.sb("xt%d" % i, [128, DM], F32) for i in range(2)]
            junk = A.sb("junk", [128, DM], BF16); xn = A.sb("xn", [128, DM], BF16)
            ssq = [A.sb("ssq%d" % i, [128, 1], F32) for i in range(2)]
            h2T = A.sb("h2T", [128, 8, 128], BF16)
            h2 = [A.sb("h2_%d" % i, [128, 8, 128], BF16) for i in range(2)]
            tmpf = A.sb("tmpf", [128, DM], F32)
            G2b = [A.sb("G2b%d" % i, [128, DM], F32) for i in range(2)]
            qT = A.sb("qT", [128, 16, 128], BF16)
            sc_sb = A.sb("sc_sb", [128, 16, 128], F32)
            v16 = A.sb("v16", [128, 16, 16], F32); i16 = A.sb("i16", [128, 16, 16], U32); i16f = A.sb("i16f", [128, 16, 16], F32)
            cand = A.sb("cand", [128, 8, 256], F32)
            sc = A.sb("sc", [128, 8, 16], F32); posu = A.sb("posu", [128, 8, 16], U32)
            au = A.sb("au", [128, 8, 16], U32); bu = A.sb("bu", [128, 8, 16], U32)
            af = A.sb("af", [128, 8, 16], F32); bf_ = A.sb("bf_", [128, 8, 16], F32)
            eq = A.sb("eq", [128, 8, 16, 16], F32)
            e1 = A.sb("e1", [128, 8, 16], F32); e2 = A.sb("e2", [128, 8, 16], F32)
            eidx = [A.sb("eidx%d" % i, [128, 128], U32) for i in range(2)]
            ex = A.sb("ex", [128, 8, 16], F32); esum = A.sb("esum", [128, 8], F32)
            gg = [A.sb("gg%d" % i, [128, 128], F32) for i in range(2)]
            av = A.sb("av", [128, 128], F32)
            wg8 = [A.sb("wg8_%d" % i, [128, 8], F32) for i in range(2)]
            NR = 24
            uvg = [A.sb("uvg%d" % i, [128, 2 * DM], BF16) for i in range(NR)]
            djunk = A.sb("djunk", [128, DM], BF16)
            dg = [A.sb("dg%d" % i, [128, 128], BF16) for i in range(4)]
            pT = A.ps("pT3", [128, 8, 128], BF16)
            psq = [A.ps("psq%d" % i, [128, 4, 128], F32) for i in range(2)]
            pso = [[A.ps("pso%d_%d" % (i, j), [128, 512], F32) for j in range(2)] for i in range(2)]
            tiles = [(s, t) for s in range(ns) for t in range(NT)]

            def routing_pieces(gi):
                s, t = tiles[gi]
                i2 = gi % 2
                r0 = s * T_SEQ + t * 128
                xti = xt[i2]; ssi = ssq[i2]; eix = eidx[i2]; h2i = h2[i2]; ggi = gg[i2]
                P = []
                M = []

                def R(eng, fn, reads=(), writes=()):
                    M.append((eng, fn, reads, writes, None))

                def RD(fn, reads=(), writes=(), primary=None):
                    M.append(("dma", fn, reads, writes, primary))

                def rms_R(ssq_ap, n, res, nm):
                    R("dve", lambda e: e.tensor_scalar(out=ssq_ap, in0=ssq_ap, scalar1=1.0 / n, scalar2=EPS, op0=ALU.mult, op1=ALU.add), [res], [res])
                    R("act", lambda e: e.activation(out=ssq_ap, in_=ssq_ap, func=AF.Sqrt), [res], [res])
                    R("dve", lambda e: e.reciprocal(out=ssq_ap, in_=ssq_ap), [res], [res])

                def p_load():
                    if t == 0:
                        gb = G2b[s % 2]
                        RD(lambda e: e.dma_start(out=gb[:], in_=rows_d[s, 5:6, :].partition_broadcast(128)), [], [gb], gb)
                    RD(lambda e: e.dma_start(out=xti[:], in_=out[r0:r0 + 128, :]), [], [xti], xti)
                    R("act", lambda e: e.activation(out=junk[:], in_=xti[:], func=AF.Square, accum_out=ssi[:, 0:1]), [xti], [junk, ssi])
                    rms_R(ssi[:, 0:1], float(DM), ssi, "x")
                    R("act", lambda e: e.activation(out=xn[:], in_=xti[:], func=AF.Copy, scale=ssi[:, 0:1]), [xti, ssi], [xn])
                    for kc in range(8):
                        R("pe", lambda e, kc=kc: e.transpose(out=pT[:, kc, :], in_=xn[:, kc * 128:(kc + 1) * 128], identity=ident_b[:]), [xn, ident_b], [pT])
                    for kc in range(8):
                        R("act", lambda e, kc=kc: e.activation(out=h2T[:, kc, :], in_=pT[:, kc, :], func=AF.Identity, scale=cols[:, 2, kc, s:s + 1], bias=cols[:, 3, kc, s:s + 1]), [pT, cols], [h2T])
                    for kc in range(8):
                        R("pe", lambda e, kc=kc: e.transpose(out=pT[:, kc, :], in_=h2T[:, kc, :], identity=ident_b[:]), [h2T, ident_b], [pT])
                    R("act", lambda e: e.activation(out=h2i[:], in_=pT[:], func=AF.Copy), [pT], [h2i])
                P.append(p_load)

                def p_q(r0_, r1_):
                    def f():
                        for r in range(r0_, r1_):
                            pq_ = psq[r % 2]
                            for j in range(4):
                                hp = r * 4 + j
                                for kc in range(8):
                                    R("pe", lambda e, pq_=pq_, j=j, hp=hp, kc=kc: e.matmul(pq_[:, j, :], lhsT=wq_bf[:, kc, hp * 128:(hp + 1) * 128], rhs=h2T[:, kc, :], start=(kc == 0), stop=(kc == 7)), [wq_bf, h2T], [pq_])
                            R("act", lambda e, pq_=pq_, r=r: e.activation(out=qT[:, r * 4:(r + 1) * 4, :], in_=pq_[:], func=AF.Copy), [pq_], [qT])
                    return f
                P.append(p_q(0, 2)); P.append(p_q(2, 4))

                def p_sc():
                    for r in range(4):
                        pq_ = psq[r % 2]
                        for j in range(4):
                            hp = r * 4 + j
                            R("pe", lambda e, pq_=pq_, j=j, hp=hp: e.matmul(pq_[:, j, :], lhsT=qT[:, hp, :], rhs=skb[:, hp, :], start=True, stop=True), [qT, skb], [pq_])
                        R("act", lambda e, pq_=pq_, r=r: e.activation(out=sc_sb[:, r * 4:(r + 1) * 4, :], in_=pq_[:], func=AF.Copy), [pq_], [sc_sb])
                P.append(p_sc)

                def p_top(h0, h1):
                    def f():
                        for hp in range(h0, h1):
                            R("dve", lambda e, hp=hp: e.max(out=v16[:, hp, 0:8], in_=sc_sb[:, hp, :]), [sc_sb], [v16])
                            R("dve", lambda e, hp=hp: e.max_index(out=i16[:, hp, 0:8], in_max=v16[:, hp, 0:8], in_values=sc_sb[:, hp, :]), [sc_sb, v16], [i16])
                            R("dve", lambda e, hp=hp: e.match_replace(out=sc_sb[:, hp, :], in_to_replace=v16[:, hp, 0:8], in_values=sc_sb[:, hp, :], imm_value=NEG), [sc_sb, v16], [sc_sb])
                            R("dve", lambda e, hp=hp: e.max(out=v16[:, hp, 8:16], in_=sc_sb[:, hp, :]), [sc_sb], [v16])
                            R("dve", lambda e, hp=hp: e.max_index(out=i16[:, hp, 8:16], in_max=v16[:, hp, 8:16], in_values=sc_sb[:, hp, :]), [sc_sb, v16], [i16])
                    return f
                for q in range(4):
                    P.append(p_top(q * 4, q * 4 + 4))

                def p_cand():
                    v4 = v16[:, :, :].rearrange("p (h a) k -> p h a k", a=2)
                    c4 = cand[:, :, :].rearrange("p h (a b) -> p h a b", b=16)
                    R("dve", lambda e: e.tensor_tensor(out=c4, in0=v4[:, :, 0, :].unsqueeze(3).to_broadcast([128, 8, 16, 16]), in1=v4[:, :, 1, :].unsqueeze(2).to_broadcast([128, 8, 16, 16]), op=ALU.add), [v16], [cand])
                P.append(p_cand)

                def p_top2(h0, h1):
                    def f():
                        for h in range(h0, h1):
                            R("dve", lambda e, h=h: e.max(out=sc[:, h, 0:8], in_=cand[:, h, :]), [cand], [sc])
                            R("dve", lambda e, h=h: e.max_index(out=posu[:, h, 0:8], in_max=sc[:, h, 0:8], in_values=cand[:, h, :]), [cand, sc], [posu])
                            R("dve", lambda e, h=h: e.match_replace(out=cand[:, h, :], in_to_replace=sc[:, h, 0:8], in_values=cand[:, h, :], imm_value=NEG), [cand, sc], [cand])
                            R("dve", lambda e, h=h: e.max(out=sc[:, h, 8:16], in_=cand[:, h, :]), [cand], [sc])
                            R("dve", lambda e, h=h: e.max_index(out=posu[:, h, 8:16], in_max=sc[:, h, 8:16], in_values=cand[:, h, :]), [cand, sc], [posu])
                    return f
                for q in range(4):
                    P.append(p_top2(q * 2, q * 2 + 2))

                def p_idx():
                    R("dve", lambda e: e.tensor_single_scalar(out=au[:], in_=posu[:], scalar=4, op=ALU.logical_shift_right), [posu], [au])
                    R("dve", lambda e: e.tensor_single_scalar(out=bu[:], in_=posu[:], scalar=15, op=ALU.bitwise_and), [posu], [bu])
                    R("dve", lambda e: e.tensor_copy(out=af[:], in_=au[:]), [au], [af])
                    R("dve", lambda e: e.tensor_copy(out=bf_[:], in_=bu[:]), [bu], [bf_])
                    R("dve", lambda e: e.tensor_copy(out=i16f[:], in_=i16[:]), [i16], [i16f])
                    i4 = i16f[:, :, :].rearrange("p (h a) k -> p h a k", a=2)
                    io4 = iota16[:, :].unsqueeze(1).unsqueeze(1).to_broadcast([128, 8, 16, 16])
                    for (sel, which, dst) in ((af, 0, e1), (bf_, 1, e2)):
                        R("dve", lambda e, sel=sel: e.tensor_tensor(out=eq[:], in0=io4, in1=sel[:, :, :].unsqueeze(3).to_broadcast([128, 8, 16, 16]), op=ALU.is_equal), [iota16, sel], [eq])
                        R("dve", lambda e, which=which: e.tensor_tensor(out=eq[:], in0=eq[:], in1=i4[:, :, which, :].unsqueeze(2).to_broadcast([128, 8, 16, 16]), op=ALU.mult), [eq, i16f], [eq])
                        R("dve", lambda e, dst=dst: e.tensor_reduce(out=dst[:], in_=eq[:], axis=AX.X, op=ALU.add), [eq], [dst])
                    R("dve", lambda e: e.scalar_tensor_tensor(out=e1[:], in0=e1[:], scalar=128.0, in1=e2[:], op0=ALU.mult, op1=ALU.add), [e1, e2], [e1])
                    R("dve", lambda e: e.tensor_copy(out=eix[:], in_=e1[:, :, :].rearrange("p h k -> p (h k)")), [e1], [eix])
                P.append(p_idx)

                def p_soft():
                    R("dve", lambda e: e.tensor_tensor(out=ex[:], in0=sc[:], in1=sc[:, :, 0:1].to_broadcast([128, 8, 16]), op=ALU.subtract), [sc], [ex])
                    R("act", lambda e: e.activation(out=ex[:], in_=ex[:], func=AF.Exp), [ex], [ex])
                    R("dve", lambda e: e.tensor_reduce(out=esum[:], in_=ex[:], axis=AX.X, op=ALU.add), [ex], [esum])
                    R("dve", lambda e: e.reciprocal(out=esum[:], in_=esum[:]), [esum], [esum])
                    R("dve", lambda e: e.tensor_tensor(out=ggi[:, :].rearrange("p (h k) -> p h k", k=16), in0=ex[:], in1=esum[:, :].unsqueeze(2).to_broadcast([128, 8, 16]), op=ALU.mult), [ex, esum], [ggi])
                P.append(p_soft)
                for p in P:
                    p()
                return M

            gcnt = {"g": 0}
            avres = [[Res("avr%d_%d" % (i, j)) for j in range(8)] for i in range(2)]

            def gather_group(gi, grp, nxt):
                s, t = tiles[gi]
                i2 = gi % 2
                eix = eidx[i2]; h2i = h2[i2]; ggi = gg[i2]
                pso_ = pso[gi % 2]
                wg = wg8[grp % 2]
                bufs = []
                for j in range(8):
                    hk = grp * 8 + j
                    u_ = uvg[gcnt["g"] % NR]; gcnt["g"] += 1
                    bufs.append(u_)
                    dma(lambda e, u_=u_, hk=hk: e.indirect_dma_start(out=u_[:], out_offset=None, in_=uv_bf, in_offset=bass.IndirectOffsetOnAxis(ap=eix[:, hk:hk + 1], axis=0)), [eix], [u_], u_, eng="pool")
                    op("dve", lambda e, u_=u_, hk=hk: e.scalar_tensor_tensor(out=djunk[:], in0=u_[:, 0:DM], scalar=1.0, in1=h2i[:, :, :].rearrange("p a b -> p (a b)"), op0=ALU.mult, op1=ALU.mult, accum_out=av[:, hk:hk + 1]), [u_, h2i], [avres[grp % 2][j]])
                    if nxt is not None and nxt.ndve > 0:
                        dots_left = 128 - hk
                        nxt.pull_dve((nxt.ndve + dots_left - 1) // dots_left)
                gs_ = slice(grp * 8, grp * 8 + 8)
                op("act", lambda e: e.activation(out=wg[:], in_=av[:, gs_], func=AF.Gelu), avres[grp % 2], [wg])
                op("dve", lambda e: e.tensor_tensor(out=wg[:], in0=wg[:], in1=ggi[:, gs_], op=ALU.mult), [wg, ggi], [wg])
                for j in range(8):
                    hk = grp * 8 + j
                    u_ = bufs[j]; d_ = dg[hk % 4]
                    op("act", lambda e, d_=d_, j=j: e.activation(out=d_[:], in_=ident_b[:], func=AF.Copy, scale=wg[:, j:j + 1]), [ident_b, wg], [d_])
                    for half in range(2):
                        op("pe", lambda e, d_=d_, u_=u_, half=half, hk=hk: e.matmul(pso_[half][:], lhsT=d_[:], rhs=u_[:, DM + half * 512:DM + (half + 1) * 512], start=(hk == 0), stop=(hk == 127)), [d_, u_], [pso_[half]])

            def final(gi):
                s, t = tiles[gi]
                r0 = s * T_SEQ + t * 128
                xti = xt[gi % 2]; pso_ = pso[gi % 2]; gb = G2b[s % 2]
                for half in range(2):
                    hs = slice(half * 512, (half + 1) * 512)
                    op("dve", lambda e, half=half, hs=hs: e.tensor_tensor(out=tmpf[:, hs], in0=pso_[half][:], in1=gb[:, hs], op=ALU.mult), [pso_[half], gb], [tmpf])
                op("dve", lambda e: e.tensor_tensor(out=xti[:], in0=xti[:], in1=tmpf[:], op=ALU.add), [xti, tmpf], [xti])
                dma(lambda e: e.dma_start(out=out[r0:r0 + 128, :], in_=xti[:]), [xti], [], xti)

            def emit_micro(mo):
                eng, fn, reads, writes, primary = mo
                if eng == "dma":
                    dma(fn, reads, writes, primary)
                else:
                    op(eng, fn, reads, writes)

            class RQ:
                def __init__(self, M):
                    self.M = M; self.i = 0
                    self.ndve = sum(1 for m in M if m[0] == "dve")

                def pull_dve(self, k):
                    while k > 0 and self.i < len(self.M):
                        mo = self.M[self.i]; self.i += 1
                        emit_micro(mo)
                        if mo[0] == "dve":
                            k -= 1; self.ndve -= 1

                def drain(self):
                    while self.i < len(self.M):
                        emit_micro(self.M[self.i]); self.i += 1

            RQ(routing_pieces(0)).drain()
            for gi in range(len(tiles)):
                nxt = RQ(routing_pieces(gi + 1)) if gi + 1 < len(tiles) else None
                for grp in range(16):
                    gather_group(gi, grp, nxt)
                if nxt is not None:
                    nxt.drain()
                final(gi)
            S.barrier(); S.flush()
    return nc


def _core_inputs(inp, b0, ns):
    m = {}
    m["x"] = np.ascontiguousarray(inp["x"][b0:b0 + ns].reshape(ns * T_SEQ, DM))
    m["c"] = np.ascontiguousarray(inp["c"][b0:b0 + ns])
    m["pos"] = np.ascontiguousarray(np.asarray(inp["positions"][b0:b0 + ns]).reshape(ns, NT, 128).transpose(0, 2, 1)).astype(np.int32)
    return m


def kernel(**inputs):
    inp = {k: np.asarray(v) for k, v in inputs.items()}
    n_cores = 8
    ns = inp["x"].shape[0] // n_cores
    shared = {}
    shared["inv"] = (np.float32(10000.0) ** (-(np.arange(0, 64, 2, dtype=np.float32) / np.float32(64)))).astype(np.float32).reshape(1, 32)
    for k in ("w_ada", "w_in", "w_branch_a", "w_branch_b", "w_out", "peer_w_q", "peer_u", "peer_v"):
        shared[k] = np.ascontiguousarray(inp[k][0], dtype=np.float32)
    for k in ("b_ada", "norm1_g", "norm2_g", "diff_q_g", "diff_k_g", "dsa_q_g", "dsa_k_g",
              "diff_lam_q1", "diff_lam_k1", "diff_lam_q2", "diff_lam_k2"):
        shared[k] = np.ascontiguousarray(inp[k][0].reshape(1, -1), dtype=np.float32)
    shared["diff_out_g"] = np.ascontiguousarray(inp["diff_out_g"][0].reshape(128, 1), dtype=np.float32)
    shared["subkT"] = np.ascontiguousarray(inp["peer_sub_keys"][0].reshape(16, 128, 128).transpose(2, 0, 1), dtype=np.float32)
    in_maps = []
    for ci in range(n_cores):
        m = dict(shared)
        m.update(_core_inputs(inp, ci * ns, ns))
        in_maps.append(m)
    nc = build(ns)
    res = run_bass_kernel_spmd(nc, in_maps, core_ids=list(range(n_cores)))
    outs = [np.asarray(r["out"]).reshape(ns, T_SEQ, DM) for r in res.results]
    return np.concatenate(outs, axis=0).astype(np.float32)
```

```python
import numpy as np, math
from contextlib import ExitStack
import concourse.bass as bass
import concourse.mybir as mybir
from concourse.bass_utils import run_bass_kernel_spmd

F32 = mybir.dt.float32
BF16 = mybir.dt.bfloat16
I32 = mybir.dt.int32
U32 = mybir.dt.uint32
ALU = mybir.AluOpType
AF = mybir.ActivationFunctionType
AX = mybir.AxisListType


class Res:
    __slots__ = ("name", "w", "r", "dsem", "dcnt")

    def __init__(self, name):
        self.name = name
        self.w = []
        self.r = []
        self.dsem = None
        self.dcnt = 0


class Sched:
    ENGS = ("pe", "act", "dve", "pool", "sp")

    def __init__(self, nc, es):
        self.nc = nc
        self.es = es
        self.items = {e: [] for e in self.ENGS}
        self.cnt = {e: 0 for e in self.ENGS}
        self.sems = {}
        for e in self.ENGS:
            if e != "sp":
                self.sems[e] = es.enter_context(nc.semaphore("sem_" + e))
        self.known = {e: {} for e in self.ENGS}
        self.dstate = {}
        self.ninst = 0

    def _dstate(self, res):
        st = self.dstate.get(res.name)
        if st is None:
            sem = self.es.enter_context(self.nc.semaphore("dsem_%d_%s" % (len(self.dstate), res.name)))
            st = [sem, 0]
            self.dstate[res.name] = st
            self.sems[("d", res.name)] = sem
        return st

    def _collect(self, eng, reads, writes):
        deps = []
        for r in reads:
            deps += r.w
        for w in writes:
            deps += w.w
            deps += w.r
        out = {}
        for (k, v) in deps:
            if eng == "pe" and k == "pe":
                continue
            if self.known[eng].get(k, 0) >= v:
                continue
            if out.get(k, 0) < v:
                out[k] = v
        for k, v in out.items():
            self.known[eng][k] = v
        return list(out.items())

    def _update(self, dep, reads, writes):
        for r in reads:
            if r not in writes:
                r.r.append(dep)
        for w in writes:
            w.w = [dep]
            w.r = []

    def op(self, eng, fn, reads=(), writes=()):
        reads = list(reads); writes = list(writes)
        waits = self._collect(eng, reads, writes)
        self.cnt[eng] += 1
        dep = (eng, self.cnt[eng])
        self.items[eng].append((waits, fn, (eng, 1)))
        self._update(dep, reads, writes)
        self.ninst += 1

    def dma(self, fn, reads=(), writes=(), primary=None, eng="sp"):
        reads = list(reads); writes = list(writes)
        waits = self._collect(eng, reads, writes)
        st = self._dstate(primary)
        st[1] += 16
        key = ("d", primary.name)
        dep = (key, st[1])
        self.items[eng].append((waits, fn, (key, 16)))
        self._update(dep, reads, writes)
        self.ninst += 1

    def barrier(self):
        allv = []
        for e in self.ENGS:
            if e != "sp" and self.cnt[e] > 0:
                allv.append((e, self.cnt[e]))
        for nm, st in self.dstate.items():
            allv.append((("d", nm), st[1]))
        for e in self.ENGS:
            waits = []
            for (k, v) in allv:
                if self.known[e].get(k, 0) < v:
                    waits.append((k, v))
                    self.known[e][k] = v
            if waits:
                self.items[e].append((waits, None, None))

    def flush(self, name=None):
        nc = self.nc
        items = self.items
        sems = self.sems

        def replay(engname):
            def run(eng):
                for (waits, fn, inc) in items[engname]:
                    for (k, v) in waits:
                        eng.wait_ge(sems[k], v)
                    if fn is not None:
                        inst = fn(eng)
                        inst.then_inc(sems[inc[0]], inc[1])
            return run

        with nc.Block() as block:
            if items["sp"]:
                block.sync(replay("sp"))
            if items["act"]:
                block.scalar(replay("act"))
            if items["dve"]:
                block.vector(replay("dve"))
            if items["pool"]:
                block.gpsimd(replay("pool"))
            if items["pe"]:
                block.tensor(replay("pe"))
        self.items = {e: [] for e in self.ENGS}
NS_FULL = 4
T_SEQ = 2048
NT = 16
DM = 1024
IN_COLS = 4808
NPROJ = 2760
GATE0 = 2760
EPS = 1e-6
LAMBDA_INIT = 0.8 - 0.6 * math.exp(-0.0)
NEG = -1.0e30
TWO_PI = 2.0 * math.pi


class T_:
    __slots__ = ("t", "r")

    def __init__(self, t, name):
        self.t = t
        self.r = Res(name)

    def __getitem__(self, k):
        return self.t[k]


class Alloc:
    def __init__(self, nc, es, prefix):
        self.nc, self.es, self.p = nc, es, prefix
        self.n = 0

    def sb(self, name, shape, dt):
        self.n += 1
        return T_(self.es.enter_context(self.nc.sbuf_tensor("%s_%s_%d" % (self.p, name, self.n), shape, dt)), name)

    def ps(self, name, shape, dt):
        self.n += 1
        return T_(self.es.enter_context(self.nc.psum_tensor("%s_%s_%d" % (self.p, name, self.n), shape, dt)), name)


def _rs(lst):
    return [x.r if isinstance(x, T_) else x for x in lst]


class K:
    def __init__(self, nc, S, ns):
        self.nc, self.S, self.ns = nc, S, ns

    def op(self, eng, fn, reads=(), writes=()):
        self.S.op(eng, fn, _rs(reads), _rs(writes))

    def dma(self, fn, reads=(), writes=(), primary=None, eng="sp"):
        self.S.dma(fn, _rs(reads), _rs(writes), primary.r if isinstance(primary, T_) else primary, eng)

def build(ns, phases=(0, 1, 2, 3), dbg=False, stages="ABC"):
    nc = bass.Bass("TRN2", target_bir_lowering=False)
    NTOK = ns * T_SEQ

    def din(name, shape, dt=F32):
        return nc.dram_tensor(name, shape, dt, kind="ExternalInput").ap()

    def dscr(name, shape, dt):
        return nc.dram_tensor(name, shape, dt, kind=("ExternalOutput" if dbg else "Internal")).ap()

    x = din("x", [NTOK, DM]); c_in = din("c", [ns, DM]); pos = din("pos", [ns, 128, NT], I32); inv = din("inv", [1, 32])
    w_ada = din("w_ada", [DM, 6 * DM]); b_ada = din("b_ada", [1, 6 * DM]); n1g = din("norm1_g", [1, DM]); w_in = din("w_in", [DM, IN_COLS])
    gains_in = [din(n, [1, 64]) for n in ("diff_q_g", "diff_k_g", "dsa_q_g", "dsa_k_g")]
    lam_in = [din(n, [1, 64]) for n in ("diff_lam_q1", "diff_lam_k1", "diff_lam_q2", "diff_lam_k2")]
    og_in = din("diff_out_g", [128, 1])
    w_ba = din("w_branch_a", [512, DM]); w_bb = din("w_branch_b", [512, DM]); w_o = din("w_out", [DM, DM]); n2g = din("norm2_g", [1, DM])
    w_q = din("peer_w_q", [DM, 2048]); subkT = din("subkT", [128, 16, 128]); pu = din("peer_u", [16384, DM]); pv = din("peer_v", [16384, DM])
    out = nc.dram_tensor("out", [NTOK, DM], F32, kind="ExternalOutput").ap()

    uv_bf = dscr("uv_bf", [16384, 2 * DM], BF16)
    rows_d = dscr("rows_d", [ns, 6, DM], F32)
    qT_s = dscr("qT_s", [ns, 64, 34, T_SEQ], BF16)
    dv_s = dscr("dv_s", [ns, T_SEQ, 576], BF16)
    iw_s = dscr("iw_s", [ns, T_SEQ, 8], F32)
    gT_s = dscr("gT_s", [ns, 128, 16, T_SEQ], BF16)
    if dbg:
        dbg_oa = dscr("dbg_oa", [ns, 128, 4, T_SEQ], BF16); dbg_ob = dscr("dbg_ob", [ns, 128, 4, T_SEQ], BF16)
        dbg_mk = dscr("dbg_mk", [ns, 128, 136, 128], BF16)

    ges = ExitStack()
    with ges:
        S = Sched(nc, ges)
        kk = K(nc, S, ns)
        op, dma = kk.op, kk.dma
        GA = Alloc(nc, ges, "g")
        ident_f = GA.sb("ident_f", [128, 128], F32); ident_b = GA.sb("ident_b", [128, 128], BF16)
        ones_b = GA.sb("ones_b", [128, 128], BF16)
        cols = GA.sb("cols", [128, 4, 8, ns], F32)
        neglam = GA.sb("neglam", [128, 1], F32); ogcol = GA.sb("ogcol", [128, 1], F32)
        gains_b = GA.sb("gains_b", [128, 4, 64], F32); inv_b = GA.sb("inv_b", [128, 32], F32)
        iota16 = GA.sb("iota16", [128, 16], F32)

        with ExitStack() as es0:
            A = Alloc(nc, es0, "p0")
            if 3 in phases:
                R_uv = Res("uvcast")
                for (src, c0) in ((pu, 0), (pv, DM)):
                    for i in range(16):
                        dma(lambda e, src=src, c0=c0, i=i: e.dma_start(out=uv_bf[i * 1024:(i + 1) * 1024, c0:c0 + DM], in_=src[i * 1024:(i + 1) * 1024, :]), [], [], R_uv, eng="pool")
            op("pool", lambda e: e.memset(ident_f[:], 0.0), [], [ident_f])
            op("pool", lambda e: e.affine_select(out=ident_f[:], in_=ident_f[:], compare_op=ALU.not_equal, fill=1.0, base=0, pattern=[[-1, 128]], channel_multiplier=1), [ident_f], [ident_f])
            op("dve", lambda e: e.tensor_copy(out=ident_b[:], in_=ident_f[:]), [ident_f], [ident_b])
            op("dve", lambda e: e.memset(ones_b[:], 1.0), [], [ones_b])
            op("pool", lambda e: e.iota(iota16[:], pattern=[[1, 16]], base=0, channel_multiplier=0, allow_small_or_imprecise_dtypes=True), [], [iota16])
            for i in range(4):
                dma(lambda e, i=i: e.dma_start(out=gains_b[:, i, :], in_=gains_in[i].partition_broadcast(128)), [], [gains_b], gains_b)
            dma(lambda e: e.dma_start(out=inv_b[:], in_=inv.partition_broadcast(128)), [], [inv_b], inv_b)
            dma(lambda e: e.dma_start(out=ogcol[:], in_=og_in), [], [ogcol], ogcol)
            op("dve", lambda e: e.tensor_scalar(out=ogcol[:], in0=ogcol[:], scalar1=1.0 - LAMBDA_INIT, scalar2=None, op0=ALU.mult), [ogcol], [ogcol])
            lamv = A.sb("lamv", [128, 4, 64], F32); lamp = A.sb("lamp", [128, 2, 64], F32); lams = A.sb("lams", [128, 2], F32)
            for i in range(4):
                dma(lambda e, i=i: e.dma_start(out=lamv[:, i, :], in_=lam_in[i].partition_broadcast(128)), [], [lamv], lamv)
            lv4 = lamv[:, :, :].rearrange("p (a b) d -> p a b d", b=2)
            op("dve", lambda e: e.tensor_tensor(out=lamp[:], in0=lv4[:, :, 0, :], in1=lv4[:, :, 1, :], op=ALU.mult), [lamv], [lamp])
            op("dve", lambda e: e.tensor_reduce(out=lams[:], in_=lamp[:], axis=AX.X, op=ALU.add), [lamp], [lams])
            op("act", lambda e: e.activation(out=lams[:], in_=lams[:], func=AF.Exp), [lams], [lams])
            op("dve", lambda e: e.tensor_tensor(out=neglam[:], in0=lams[:, 1:2], in1=lams[:, 0:1], op=ALU.subtract), [lams], [neglam])
            op("dve", lambda e: e.tensor_scalar(out=neglam[:], in0=neglam[:], scalar1=-LAMBDA_INIT, scalar2=None, op0=ALU.add), [neglam], [neglam])

            c_sb = A.sb("c_sb", [ns, DM], F32); sg = A.sb("sg", [ns, DM], F32); scT = A.sb("scT", [128, 8, ns], F32)
            pc0 = A.ps("pc0", [128, 8, ns], F32)
            dma(lambda e: e.dma_start(out=c_sb[:], in_=c_in), [], [c_sb], c_sb)
            op("act", lambda e: e.activation(out=sg[:], in_=c_sb[:], func=AF.Sigmoid), [c_sb], [sg])
            op("dve", lambda e: e.tensor_tensor(out=c_sb[:], in0=c_sb[:], in1=sg[:], op=ALU.mult), [c_sb, sg], [c_sb])
            for kc in range(8):
                op("pe", lambda e, kc=kc: e.transpose(out=pc0[:, kc, :], in_=c_sb[0:ns, kc * 128:(kc + 1) * 128], identity=ident_f[0:ns, 0:ns]), [c_sb, ident_f], [pc0])
            op("dve", lambda e: e.tensor_copy(out=scT[:], in_=pc0[:]), [pc0], [scT])
            wst = [A.sb("wst%d" % i, [128, 8, 512], F32) for i in range(2)]
            b_b = A.sb("b_b", [ns, 6 * DM], F32); mod_sb = A.sb("mod_sb", [ns, 6 * DM], F32)
            pm = [A.ps("pm%d" % i, [ns, 512], F32) for i in range(2)]
            dma(lambda e: e.dma_start(out=b_b[:], in_=b_ada.partition_broadcast(ns)), [], [b_b], b_b)
            w_ada_v = w_ada.rearrange("(kc p) n -> p kc n", p=128)
            for j in range(12):
                ws = wst[j % 2]; pmj = pm[j % 2]
                dma(lambda e, ws=ws, j=j: e.dma_start(out=ws[:], in_=w_ada_v[:, :, j * 512:(j + 1) * 512]), [], [ws], ws)
                for kc in range(8):
                    op("pe", lambda e, ws=ws, pmj=pmj, kc=kc: e.matmul(pmj[:], lhsT=scT[:, kc, :], rhs=ws[:, kc, :], start=(kc == 0), stop=(kc == 7)), [scT, ws], [pmj])
                op("dve", lambda e, pmj=pmj, j=j: e.tensor_tensor(out=mod_sb[:, j * 512:(j + 1) * 512], in0=pmj[:], in1=b_b[:, j * 512:(j + 1) * 512], op=ALU.add), [pmj, b_b], [mod_sb])
            rows_sb = A.sb("rows_sb", [ns, 6, DM], F32); ng_b = A.sb("ng_b", [ns, 2, DM], F32)
            dma(lambda e: e.dma_start(out=ng_b[:, 0, :], in_=n1g.partition_broadcast(ns)), [], [ng_b], ng_b)
            dma(lambda e: e.dma_start(out=ng_b[:, 1, :], in_=n2g.partition_broadcast(ns)), [], [ng_b], ng_b)
            for half in range(2):
                o = half * 3 * DM
                op("dve", lambda e, o=o, half=half: e.scalar_tensor_tensor(out=rows_sb[:, half * 3 + 0, :], in0=mod_sb[:, o + DM:o + 2 * DM], scalar=1.0, in1=ng_b[:, half, :], op0=ALU.add, op1=ALU.mult), [mod_sb, ng_b], [rows_sb])
                op("dve", lambda e, o=o, half=half: e.tensor_copy(out=rows_sb[:, half * 3 + 1, :], in_=mod_sb[:, o:o + DM]), [mod_sb], [rows_sb])
                op("dve", lambda e, o=o, half=half: e.tensor_copy(out=rows_sb[:, half * 3 + 2, :], in_=mod_sb[:, o + 2 * DM:o + 3 * DM]), [mod_sb], [rows_sb])
            dma(lambda e: e.dma_start(out=rows_d, in_=rows_sb[:]), [rows_sb], [], rows_sb)
            pc1 = A.ps("pc1", [128, 4, 8, ns], F32)
            for wi, ri in enumerate((0, 1, 3, 4)):
                for kc in range(8):
                    op("pe", lambda e, wi=wi, ri=ri, kc=kc: e.transpose(out=pc1[:, wi, kc, :], in_=rows_sb[0:ns, ri, kc * 128:(kc + 1) * 128], identity=ident_f[0:ns, 0:ns]), [rows_sb, ident_f], [pc1])
            op("dve", lambda e: e.tensor_copy(out=cols[:], in_=pc1[:]), [pc1], [cols])

            S.barrier()
            S.flush()

        def rope_alloc(A):
            return (A.sb("posi", [128, NT], I32), A.sb("posf", [128, NT], F32), A.sb("ang", [128, NT, 32], F32),
                    A.sb("a2", [128, NT, 32], F32), A.sb("ki", [128, NT, 32], I32), A.sb("kf", [128, NT, 32], F32))

        def rope_tables(rb, s, cos_t, sin_t):
            posi, posf, ang, a2, ki, kf = rb
            dma(lambda e: e.dma_start(out=posi[:], in_=pos[s]), [], [posi], posi)
            op("dve", lambda e: e.tensor_copy(out=posf[:], in_=posi[:]), [posi], [posf])
            op("dve", lambda e: e.tensor_tensor(out=ang[:], in0=posf[:, :].unsqueeze(2).to_broadcast([128, NT, 32]), in1=inv_b[:, :].unsqueeze(1).to_broadcast([128, NT, 32]), op=ALU.mult), [posf, inv_b], [ang])
            for (shift, dst) in ((0.0, sin_t), (0.5 * math.pi, cos_t)):
                op("dve", lambda e, shift=shift: e.tensor_scalar(out=a2[:], in0=ang[:], scalar1=shift, scalar2=None, op0=ALU.add), [ang], [a2])
                op("dve", lambda e: e.tensor_scalar(out=ki[:], in0=a2[:], scalar1=1.0 / TWO_PI, scalar2=None, op0=ALU.mult), [a2], [ki])
                op("dve", lambda e: e.tensor_copy(out=kf[:], in_=ki[:]), [ki], [kf])
                op("dve", lambda e: e.scalar_tensor_tensor(out=a2[:], in0=kf[:], scalar=-TWO_PI, in1=a2[:], op0=ALU.mult, op1=ALU.add), [kf, a2], [a2])
                op("dve", lambda e: e.tensor_scalar(out=kf[:], in0=a2[:], scalar1=math.pi, scalar2=TWO_PI, op0=ALU.is_gt, op1=ALU.mult), [a2], [kf])
                op("dve", lambda e: e.tensor_tensor(out=a2[:], in0=a2[:], in1=kf[:], op=ALU.subtract), [a2, kf], [a2])
                op("dve", lambda e: e.tensor_scalar(out=kf[:], in0=a2[:], scalar1=-math.pi, scalar2=TWO_PI, op0=ALU.is_lt, op1=ALU.mult), [a2], [kf])
                op("dve", lambda e: e.tensor_tensor(out=a2[:], in0=a2[:], in1=kf[:], op=ALU.add), [a2, kf], [a2])
                op("act", lambda e, dst=dst: e.activation(out=dst[:], in_=a2[:], func=AF.Sin), [a2], [dst])

        def rms_rstd(A, ssq_ap, n, res, nm):
            op("dve", lambda e: e.tensor_scalar(out=ssq_ap, in0=ssq_ap, scalar1=1.0 / n, scalar2=EPS, op0=ALU.mult, op1=ALU.add), [res], [res])
            op("act", lambda e: e.activation(out=ssq_ap, in_=ssq_ap, func=AF.Sqrt), [res], [res])
            op("dve", lambda e: e.reciprocal(out=ssq_ap, in_=ssq_ap), [res], [res])

        if 1 in phases:
          with ExitStack() as es1:
            A = Alloc(nc, es1, "p1")
            w_in_bf = A.sb("w_in_bf", [128, 8, IN_COLS], BF16)
            w_in_v = w_in.rearrange("(kc p) n -> p kc n", p=128)
            nch = (IN_COLS + 511) // 512
            for j in range(nch):
                lo = j * 512; hi = min(IN_COLS, lo + 512)
                dma(lambda e, lo=lo, hi=hi: e.dma_start(out=w_in_bf[:, :, lo:hi], in_=w_in_v[:, :, lo:hi]), [], [w_in_bf], w_in_bf, eng="pool")
            cos_tr = [A.sb("cos_t%d" % i, [128, NT, 32], F32) for i in range(2)]; sin_tr = [A.sb("sin_t%d" % i, [128, NT, 32], F32) for i in range(2)]
            xt = [A.sb("xt%d" % i, [128, DM], F32) for i in range(2)]
            junk = A.sb("junk", [128, DM], BF16); xnr = [A.sb("xn%d" % i, [128, DM], BF16) for i in range(2)]
            ssq = [A.sb("ssq%d" % i, [128, 1], F32) for i in range(2)]
            hT = [A.sb("hT%d" % i, [128, 8, 128], BF16) for i in range(2)]
            projr = [A.sb("proj%d" % i, [128, NPROJ], F32) for i in range(2)]
            sqbr = [A.sb("sqb%d" % i, [128, 1600], F32) for i in range(2)]; ssqgr = [A.sb("ssqg%d" % i, [128, 25], F32) for i in range(2)]
            rqr = [A.sb("rq%d" % i, [128, 34, 64], BF16) for i in range(2)]
            tv = [A.sb("tv%d" % i, [128, 16, 32], F32) for i in range(2)]
            tp = [A.sb("tp%d" % i, [128, 9, 32], F32) for i in range(2)]
            dvb = [A.sb("dvb%d" % i, [128, 576], BF16) for i in range(2)]
            iwb = [A.sb("iwb%d" % i, [128, 8], F32) for i in range(2)]
            gs = [A.sb("gs%d" % i, [128, 16, 128], BF16) for i in range(2)]
            qTs = [A.sb("qTs%d" % i, [64, 34, 128], BF16) for i in range(2)]
            pTr = [A.ps("pT%d" % i, [128, 8, 128], BF16) for i in range(2)]
            pp = [A.ps("pp%d" % i, [128, 512], F32) for i in range(2)]
            pg = [A.ps("pg%d" % i, [128, 4, 128], F32) for i in range(2)]
            pq = [A.ps("pq%d" % i, [64, 8, 128], BF16) for i in range(2)]
            bounds = [0, 512, 1024, 1536, 2048, 2560, NPROJ]
            rb = rope_alloc(A)
            def p1_tile(s, t, gi):
                i2 = gi % 2
                r0 = s * T_SEQ + t * 128
                xti = xt[i2]; ssi = ssq[i2]; hTi = hT[i2]
                xn = xnr[i2]; proj = projr[i2]; sqb = sqbr[i2]; ssqg = ssqgr[i2]; rq = rqr[i2]; pT = pTr[i2]
                cos_t = cos_tr[s % 2]; sin_t = sin_tr[s % 2]
                dma(lambda e, xti=xti, r0=r0: e.dma_start(out=xti[:], in_=x[r0:r0 + 128, :]), [], [xti], xti)
                op("act", lambda e, xti=xti, ssi=ssi: e.activation(out=junk[:], in_=xti[:], func=AF.Square, accum_out=ssi[:, 0:1]), [xti], [junk, ssi])
                rms_rstd(A, ssi[:, 0:1], float(DM), ssi, "x")
                op("act", lambda e, xti=xti, ssi=ssi: e.activation(out=xn[:], in_=xti[:], func=AF.Copy, scale=ssi[:, 0:1]), [xti, ssi], [xn])
                yield
                for kc in range(8):
                    op("pe", lambda e, kc=kc: e.transpose(out=pT[:, kc, :], in_=xn[:, kc * 128:(kc + 1) * 128], identity=ident_b[:]), [xn, ident_b], [pT])
                for kc in range(8):
                    op("dve", lambda e, kc=kc, hTi=hTi, s=s: e.tensor_scalar(out=hTi[:, kc, :], in0=pT[:, kc, :], scalar1=cols[:, 0, kc, s:s + 1], scalar2=cols[:, 1, kc, s:s + 1], op0=ALU.mult, op1=ALU.add), [pT, cols], [hTi])
                yield
                for j in range(6):
                    lo, hi = bounds[j], bounds[j + 1]; w = hi - lo
                    ppj = pp[j % 2]
                    for kc in range(8):
                        op("pe", lambda e, ppj=ppj, kc=kc, lo=lo, hi=hi, w=w, hTi=hTi: e.matmul(ppj[:, 0:w], lhsT=hTi[:, kc, :], rhs=w_in_bf[:, kc, lo:hi], start=(kc == 0), stop=(kc == 7)), [hTi, w_in_bf], [ppj])
                    op("act", lambda e, ppj=ppj, lo=lo, hi=hi, w=w: e.activation(out=proj[:, lo:hi], in_=ppj[:, 0:w], func=AF.Copy), [ppj], [proj])
                    if j == 2 or j == 5:
                        yield
                gsi = gs[i2]

                def gates_half(ra, rb_):
                    for r in range(ra, rb_):
                        pgr = pg[r % 2]
                        for j in range(4):
                            gc = r * 4 + j
                            for kc in range(8):
                                op("pe", lambda e, pgr=pgr, j=j, gc=gc, kc=kc: e.matmul(pgr[:, j, :], lhsT=w_in_bf[:, kc, GATE0 + gc * 128:GATE0 + (gc + 1) * 128], rhs=hTi[:, kc, :], start=(kc == 0), stop=(kc == 7)), [hTi, w_in_bf], [pgr])
                        op("act", lambda e, pgr=pgr, r=r: e.activation(out=gsi[:, r * 4:(r + 1) * 4, :], in_=pgr[:], func=AF.Sigmoid), [pgr], [gsi])
                gates_half(0, 2)
                yield
                dvi = dvb[i2]; iwi = iwb[i2]
                op("pool", lambda e, dvi=dvi: e.tensor_copy(out=dvi[:, 0:512], in_=proj[:, 1024:1536]), [proj], [dvi])
                op("pool", lambda e, dvi=dvi: e.tensor_copy(out=dvi[:, 512:576], in_=proj[:, 2112:2176]), [proj], [dvi])
                op("pool", lambda e, iwi=iwi: e.tensor_copy(out=iwi[:], in_=proj[:, 2752:2760]), [proj], [iwi])
                dma(lambda e, dvi=dvi, s=s, t=t: e.dma_start(out=dv_s[s][t * 128:(t + 1) * 128, :], in_=dvi[:]), [dvi], [], dvi)
                dma(lambda e, iwi=iwi, s=s, t=t: e.dma_start(out=iw_s[s][t * 128:(t + 1) * 128, :], in_=iwi[:]), [iwi], [], iwi)
                op("pool", lambda e: e.tensor_tensor(out=sqb[:, 0:1024], in0=proj[:, 0:1024], in1=proj[:, 0:1024], op=ALU.mult), [proj], [sqb])
                op("pool", lambda e: e.tensor_tensor(out=sqb[:, 1024:1600], in0=proj[:, 1536:2112], in1=proj[:, 1536:2112], op=ALU.mult), [proj], [sqb])
                op("dve", lambda e: e.tensor_reduce(out=ssqg[:, 0:25], in_=sqb[:, :].rearrange("p (g d) -> p g d", d=64), axis=AX.X, op=ALU.add), [sqb], [ssqg])
                rms_rstd(A, ssqg[:, :], 64.0, ssqg, "g")
                pA = proj[:, 0:1024].rearrange("p (g d) -> p g d", d=64)
                pB = proj[:, 1536:2112].rearrange("p (g d) -> p g d", d=64)
                op("dve", lambda e, pA=pA: e.tensor_tensor(out=pA, in0=pA, in1=ssqg[:, 0:16].unsqueeze(2).to_broadcast([128, 16, 64]), op=ALU.mult), [proj, ssqg], [proj])
                op("dve", lambda e, pB=pB: e.tensor_tensor(out=pB, in0=pB, in1=ssqg[:, 16:25].unsqueeze(2).to_broadcast([128, 9, 64]), op=ALU.mult), [proj, ssqg], [proj])
                op("dve", lambda e, pA=pA: e.tensor_tensor(out=pA[:, 0:8, :], in0=pA[:, 0:8, :], in1=gains_b[:, 0, :].unsqueeze(1).to_broadcast([128, 8, 64]), op=ALU.mult), [proj, gains_b], [proj])
                op("dve", lambda e, pA=pA: e.tensor_tensor(out=pA[:, 8:16, :], in0=pA[:, 8:16, :], in1=gains_b[:, 1, :].unsqueeze(1).to_broadcast([128, 8, 64]), op=ALU.mult), [proj, gains_b], [proj])
                op("dve", lambda e, pB=pB: e.tensor_tensor(out=pB[:, 0:8, :], in0=pB[:, 0:8, :], in1=gains_b[:, 2, :].unsqueeze(1).to_broadcast([128, 8, 64]), op=ALU.mult), [proj, gains_b], [proj])
                op("dve", lambda e, pB=pB: e.tensor_tensor(out=pB[:, 8:9, :], in0=pB[:, 8:9, :], in1=gains_b[:, 3, :].unsqueeze(1).to_broadcast([128, 1, 64]), op=ALU.mult), [proj, gains_b], [proj])
                yield
                gates_half(2, 4)
                dma(lambda e: e.dma_start(out=gT_s[s][:, :, t * 128:(t + 1) * 128], in_=gsi[:]), [gsi], [], gsi)
                yield
                for (lo, G, g0, eng, tmps) in ((0, 16, 0, "dve", tv), (1536, 9, 16, "dve", tv), (2176, 9, 25, "pool", tp)):
                    X = proj[:, lo:lo + G * 64].rearrange("p (g h d) -> p g h d", h=2, d=32)
                    O = rq[:, g0:g0 + G, :].rearrange("p g (h d) -> p g h d", h=2)
                    cb = cos_t[:, t, :].unsqueeze(1).to_broadcast([128, G, 32])
                    sb_ = sin_t[:, t, :].unsqueeze(1).to_broadcast([128, G, 32])
                    t1 = tmps[0][:, 0:G, :]; t2 = tmps[1][:, 0:G, :]
                    rr = [proj, cos_t, sin_t]
                    op(eng, lambda e, X=X, cb=cb, t1=t1: e.tensor_tensor(out=t1, in0=X[:, :, 0, :], in1=cb, op=ALU.mult), rr, [tmps[0]])
                    op(eng, lambda e, X=X, sb_=sb_, t2=t2: e.tensor_tensor(out=t2, in0=X[:, :, 1, :], in1=sb_, op=ALU.mult), rr, [tmps[1]])
                    op(eng, lambda e, O=O, t1=t1, t2=t2: e.tensor_tensor(out=O[:, :, 0, :], in0=t1, in1=t2, op=ALU.subtract), [tmps[0], tmps[1]], [rq])
                    op(eng, lambda e, X=X, cb=cb, t1=t1: e.tensor_tensor(out=t1, in0=X[:, :, 1, :], in1=cb, op=ALU.mult), rr, [tmps[0]])
                    op(eng, lambda e, X=X, sb_=sb_, t2=t2: e.tensor_tensor(out=t2, in0=X[:, :, 0, :], in1=sb_, op=ALU.mult), rr, [tmps[1]])
                    op(eng, lambda e, O=O, t1=t1, t2=t2: e.tensor_tensor(out=O[:, :, 1, :], in0=t1, in1=t2, op=ALU.add), [tmps[0], tmps[1]], [rq])
                yield
                qTi = qTs[i2]
                for bi, (g0, g1) in enumerate(((0, 8), (8, 16), (16, 24), (24, 32), (32, 34))):
                    pqb = pq[bi % 2]
                    for g in range(g0, g1):
                        op("pe", lambda e, pqb=pqb, g=g, g0=g0: e.transpose(out=pqb[:, g - g0, :], in_=rq[:, g, :], identity=ident_b[:]), [rq, ident_b], [pqb])
                    op("act", lambda e, pqb=pqb, g0=g0, g1=g1, qTi=qTi: e.activation(out=qTi[:, g0:g1, :], in_=pqb[:, 0:g1 - g0, :], func=AF.Copy), [pqb], [qTi])
                dma(lambda e, qTi=qTi, s=s, t=t: e.dma_start(out=qT_s[s][:, :, t * 128:(t + 1) * 128], in_=qTi[:]), [qTi], [], qTi)

            tl = [(s_, t_) for s_ in range(ns) for t_ in range(NT)]
            gens = {}

            def pull(i):
                if i < 0 or i >= len(tl):
                    return
                if i not in gens:
                    s_, t_ = tl[i]
                    if t_ == 0:
                        rope_tables(rb, s_, cos_tr[s_ % 2], sin_tr[s_ % 2])
                    gens[i] = p1_tile(s_, t_, i)
                next(gens[i], None)

            pull(0); pull(0); pull(1)
            for i in range(len(tl) + 1):
                pull(i)
                pull(i - 1)
                pull(i)
                pull(i + 1)
                pull(i)
                pull(i)
                pull(i)
                pull(i)
                pull(i + 2)
            S.barrier()
            S.flush()

        if 2 in phases:
          with ExitStack() as es2:
            P2 = Alloc(nc, es2, "p2")
            wba = P2.sb("wba", [128, 4, DM], BF16); wbb = P2.sb("wbb", [128, 4, DM], BF16); wo = P2.sb("wo", [128, 8, DM], BF16)
            oaT = P2.sb("oaT", [128, 4, T_SEQ], BF16); obT = P2.sb("obT", [128, 4, T_SEQ], BF16)
            NMT = NT * (NT + 1) // 2
            maskT_all = P2.sb("maskT_all", [128, NMT, 128], BF16)
            moff = [t * (t + 1) // 2 for t in range(NT)]
            for (src, dst) in ((w_ba, wba), (w_bb, wbb), (w_o, wo)):
                sv_ = src.rearrange("(j p) n -> p j n", p=128)
                for j0 in range(0, sv_.shape[1], 2):
                    dma(lambda e, sv_=sv_, dst=dst, j0=j0: e.dma_start(out=dst[:, j0:j0 + 2, :], in_=sv_[:, j0:j0 + 2, :]), [], [dst], dst, eng="pool")
            for s in range(ns):
                with ExitStack() as esa:
                  if 'A' in stages:
                      A = Alloc(nc, esa, "p2a%d" % s)
                      dvs = A.sb("dvs", [128, NT, 512], BF16)
                      dkq = [A.sb("dkq%d" % i, [64, 2, T_SEQ], BF16) for i in range(2)]
                      PT = [A.sb("PT%d" % i, [128, 512], BF16) for i in range(3)]
                      o0n = A.sb("o0n", [128, T_SEQ], F32)
                      rz = A.sb("rz", [128, 512], F32); o1n = A.sb("o1n", [128, 512], F32); dd = A.sb("dd", [128, 512], F32)
                      dsq = A.sb("dsq", [128, 512], BF16); rstd = A.sb("rstd", [128, 512], F32)
                      ps_s = [A.ps("ps_s%d" % i, [128, 512], F32) for i in range(2)]
                      ps_o = [A.ps("ps_o%d" % i, [128, 512], F32) for i in range(2)]; ps_z = [A.ps("ps_z%d" % i, [128, 512], F32) for i in range(2)]
                      ikT = A.sb("ikT", [64, T_SEQ], BF16); iws = A.sb("iws", [128, NT, 8], F32)
                      iqT = [A.sb("iqT%d" % i, [64, 8, 128], BF16) for i in range(2)]
                      score2 = [A.sb("score%d" % i, [128, T_SEQ], F32) for i in range(2)]
                      rl = [A.sb("rl%d" % i, [128, 512], F32) for i in range(2)]
                      blo = [A.sb("blo%d" % i, [128, 1], F32) for i in range(2)]; bd0 = [A.sb("bd0%d" % i, [128, 1], F32) for i in range(2)]; bmid = [A.sb("bmid%d" % i, [128, 1], F32) for i in range(2)]
                      bcnt = [A.sb("bcnt%d" % i, [128, 1], F32) for i in range(2)]; bg = [A.sb("bg%d" % i, [128, 1], F32) for i in range(2)]; bjunk = A.sb("bjunk", [128, T_SEQ], BF16)
                      maskb = A.sb("maskb", [128, T_SEQ], BF16)
                      ps_i = A.ps("ps_i", [128, 512], F32); ps_m = A.ps("ps_m", [128, 8, 128], BF16)
                      dma(lambda e: e.dma_start(out=dvs[:], in_=dv_s[s].rearrange("(t p) c -> p t c", p=128)[:, :, 0:512]), [], [dvs], dvs)
                      dma(lambda e: e.dma_start(out=ikT[:], in_=qT_s[s][:, 33, :]), [], [ikT], ikT)
                      dma(lambda e: e.dma_start(out=iws[:], in_=iw_s[s].rearrange("(t p) c -> p t c", p=128)), [], [iws], iws)

                      def b_part1_gen():
                          ii = 0
                          for t0 in range(0, NT, 2):
                              pair = (t0, t0 + 1)
                              for pi, t in enumerate(pair):
                                  iq_ = iqT[t % 2]; sc_ = score2[pi]
                                  tsl = slice(t * 128, (t + 1) * 128)
                                  dma(lambda e, iq_=iq_, tsl=tsl: e.dma_start(out=iq_[:], in_=qT_s[s][:, 25:33, tsl]), [], [iq_], iq_)
                                  nk = (t + 1) * 128
                                  for kc in range((nk + 511) // 512):
                                      w = min(512, nk - kc * 512)
                                      ksl = slice(kc * 512, kc * 512 + w)
                                      for hh in range(8):
                                          rli = rl[ii % 2]; ii += 1
                                          op("pe", lambda e, iq_=iq_, hh=hh, ksl=ksl, w=w: e.matmul(ps_i[:, 0:w], lhsT=iq_[:, hh, :], rhs=ikT[:, ksl], start=True, stop=True), [iq_, ikT], [ps_i])
                                          op("act", lambda e, rli=rli, w=w: e.activation(out=rli[:, 0:w], in_=ps_i[:, 0:w], func=AF.Relu), [ps_i], [rli])
                                          if hh == 0:
                                              op("dve", lambda e, rli=rli, ksl=ksl, w=w, hh=hh, t=t, sc_=sc_: e.tensor_scalar(out=sc_[:, ksl], in0=rli[:, 0:w], scalar1=iws[:, t, hh:hh + 1], scalar2=None, op0=ALU.mult), [rli, iws], [sc_])
                                          else:
                                              op("dve", lambda e, rli=rli, ksl=ksl, w=w, hh=hh, t=t, sc_=sc_: e.scalar_tensor_tensor(out=sc_[:, ksl], in0=rli[:, 0:w], scalar=iws[:, t, hh:hh + 1], in1=sc_[:, ksl], op0=ALU.mult, op1=ALU.add), [rli, iws, sc_], [sc_])
                                          yield (w / 960.0 + 0.15, 0.4)
                                  op("dve", lambda e, t=t, sc_=sc_: e.memset(sc_[0:64, t * 128 + 64:(t + 1) * 128], NEG), [], [sc_])
                              if t0 >= 2:
                                  for pi, t in enumerate(pair):
                                      nk = (t + 1) * 128; sc_ = score2[pi]; lo_ = blo[pi]; d0_ = bd0[pi]
                                      op("dve", lambda e, nk=nk, sc_=sc_, lo_=lo_: e.tensor_reduce(out=lo_[:], in_=sc_[:, 0:nk - 64], axis=AX.X, op=ALU.min), [sc_], [lo_])
                                      op("dve", lambda e, nk=nk, sc_=sc_, d0_=d0_: e.tensor_reduce(out=d0_[:], in_=sc_[:, 0:nk], axis=AX.X, op=ALU.max), [sc_], [d0_])
                                  for pi in range(2):
                                      lo_ = blo[pi]; d0_ = bd0[pi]
                                      op("dve", lambda e, lo_=lo_, d0_=d0_: e.tensor_tensor(out=d0_[:], in0=d0_[:], in1=lo_[:], op=ALU.subtract), [d0_, lo_], [d0_])
                                  for k in range(24):
                                      ck = 2.0 ** (-(k + 1))
                                      for pi in range(2):
                                          lo_ = blo[pi]; d0_ = bd0[pi]; mid_ = bmid[pi]
                                          op("dve", lambda e, ck=ck, lo_=lo_, d0_=d0_, mid_=mid_: e.scalar_tensor_tensor(out=mid_[:], in0=d0_[:], scalar=ck, in1=lo_[:], op0=ALU.mult, op1=ALU.add), [d0_, lo_], [mid_])
                                      for pi, t in enumerate(pair):
                                          nk = (t + 1) * 128; sc_ = score2[pi]; mid_ = bmid[pi]; cnt_ = bcnt[pi]
                                          op("dve", lambda e, nk=nk, sc_=sc_, mid_=mid_, cnt_=cnt_: e.tensor_scalar(out=bjunk[:, 0:nk], in0=sc_[:, 0:nk], scalar1=mid_[:, 0:1], scalar2=None, op0=ALU.is_ge, op1=ALU.add, accum_out=cnt_[:, 0:1]), [sc_, mid_], [cnt_])
                                      for pi in range(2):
                                          cnt_ = bcnt[pi]; g_ = bg[pi]
                                          op("dve", lambda e, ck=ck, cnt_=cnt_, g_=g_: e.tensor_scalar(out=g_[:], in0=cnt_[:], scalar1=255.5, scalar2=ck, op0=ALU.is_ge, op1=ALU.mult), [cnt_], [g_])
                                      for pi in range(2):
                                          lo_ = blo[pi]; d0_ = bd0[pi]; g_ = bg[pi]
                                          op("dve", lambda e, lo_=lo_, d0_=d0_, g_=g_: e.scalar_tensor_tensor(out=lo_[:], in0=g_[:], scalar=d0_[:, 0:1], in1=lo_[:], op0=ALU.mult, op1=ALU.add), [g_, d0_, lo_], [lo_])
                                      yield (((pair[0] + 1) * 128 + (pair[1] + 1) * 128) / 960.0 + 1.2, 0.0)
                              for pi, t in enumerate(pair):
                                  nk = (t + 1) * 128; sc_ = score2[pi]; lo_ = blo[pi]
                                  if t0 >= 2:
                                      op("dve", lambda e, nk=nk, sc_=sc_, lo_=lo_: e.tensor_scalar(out=maskb[:, 0:nk], in0=sc_[:, 0:nk], scalar1=lo_[:, 0:1], scalar2=None, op0=ALU.is_ge), [sc_, lo_], [maskb])
                                  else:
                                      op("dve", lambda e, nk=nk, sc_=sc_: e.tensor_scalar(out=maskb[:, 0:nk], in0=sc_[:, 0:nk], scalar1=-1.0e29, scalar2=None, op0=ALU.is_ge), [sc_], [maskb])
                                  yield (nk / 960.0 + 0.2, 0.0)
                                  for kb in range((t + 8) // 8):
                                      k1 = min(t + 1, kb * 8 + 8)
                                      for kt in range(kb * 8, k1):
                                          op("pe", lambda e, kt=kt, kb=kb: e.transpose(out=ps_m[:, kt - kb * 8, :], in_=maskb[:, kt * 128:(kt + 1) * 128], identity=ident_b[:]), [maskb, ident_b], [ps_m])
                                      op("act", lambda e, kb=kb, k1=k1, t=t: e.activation(out=maskT_all[:, moff[t] + kb * 8:moff[t] + k1, :], in_=ps_m[:, 0:k1 - kb * 8, :], func=AF.Copy), [ps_m], [maskT_all])
                                      yield (0.0, 0.1 * (k1 - kb * 8))

                      its = []
                      for h in range(4):
                          for m in range(2):
                              for G in range(4):
                                  for kt in range(4 * G + 4):
                                      its.append((h, m, G, kt, 4 * G + 4))

                      def a_loads(g):
                          dk_ = dkq[g % 2]
                          dma(lambda e, dk_=dk_, g=g: e.dma_start(out=dk_[:, 0, :], in_=qT_s[s][:, g, :]), [], [dk_], dk_)
                          dma(lambda e, dk_=dk_, g=g: e.dma_start(out=dk_[:, 1, :], in_=qT_s[s][:, 8 + g, :]), [], [dk_], dk_)

                      def a_stage1(i):
                          h, m, G, kt, nkt = its[i]
                          dk_ = dkq[(2 * h + m) % 2]; pss = ps_s[acnt["s"] % 2]; acnt["s"] += 1; pt = PT[i % 3]
                          op("pe", lambda e: e.matmul(pss[:], lhsT=dk_[:, 1, kt * 128:(kt + 1) * 128], rhs=dk_[:, 0, G * 512:(G + 1) * 512], start=True, stop=True), [dk_], [pss])
                          op("act", lambda e: e.activation(out=pt[:], in_=pss[:], func=AF.Exp, scale=0.125), [pss], [pt])
                          if kt >= 4 * G:
                              j = kt - 4 * G
                              if j > 0:
                                  op("pool", lambda e: e.memset(pt[:, 0:j * 128], 0.0), [], [pt])
                              op("pool", lambda e: e.memset(pt[64:128, j * 128:j * 128 + 64], 0.0), [], [pt])

                      def a_stage2(i):
                          h, m, G, kt, nkt = its[i]
                          pt = PT[i % 3]
                          grp = (h * 2 + m) * 4 + G
                          pso_ = ps_o[grp % 2]; psz_ = ps_z[grp % 2]
                          op("pe", lambda e: e.matmul(pso_[:], lhsT=dvs[:, kt, h * 128:(h + 1) * 128], rhs=pt[:], start=(kt == 0), stop=(kt == nkt - 1)), [dvs, pt], [pso_])
                          op("pe", lambda e: e.matmul(psz_[:], lhsT=ones_b[:], rhs=pt[:], start=(kt == 0), stop=(kt == nkt - 1)), [ones_b, pt], [psz_])
                          if kt != nkt - 1:
                              return
                          op("dve", lambda e: e.reciprocal(out=rz[:], in_=psz_[:]), [psz_], [rz])
                          gsl = slice(G * 512, (G + 1) * 512)
                          if m == 0:
                              op("dve", lambda e: e.tensor_tensor(out=o0n[:, gsl], in0=pso_[:], in1=rz[:], op=ALU.mult), [pso_, rz], [o0n])
                          else:
                              op("dve", lambda e: e.tensor_tensor(out=o1n[:], in0=pso_[:], in1=rz[:], op=ALU.mult), [pso_, rz], [o1n])
                              op("dve", lambda e: e.scalar_tensor_tensor(out=dd[:], in0=o1n[:], scalar=neglam[:, 0:1], in1=o0n[:, gsl], op0=ALU.mult, op1=ALU.add), [o1n, neglam, o0n], [dd])
                              op("act", lambda e: e.activation(out=dsq[:], in_=dd[:], func=AF.Square), [dd], [dsq])
                              ps_q = ps_s[acnt["s"] % 2]; acnt["s"] += 1
                              op("pe", lambda e: e.matmul(ps_q[:], lhsT=ones_b[:], rhs=dsq[:], start=True, stop=True), [ones_b, dsq], [ps_q])
                              op("dve", lambda e: e.tensor_scalar(out=rstd[:], in0=ps_q[:], scalar1=1.0 / 128, scalar2=EPS, op0=ALU.mult, op1=ALU.add), [ps_q], [rstd])
                              op("act", lambda e: e.activation(out=rstd[:], in_=rstd[:], func=AF.Sqrt), [rstd], [rstd])
                              op("dve", lambda e: e.reciprocal(out=rstd[:], in_=rstd[:]), [rstd], [rstd])
                              op("dve", lambda e: e.tensor_tensor(out=dd[:], in0=dd[:], in1=rstd[:], op=ALU.mult), [dd, rstd], [dd])
                              op("dve", lambda e: e.tensor_scalar(out=oaT[:, h, gsl], in0=dd[:], scalar1=ogcol[:, 0:1], scalar2=None, op0=ALU.mult), [dd, ogcol], [oaT])

                      acnt = {"s": 0}
                      clk = {"pe": 0.0, "dve": 0.0}
                      bgen = b_part1_gen()
                      a_loads(0)
                      a_stage1(0)
                      for i in range(len(its)):
                          h, m, G, kt, nkt = its[i]
                          if G == 0 and kt == 0 and 2 * h + m + 1 < 8:
                              a_loads(2 * h + m + 1)
                          if i + 1 < len(its):
                              a_stage1(i + 1)
                          a_stage2(i)
                          clk["pe"] += 1.25
                          while clk["dve"] < clk["pe"]:
                              c = next(bgen, None)
                              if c is None:
                                  break
                              clk["dve"] += c[0]; clk["pe"] += c[1]
                      for _ in bgen:
                          pass
                      S.barrier(); S.flush()

                with ExitStack() as esb:
                  if 'B' in stages:
                      A = Alloc(nc, esb, "p2b%d" % s)
                      skT = A.sb("skT", [64, T_SEQ], BF16)
                      svd = A.sb("svd", [128, NT, 128], BF16)
                      sqT = [A.sb("sqT%d" % i, [64, 8, 128], BF16) for i in range(3)]
                      PT = [A.sb("PTb%d" % i, [128, 4, 128], BF16) for i in range(4)]
                      oS = [A.sb("oS%d" % i, [128, 4, 128], F32) for i in range(2)]; zS = [A.sb("zS%d" % i, [128, 4, 128], F32) for i in range(2)]
                      ps_s = [A.ps("ps_sb%d" % i, [128, 4, 128], F32) for i in range(3)]
                      ps_o = [A.ps("ps_ob%d" % i, [128, 4, 128], F32) for i in range(2)]; ps_z = [A.ps("ps_zb%d" % i, [128, 4, 128], F32) for i in range(2)]
                      dma(lambda e: e.dma_start(out=skT[:], in_=qT_s[s][:, 24, :]), [], [skT], skT)
                      svv = dv_s[s].rearrange("(t p) c -> p t c", p=128)
                      dma(lambda e: e.dma_start(out=svd[:, :, 0:64], in_=svv[:, :, 512:576]), [], [svd], svd)
                      dma(lambda e: e.dma_start(out=svd[:, :, 64:128], in_=svv[:, :, 512:576]), [], [svd], svd)
                      its = [(t, half, kt) for t in range(NT) for half in range(2) for kt in range(t + 1)]

                      def sq_load(t):
                          sq_ = sqT[t % 3]
                          dma(lambda e: e.dma_start(out=sq_[:], in_=qT_s[s][:, 16:24, t * 128:(t + 1) * 128]), [], [sq_], sq_)

                      def st1(i):
                          t, half, kt = its[i]
                          sq_ = sqT[t % 3]
                          pss = ps_s[i % 3]; pt = PT[i % 4]
                          op("pe", lambda e: e.matmul(pss[:], lhsT=skT[:, kt * 128:(kt + 1) * 128], rhs=sq_[:, 4 * half:4 * half + 4, :], start=True, stop=True), [skT, sq_], [pss])
                          op("act", lambda e: e.activation(out=pt[:], in_=pss[:], func=AF.Exp, scale=0.125), [pss], [pt])
                          op("dve", lambda e: e.tensor_tensor(out=pt[:], in0=pt[:], in1=maskT_all[:, moff[t] + kt, :].unsqueeze(1).to_broadcast([128, 4, 128]), op=ALU.mult), [pt, maskT_all], [pt])

                      def st2(i):
                          t, half, kt = its[i]
                          pt = PT[i % 4]
                          grp = 2 * t + half
                          pso_ = ps_o[grp % 2]; psz_ = ps_z[grp % 2]
                          tsl = slice(t * 128, (t + 1) * 128)
                          op("pe", lambda e: e.matmul(pso_[:], lhsT=svd[:, kt, :], rhs=pt[:], start=(kt == 0), stop=(kt == t)), [svd, pt], [pso_])
                          op("pe", lambda e: e.matmul(psz_[:], lhsT=ones_b[:], rhs=pt[:], start=(kt == 0), stop=(kt == t)), [ones_b, pt], [psz_])
                          if kt != t:
                              return
                          oS_ = oS[grp % 2]; zS_ = zS[grp % 2]
                          op("act", lambda e: e.activation(out=oS_[:], in_=pso_[:], func=AF.Copy), [pso_], [oS_])
                          op("dve", lambda e: e.reciprocal(out=zS_[:], in_=psz_[:]), [psz_], [zS_])
                          for par in range(2):
                              psl = slice(par * 64, par * 64 + 64)
                              op("dve", lambda e, psl=psl, par=par: e.tensor_tensor(out=obT[psl, 2 * half:2 * half + 2, tsl], in0=oS_[psl, par::2, :], in1=zS_[psl, par::2, :], op=ALU.mult), [oS_, zS_], [obT])

                      sq_load(0); sq_load(1)
                      st1(0); st1(1)
                      for i in range(len(its)):
                          t, half, kt = its[i]
                          if half == 0 and kt == 0 and t + 2 < NT:
                              sq_load(t + 2)
                          if i + 2 < len(its):
                              st1(i + 2)
                          st2(i)
                      S.barrier(); S.flush()

                with ExitStack() as esc:
                  if 'C' in stages:
                      A = Alloc(nc, esc, "p2c%d" % s)
                      gT = [A.sb("gT%d" % i, [128, 2, 512], BF16) for i in range(2)]
                      t1 = A.sb("t1", [128, 512], F32); t2 = A.sb("t2", [128, 512], F32)
                      mT = A.sb("mT", [128, 8, 512], BF16)
                      xt = [A.sb("xtc%d" % i, [128, DM], F32) for i in range(2)]
                      g1b = A.sb("g1b", [128, DM], F32); tmp3 = A.sb("tmp3", [128, DM], F32)
                      ps_a = A.ps("ps_a", [128, 512], F32); ps_b = A.ps("ps_b", [128, 512], F32)
                      ps_x = [A.ps("ps_x%d" % i, [128, 512], F32) for i in range(2)]
                      dma(lambda e: e.dma_start(out=g1b[:], in_=rows_d[s, 2:3, :].partition_broadcast(128)), [], [g1b], g1b)
                      if dbg:
                          dma(lambda e: e.dma_start(out=dbg_oa[s], in_=oaT[:]), [oaT], [], oaT)
                          dma(lambda e: e.dma_start(out=dbg_ob[s], in_=obT[:]), [obT], [], obT)
                          dma(lambda e: e.dma_start(out=dbg_mk[s], in_=maskT_all[:]), [maskT_all], [], maskT_all)
                      ic = 0
                      for G in range(4):
                          gsl = slice(G * 512, (G + 1) * 512)
                          for c in range(8):
                              gTi = gT[ic % 2]; ic += 1
                              dma(lambda e, gTi=gTi, c=c, gsl=gsl: e.dma_start(out=gTi[:], in_=gT_s[s][:, c::8, gsl]), [], [gTi], gTi)
                              for j in range(4):
                                  op("pe", lambda e, j=j, c=c, gsl=gsl: e.matmul(ps_a[:], lhsT=wba[:, j, c * 128:(c + 1) * 128], rhs=oaT[:, j, gsl], start=(j == 0), stop=(j == 3)), [wba, oaT], [ps_a])
                              for j in range(4):
                                  op("pe", lambda e, j=j, c=c, gsl=gsl: e.matmul(ps_b[:], lhsT=wbb[:, j, c * 128:(c + 1) * 128], rhs=obT[:, j, gsl], start=(j == 0), stop=(j == 3)), [wbb, obT], [ps_b])
                              op("dve", lambda e, gTi=gTi: e.tensor_tensor(out=t1[:], in0=ps_a[:], in1=gTi[:, 0, :], op=ALU.mult), [ps_a, gTi], [t1])
                              op("dve", lambda e, gTi=gTi: e.tensor_tensor(out=t2[:], in0=ps_b[:], in1=gTi[:, 1, :], op=ALU.mult), [ps_b, gTi], [t2])
                              op("pool", lambda e, c=c: e.tensor_tensor(out=mT[:, c, :], in0=t1[:], in1=t2[:], op=ALU.add), [t1, t2], [mT])
                          for q in range(4):
                              tt = 4 * G + q
                              r0 = s * T_SEQ + tt * 128
                              xti = xt[tt % 2]
                              dma(lambda e, xti=xti, r0=r0: e.dma_start(out=xti[:], in_=x[r0:r0 + 128, :]), [], [xti], xti)
                              for half in range(2):
                                  psx = ps_x[half]
                                  for c in range(8):
                                      op("pe", lambda e, psx=psx, c=c, q=q, half=half: e.matmul(psx[:], lhsT=mT[:, c, q * 128:(q + 1) * 128], rhs=wo[:, c, half * 512:(half + 1) * 512], start=(c == 0), stop=(c == 7)), [mT, wo], [psx])
                                  op("dve", lambda e, psx=psx, half=half: e.tensor_tensor(out=tmp3[:, half * 512:(half + 1) * 512], in0=psx[:], in1=g1b[:, half * 512:(half + 1) * 512], op=ALU.mult), [psx, g1b], [tmp3])
                              op("pool", lambda e, xti=xti: e.tensor_tensor(out=xti[:], in0=xti[:], in1=tmp3[:], op=ALU.add), [xti, tmp3], [xti])
                              dma(lambda e, xti=xti, r0=r0: e.dma_start(out=out[r0:r0 + 128, :], in_=xti[:]), [xti], [], xti)
                      S.barrier(); S.flush()

        if 3 in phases:
          with ExitStack() as es3:
            A = Alloc(nc, es3, "p3")
            wq_bf = A.sb("wq_bf", [128, 8, 2048], BF16); skb = A.sb("skb", [128, 16, 128], BF16)
            w_q_v = w_q.rearrange("(kc p) n -> p kc n", p=128)
            for j in range(4):
                dma(lambda e, j=j: e.dma_start(out=wq_bf[:, :, j * 512:(j + 1) * 512], in_=w_q_v[:, :, j * 512:(j + 1) * 512]), [], [wq_bf], wq_bf, eng="pool")
            dma(lambda e: e.dma_start(out=skb[:], in_=subkT), [], [skb], skb, eng="pool")
            xt = [A.sb("xt%d" % i, [128, DM], F32) for i in range(2)]
            junk = A.sb("junk", [128, DM], BF16); xn = A.sb("xn", [128, DM], BF16)
            ssq = [A.sb("ssq%d" % i, [128, 1], F32) for i in range(2)]
            h2T = A.sb("h2T", [128, 8, 128], BF16)
            h2 = [A.sb("h2_%d" % i, [128, 8, 128], BF16) for i in range(2)]
            tmpf = A.sb("tmpf", [128, DM], F32)
            G2b = [A.sb("G2b%d" % i, [128, DM], F32) for i in range(2)]
            qT = A.sb("qT", [128, 16, 128], BF16)
            sc_sb = A.sb("sc_sb", [128, 16, 128], F32)
            v16 = A.sb("v16", [128, 16, 16], F32); i16 = A.sb("i16", [128, 16, 16], U32); i16f = A.sb("i16f", [128, 16, 16], F32)
            cand = A.sb("cand", [128, 8, 256], F32)
            sc = A.sb("sc", [128, 8, 16], F32); posu = A.sb("posu", [128, 8, 16], U32)
            au = A.sb("au", [128, 8, 16], U32); bu = A.sb("bu", [128, 8, 16], U32)
            af = A.sb("af", [128, 8, 16], F32); bf_ = A.sb("bf_", [128, 8, 16], F32)
            eq = A.sb("eq", [128, 8, 16, 16], F32)
            e1 = A.sb("e1", [128, 8, 16], F32); e2 = A.sb("e2", [128, 8, 16], F32)
            eidx = [A.sb("eidx%d" % i, [128, 128], U32) for i in range(2)]
            ex = A.sb("ex", [128, 8, 16], F32); esum = A.sb("esum", [128, 8], F32)
            gg = [A.sb("gg%d" % i, [128, 128], F32) for i in range(2)]
            av = A.sb("av", [128, 128], F32)
            wg8 = [A.sb("wg8_%d" % i, [128, 8], F32) for i in range(2)]
            NR = 22
            uvg = [A.sb("uvg%d" % i, [128, 2 * DM], BF16) for i in range(NR)]
            djunk = A.sb("djunk", [128, DM], BF16)
            dg = [A.sb("dg%d" % i, [128, 128], BF16) for i in range(4)]
            dg1 = [A.sb("dg1_%d" % i, [128, 128], F32) for i in range(4)]
            pT = A.ps("pT3", [128, 8, 128], BF16)
            psq = [A.ps("psq%d" % i, [128, 4, 128], F32) for i in range(2)]
            pso = [[A.ps("pso%d_%d" % (i, j), [128, 512], F32) for j in range(2)] for i in range(2)]
            tiles = [(s, t) for s in range(ns) for t in range(NT)]

            def routing_pieces(gi):
                s, t = tiles[gi]
                i2 = gi % 2
                r0 = s * T_SEQ + t * 128
                xti = xt[i2]; ssi = ssq[i2]; eix = eidx[i2]; h2i = h2[i2]; ggi = gg[i2]
                P = []
                M = []

                def R(eng, fn, reads=(), writes=()):
                    M.append((eng, fn, reads, writes, None))

                def RD(fn, reads=(), writes=(), primary=None):
                    M.append(("dma", fn, reads, writes, primary))

                def rms_R(ssq_ap, n, res, nm):
                    R("dve", lambda e: e.tensor_scalar(out=ssq_ap, in0=ssq_ap, scalar1=1.0 / n, scalar2=EPS, op0=ALU.mult, op1=ALU.add), [res], [res])
                    R("act", lambda e: e.activation(out=ssq_ap, in_=ssq_ap, func=AF.Sqrt), [res], [res])
                    R("dve", lambda e: e.reciprocal(out=ssq_ap, in_=ssq_ap), [res], [res])

                def p_load():
                    if t == 0:
                        gb = G2b[s % 2]
                        RD(lambda e: e.dma_start(out=gb[:], in_=rows_d[s, 5:6, :].partition_broadcast(128)), [], [gb], gb)
                    RD(lambda e: e.dma_start(out=xti[:], in_=out[r0:r0 + 128, :]), [], [xti], xti)
                    R("act", lambda e: e.activation(out=junk[:], in_=xti[:], func=AF.Square, accum_out=ssi[:, 0:1]), [xti], [junk, ssi])
                    rms_R(ssi[:, 0:1], float(DM), ssi, "x")
                    R("act", lambda e: e.activation(out=xn[:], in_=xti[:], func=AF.Copy, scale=ssi[:, 0:1]), [xti, ssi], [xn])
                    for kc in range(8):
                        R("pe", lambda e, kc=kc: e.transpose(out=pT[:, kc, :], in_=xn[:, kc * 128:(kc + 1) * 128], identity=ident_b[:]), [xn, ident_b], [pT])
                    for kc in range(8):
                        R("act", lambda e, kc=kc: e.activation(out=h2T[:, kc, :], in_=pT[:, kc, :], func=AF.Identity, scale=cols[:, 2, kc, s:s + 1], bias=cols[:, 3, kc, s:s + 1]), [pT, cols], [h2T])
                    for kc in range(8):
                        R("pe", lambda e, kc=kc: e.transpose(out=pT[:, kc, :], in_=h2T[:, kc, :], identity=ident_b[:]), [h2T, ident_b], [pT])
                    R("act", lambda e: e.activation(out=h2i[:], in_=pT[:], func=AF.Copy), [pT], [h2i])
                P.append(p_load)

                def p_q(r0_, r1_):
                    def f():
                        for r in range(r0_, r1_):
                            pq_ = psq[r % 2]
                            for j in range(4):
                                hp = r * 4 + j
                                for kc in range(8):
                                    R("pe", lambda e, pq_=pq_, j=j, hp=hp, kc=kc: e.matmul(pq_[:, j, :], lhsT=wq_bf[:, kc, hp * 128:(hp + 1) * 128], rhs=h2T[:, kc, :], start=(kc == 0), stop=(kc == 7)), [wq_bf, h2T], [pq_])
                            R("act", lambda e, pq_=pq_, r=r: e.activation(out=qT[:, r * 4:(r + 1) * 4, :], in_=pq_[:], func=AF.Copy), [pq_], [qT])
                    return f
                P.append(p_q(0, 2)); P.append(p_q(2, 4))

                def p_sc():
                    for r in range(4):
                        pq_ = psq[r % 2]
                        for j in range(4):
                            hp = r * 4 + j
                            R("pe", lambda e, pq_=pq_, j=j, hp=hp: e.matmul(pq_[:, j, :], lhsT=qT[:, hp, :], rhs=skb[:, hp, :], start=True, stop=True), [qT, skb], [pq_])
                        R("act", lambda e, pq_=pq_, r=r: e.activation(out=sc_sb[:, r * 4:(r + 1) * 4, :], in_=pq_[:], func=AF.Copy), [pq_], [sc_sb])
                P.append(p_sc)

                def p_top(h0, h1):
                    def f():
                        for hp in range(h0, h1):
                            R("dve", lambda e, hp=hp: e.max(out=v16[:, hp, 0:8], in_=sc_sb[:, hp, :]), [sc_sb], [v16])
                            R("dve", lambda e, hp=hp: e.max_index(out=i16[:, hp, 0:8], in_max=v16[:, hp, 0:8], in_values=sc_sb[:, hp, :]), [sc_sb, v16], [i16])
                            R("dve", lambda e, hp=hp: e.match_replace(out=sc_sb[:, hp, :], in_to_replace=v16[:, hp, 0:8], in_values=sc_sb[:, hp, :], imm_value=NEG), [sc_sb, v16], [sc_sb])
                            R("dve", lambda e, hp=hp: e.max(out=v16[:, hp, 8:16], in_=sc_sb[:, hp, :]), [sc_sb], [v16])
                            R("dve", lambda e, hp=hp: e.max_index(out=i16[:, hp, 8:16], in_max=v16[:, hp, 8:16], in_values=sc_sb[:, hp, :]), [sc_sb, v16], [i16])
                    return f
                for q in range(4):
                    P.append(p_top(q * 4, q * 4 + 4))

                def p_cand():
                    v4 = v16[:, :, :].rearrange("p (h a) k -> p h a k", a=2)
                    c4 = cand[:, :, :].rearrange("p h (a b) -> p h a b", b=16)
                    R("dve", lambda e: e.tensor_tensor(out=c4, in0=v4[:, :, 0, :].unsqueeze(3).to_broadcast([128, 8, 16, 16]), in1=v4[:, :, 1, :].unsqueeze(2).to_broadcast([128, 8, 16, 16]), op=ALU.add), [v16], [cand])
                P.append(p_cand)

                def p_top2(h0, h1):
                    def f():
                        for h in range(h0, h1):
                            R("dve", lambda e, h=h: e.max(out=sc[:, h, 0:8], in_=cand[:, h, :]), [cand], [sc])
                            R("dve", lambda e, h=h: e.max_index(out=posu[:, h, 0:8], in_max=sc[:, h, 0:8], in_values=cand[:, h, :]), [cand, sc], [posu])
                            R("dve", lambda e, h=h: e.match_replace(out=cand[:, h, :], in_to_replace=sc[:, h, 0:8], in_values=cand[:, h, :], imm_value=NEG), [cand, sc], [cand])
                            R("dve", lambda e, h=h: e.max(out=sc[:, h, 8:16], in_=cand[:, h, :]), [cand], [sc])
                            R("dve", lambda e, h=h: e.max_index(out=posu[:, h, 8:16], in_max=sc[:, h, 8:16], in_values=cand[:, h, :]), [cand, sc], [posu])
                    return f
                for q in range(4):
                    P.append(p_top2(q * 2, q * 2 + 2))

                def p_idx():
                    R("dve", lambda e: e.tensor_single_scalar(out=au[:], in_=posu[:], scalar=4, op=ALU.logical_shift_right), [posu], [au])
                    R("dve", lambda e: e.tensor_single_scalar(out=bu[:], in_=posu[:], scalar=15, op=ALU.bitwise_and), [posu], [bu])
                    R("dve", lambda e: e.tensor_copy(out=af[:], in_=au[:]), [au], [af])
                    R("dve", lambda e: e.tensor_copy(out=bf_[:], in_=bu[:]), [bu], [bf_])
                    R("dve", lambda e: e.tensor_copy(out=i16f[:], in_=i16[:]), [i16], [i16f])
                    i4 = i16f[:, :, :].rearrange("p (h a) k -> p h a k", a=2)
                    io4 = iota16[:, :].unsqueeze(1).unsqueeze(1).to_broadcast([128, 8, 16, 16])
                    for (sel, which, dst) in ((af, 0, e1), (bf_, 1, e2)):
                        R("dve", lambda e, sel=sel: e.tensor_tensor(out=eq[:], in0=io4, in1=sel[:, :, :].unsqueeze(3).to_broadcast([128, 8, 16, 16]), op=ALU.is_equal), [iota16, sel], [eq])
                        R("dve", lambda e, which=which: e.tensor_tensor(out=eq[:], in0=eq[:], in1=i4[:, :, which, :].unsqueeze(2).to_broadcast([128, 8, 16, 16]), op=ALU.mult), [eq, i16f], [eq])
                        R("dve", lambda e, dst=dst: e.tensor_reduce(out=dst[:], in_=eq[:], axis=AX.X, op=ALU.add), [eq], [dst])
                    R("dve", lambda e: e.scalar_tensor_tensor(out=e1[:], in0=e1[:], scalar=128.0, in1=e2[:], op0=ALU.mult, op1=ALU.add), [e1, e2], [e1])
                    R("dve", lambda e: e.tensor_copy(out=eix[:], in_=e1[:, :, :].rearrange("p h k -> p (h k)")), [e1], [eix])
                P.append(p_idx)

                def p_soft():
                    R("dve", lambda e: e.tensor_tensor(out=ex[:], in0=sc[:], in1=sc[:, :, 0:1].to_broadcast([128, 8, 16]), op=ALU.subtract), [sc], [ex])
                    R("act", lambda e: e.activation(out=ex[:], in_=ex[:], func=AF.Exp), [ex], [ex])
                    R("dve", lambda e: e.tensor_reduce(out=esum[:], in_=ex[:], axis=AX.X, op=ALU.add), [ex], [esum])
                    R("dve", lambda e: e.reciprocal(out=esum[:], in_=esum[:]), [esum], [esum])
                    R("dve", lambda e: e.tensor_tensor(out=ggi[:, :].rearrange("p (h k) -> p h k", k=16), in0=ex[:], in1=esum[:, :].unsqueeze(2).to_broadcast([128, 8, 16]), op=ALU.mult), [ex, esum], [ggi])
                P.append(p_soft)
                npre = 0
                for pi_, p in enumerate(P):
                    p()
                    if pi_ == 3:
                        npre = len(M)
                return M, npre

            gcnt = {"g": 0}
            avres = [[Res("avr%d_%d" % (i, j)) for j in range(8)] for i in range(2)]

            def gather_group(gi, grp, nxt):
                s, t = tiles[gi]
                i2 = gi % 2
                eix = eidx[i2]; h2i = h2[i2]; ggi = gg[i2]
                pso_ = pso[gi % 2]
                wg = wg8[grp % 2]
                bufs = []
                for j in range(8):
                    hk = grp * 8 + j
                    u_ = uvg[gcnt["g"] % NR]; gcnt["g"] += 1
                    bufs.append(u_)
                    dma(lambda e, u_=u_, hk=hk: e.indirect_dma_start(out=u_[:], out_offset=None, in_=uv_bf, in_offset=bass.IndirectOffsetOnAxis(ap=eix[:, hk:hk + 1], axis=0)), [eix], [u_], u_, eng="pool")
                    op("dve", lambda e, u_=u_, hk=hk: e.scalar_tensor_tensor(out=djunk[:], in0=u_[:, 0:DM], scalar=1.0, in1=h2i[:, :, :].rearrange("p a b -> p (a b)"), op0=ALU.mult, op1=ALU.mult, accum_out=av[:, hk:hk + 1]), [u_, h2i], [avres[grp % 2][j]])
                    if nxt is not None:
                        PRE = 8
                        if hk < PRE:
                            rem = nxt.npre - nxt.i
                            if rem > 0:
                                nxt.pull_n((rem + (PRE - hk) - 1) // (PRE - hk))
                        elif nxt.ndve > 0:
                            dots_left = max(1, 128 - hk)
                            nxt.pull_dve((nxt.ndve + dots_left - 1) // dots_left)
                gs_ = slice(grp * 8, grp * 8 + 8)
                op("act", lambda e: e.activation(out=wg[:], in_=av[:, gs_], func=AF.Gelu), avres[grp % 2], [wg])
                for j in range(8):
                    hk = grp * 8 + j
                    u_ = bufs[j]; d_ = dg[hk % 4]
                    d1_ = dg1[hk % 4]
                    op("act", lambda e, d1_=d1_, j=j: e.activation(out=d1_[:], in_=ident_f[:], func=AF.Copy, scale=wg[:, j:j + 1]), [ident_f, wg], [d1_])
                    op("act", lambda e, d_=d_, d1_=d1_, hk=hk: e.activation(out=d_[:], in_=d1_[:], func=AF.Copy, scale=ggi[:, hk:hk + 1]), [d1_, ggi], [d_])
                    for half in range(2):
                        op("pe", lambda e, d_=d_, u_=u_, half=half, hk=hk: e.matmul(pso_[half][:], lhsT=d_[:], rhs=u_[:, DM + half * 512:DM + (half + 1) * 512], start=(hk == 0), stop=(hk == 127)), [d_, u_], [pso_[half]])

            def final(gi):
                s, t = tiles[gi]
                r0 = s * T_SEQ + t * 128
                xti = xt[gi % 2]; pso_ = pso[gi % 2]; gb = G2b[s % 2]
                for half in range(2):
                    hs = slice(half * 512, (half + 1) * 512)
                    op("dve", lambda e, half=half, hs=hs: e.tensor_tensor(out=tmpf[:, hs], in0=pso_[half][:], in1=gb[:, hs], op=ALU.mult), [pso_[half], gb], [tmpf])
                op("dve", lambda e: e.tensor_tensor(out=xti[:], in0=xti[:], in1=tmpf[:], op=ALU.add), [xti, tmpf], [xti])
                dma(lambda e: e.dma_start(out=out[r0:r0 + 128, :], in_=xti[:]), [xti], [], xti)

            def emit_micro(mo):
                eng, fn, reads, writes, primary = mo
                if eng == "dma":
                    dma(fn, reads, writes, primary)
                else:
                    op(eng, fn, reads, writes)

            class RQ:
                def __init__(self, Mn):
                    self.M, self.npre = Mn; self.i = 0
                    self.ndve = sum(1 for m in self.M if m[0] == "dve")

                def pull_n(self, n):
                    while n > 0 and self.i < len(self.M):
                        mo = self.M[self.i]; self.i += 1
                        emit_micro(mo)
                        if mo[0] == "dve":
                            self.ndve -= 1
                        n -= 1

                def pull_dve(self, k):
                    while k > 0 and self.i < len(self.M):
                        mo = self.M[self.i]; self.i += 1
                        emit_micro(mo)
                        if mo[0] == "dve":
                            k -= 1; self.ndve -= 1

                def drain(self):
                    while self.i < len(self.M):
                        emit_micro(self.M[self.i]); self.i += 1

            RQ(routing_pieces(0)).drain()
            for gi in range(len(tiles)):
                nxt = RQ(routing_pieces(gi + 1)) if gi + 1 < len(tiles) else None
                for grp in range(16):
                    gather_group(gi, grp, nxt)
                if nxt is not None:
                    nxt.drain()
                final(gi)
            S.barrier(); S.flush()
    return nc


def _core_inputs(inp, b0, ns):
    m = {}
    m["x"] = np.ascontiguousarray(inp["x"][b0:b0 + ns].reshape(ns * T_SEQ, DM))
    m["c"] = np.ascontiguousarray(inp["c"][b0:b0 + ns])
    m["pos"] = np.ascontiguousarray(np.asarray(inp["positions"][b0:b0 + ns]).reshape(ns, NT, 128).transpose(0, 2, 1)).astype(np.int32)
    return m


def kernel(**inputs):
    inp = {k: np.asarray(v) for k, v in inputs.items()}
    n_cores = 8
    ns = inp["x"].shape[0] // n_cores
    shared = {}
    shared["inv"] = (np.float32(10000.0) ** (-(np.arange(0, 64, 2, dtype=np.float32) / np.float32(64)))).astype(np.float32).reshape(1, 32)
    for k in ("w_ada", "w_in", "w_branch_a", "w_branch_b", "w_out", "peer_w_q", "peer_u", "peer_v"):
        shared[k] = np.ascontiguousarray(inp[k][0], dtype=np.float32)
    for k in ("b_ada", "norm1_g", "norm2_g", "diff_q_g", "diff_k_g", "dsa_q_g", "dsa_k_g",
              "diff_lam_q1", "diff_lam_k1", "diff_lam_q2", "diff_lam_k2"):
        shared[k] = np.ascontiguousarray(inp[k][0].reshape(1, -1), dtype=np.float32)
    shared["diff_out_g"] = np.ascontiguousarray(inp["diff_out_g"][0].reshape(128, 1), dtype=np.float32)
    shared["subkT"] = np.ascontiguousarray(inp["peer_sub_keys"][0].reshape(16, 128, 128).transpose(2, 0, 1), dtype=np.float32)
    in_maps = []
    for ci in range(n_cores):
        m = dict(shared)
        m.update(_core_inputs(inp, ci * ns, ns))
        in_maps.append(m)
    nc = build(ns)
    res = run_bass_kernel_spmd(nc, in_maps, core_ids=list(range(n_cores)))
    outs = [np.asarray(r["out"]).reshape(ns, T_SEQ, DM) for r in res.results]
    return np.concatenate(outs, axis=0).astype(np.float32)
```
